# Optimizing a Trainium2 kernel written in Bass

```python
import math
import jax
import jax.numpy as jnp
from jax import lax
import numpy as np

D_MODEL = 1024
BATCH = 4
SEQ = 8192
DEPTH = 2

GRID_W = 64
CTX_LEN = 256
SSM_WIDTH = D_MODEL // 4
SSM_GROUP = 16
SSM_GROUPS = SSM_WIDTH // SSM_GROUP
SSM_STATE = 64
SSM_DT_MIN = 1e-3
SSM_DT_MAX = 1e-1
SSM_RE_MAX = -1e-4
DIFF_HEADS = D_MODEL // 256
DIFF_DH = 64
DIFF_WIDTH = DIFF_HEADS * 2 * DIFF_DH
NA_HEADS = D_MODEL // 256
NA_DH = 64
NA_WIDTH = NA_HEADS * NA_DH
NA_WIN_H = 8
NA_WIN_W = 16
ATTN_BLOCK = 128
ROPE_THETA = 10000.0
ROPE_FREQS = DIFF_DH // 4
IN_SPLITS = (SSM_WIDTH, DIFF_WIDTH, DIFF_WIDTH, DIFF_WIDTH, NA_WIDTH, NA_WIDTH, NA_WIDTH, D_MODEL, D_MODEL, D_MODEL)
IN_WIDTH = sum(IN_SPLITS)
PEER_HEADS = 8
PEER_NKEYS = 128
PEER_EXPERTS = PEER_NKEYS ** 2
PEER_DHALF = 128
PEER_TOPK = 16
PEER_CHUNK = 128
RMS_EPS = 1e-6
NEG_INF = -1e30

kernel_name = 'hybrid_s5_diffattn_natten_peer'


def rmsnorm(x, g):
    xf = x.astype(jnp.float32)
    y = xf * lax.rsqrt(jnp.mean(xf * xf, axis=-1, keepdims=True) + RMS_EPS)
    return (y * g.astype(jnp.float32)).astype(x.dtype)


def rope_half(x, ang):
    f = ang.shape[-1]
    cos = jnp.cos(ang).astype(x.dtype)
    sin = jnp.sin(ang).astype(x.dtype)
    x1, x2 = x[..., :f], x[..., f:]
    return jnp.concatenate([x1 * cos - x2 * sin, x2 * cos + x1 * sin], axis=-1)


def rope_2d(x, ang_r, ang_c):
    half = x.shape[-1] // 2
    return jnp.concatenate([rope_half(x[..., :half], ang_r), rope_half(x[..., half:], ang_c)], axis=-1)


def _lin_combine(e1, e2):
    a1, b1 = e1
    a2, b2 = e2
    return a1 * a2, a2 * b1 + b2


def ssm_discretise(lam_re, lam_im, log_step, b_re, b_im):
    lam = lax.complex(jnp.minimum(lam_re.astype(jnp.float32), SSM_RE_MAX), lam_im.astype(jnp.float32))
    lam_dt = lam * jnp.exp(log_step.astype(jnp.float32))[:, None]
    lam_bar = jnp.exp(lam_dt)
    b = lax.complex(b_re.astype(jnp.float32), b_im.astype(jnp.float32))
    b_bar = ((lam_bar - 1.0) / lam)[..., None] * b
    return lam_dt, lam_bar, b_bar


def ssm_scan(u, lam_bar, b_bar, reverse):
    bu = jnp.einsum('blgp,gnp->blgn', u.astype(jnp.complex64), b_bar)
    a = jnp.broadcast_to(lam_bar, (1, u.shape[1]) + lam_bar.shape)
    _, s = lax.associative_scan(_lin_combine, (a, bu), reverse=reverse, axis=1)
    return s


def ssm_glu(y, glu_w, dtype):
    bn, length = y.shape[:2]
    z = jax.nn.gelu(y.reshape(bn, length, SSM_WIDTH)).astype(dtype) @ glu_w
    val, gate = jnp.split(z, 2, axis=-1)
    return val * jax.nn.sigmoid(gate)


def ssm_mixer(u_lat, u_ctx, lam_re, lam_im, log_step, b_re, b_im, c_re, c_im, d, glu_w, with_ctx):
    bn, length, _ = u_lat.shape
    ul = u_lat.astype(jnp.float32).reshape(bn, length, SSM_GROUPS, SSM_GROUP)
    uc = u_ctx.astype(jnp.float32).reshape(bn, u_ctx.shape[1], SSM_GROUPS, SSM_GROUP)
    dd = d.astype(jnp.float32).reshape(SSM_GROUPS, SSM_GROUP)
    y_lat = dd * ul
    y_ctx = dd * uc if with_ctx else None
    for direction, reverse in ((0, False), (1, True)):
        lam_dt, lam_bar, b_bar = ssm_discretise(lam_re[direction], lam_im[direction], log_step[direction],
                                                b_re[direction], b_im[direction])
        cm = lax.complex(c_re[direction].astype(jnp.float32), c_im[direction].astype(jnp.float32))
        s_ctx = ssm_scan(uc, lam_bar, b_bar, reverse)
        s0 = s_ctx[:, 0] if reverse else s_ctx[:, -1]
        steps = (jnp.arange(length, 0, -1) if reverse else jnp.arange(1, length + 1)).astype(jnp.float32)
        carry = jnp.exp(lam_dt[None] * steps[:, None, None])
        s_lat = ssm_scan(ul, lam_bar, b_bar, reverse) + carry[None] * s0[:, None]
        y_lat = y_lat + jnp.einsum('blgn,gpn->blgp', s_lat, cm).real
        if with_ctx:
            y_ctx = y_ctx + jnp.einsum('blgn,gpn->blgp', s_ctx, cm).real
    out_lat = ssm_glu(y_lat, glu_w, u_lat.dtype)
    out_ctx = ssm_glu(y_ctx, glu_w, u_lat.dtype) if with_ctx else None
    return out_lat, out_ctx


def diff_attend(q, k, v, lam):
    bn, nh, _, lq, dh = q.shape
    nb = lq // ATTN_BLOCK
    qb = jnp.moveaxis(q.reshape(bn, nh, 2, nb, ATTN_BLOCK, dh), 3, 0)
    scale = dh ** -0.5

    def block(qi):
        s = jnp.einsum('bhmqd,bhmkd->bhmqk', qi, k).astype(jnp.float32) * scale
        p = jax.nn.softmax(s, axis=-1)
        a = (p[:, :, 0] - lam * p[:, :, 1]).astype(v.dtype)
        return jnp.einsum('bhqk,bhkd->bhqd', a, v)

    o = lax.map(block, qb)
    return jnp.moveaxis(o, 0, 2).reshape(bn, nh, lq, v.shape[-1])


def diff_mixer(q_lat, k_lat, v_lat, q_ctx, k_ctx, v_ctx, lam_qk, subln_g, lam_init, ang_r, ang_c, with_ctx):
    def qk_heads(t):
        return t.reshape(t.shape[0], t.shape[1], DIFF_HEADS, 2, DIFF_DH).transpose(0, 2, 3, 1, 4)

    def v_heads(t):
        return t.reshape(t.shape[0], t.shape[1], DIFF_HEADS, 2 * DIFF_DH).transpose(0, 2, 1, 3)

    def finish(o):
        o = rmsnorm(o, subln_g) * (1.0 - lam_init)
        return o.transpose(0, 2, 1, 3).reshape(o.shape[0], o.shape[2], DIFF_WIDTH)

    lq = lam_qk.astype(jnp.float32)
    lam = jnp.exp(jnp.sum(lq[0] * lq[1])) - jnp.exp(jnp.sum(lq[2] * lq[3])) + lam_init
    ql = rope_2d(qk_heads(q_lat), ang_r, ang_c)
    kl = rope_2d(qk_heads(k_lat), ang_r, ang_c)
    kc = qk_heads(k_ctx)
    vc = v_heads(v_ctx)
    k_all = jnp.concatenate([kc, kl], axis=3)
    v_all = jnp.concatenate([vc, v_heads(v_lat)], axis=2)
    out_lat = finish(diff_attend(ql, k_all, v_all, lam))
    out_ctx = finish(diff_attend(qk_heads(q_ctx), kc, vc, lam)) if with_ctx else None
    return out_lat, out_ctx


def dense_attend(q, k, v):
    s = jnp.einsum('bhqd,bhkd->bhqk', q, k).astype(jnp.float32) * q.shape[-1] ** -0.5
    p = jax.nn.softmax(s, axis=-1).astype(v.dtype)
    return jnp.einsum('bhqk,bhkd->bhqd', p, v)


def na_mixer(q_lat, k_lat, v_lat, q_ctx, k_ctx, v_ctx, rpb, with_ctx):
    bn, length, _ = q_lat.shape
    rows = length // GRID_W
    kh = min(NA_WIN_H, rows)

    def grid_heads(t):
        return t.reshape(bn, rows, GRID_W, NA_HEADS, NA_DH).transpose(0, 3, 1, 2, 4)

    def seq_heads(t):
        return t.reshape(bn, t.shape[1], NA_HEADS, NA_DH).transpose(0, 2, 1, 3)

    qg, kg, vg = grid_heads(q_lat), grid_heads(k_lat), grid_heads(v_lat)
    kc, vc = seq_heads(k_ctx), seq_heads(v_ctx)
    row_idx = np.clip(np.arange(rows) - kh // 2, 0, rows - kh)[:, None] + np.arange(kh)[None, :]
    kw = kg[:, :, row_idx].reshape(bn, NA_HEADS, rows, kh * GRID_W, NA_DH)
    vw = vg[:, :, row_idx].reshape(bn, NA_HEADS, rows, kh * GRID_W, NA_DH)
    cols = np.arange(GRID_W)
    col_start = np.clip(cols - NA_WIN_W // 2, 0, GRID_W - NA_WIN_W)
    col_ok = (cols[None, :] >= col_start[:, None]) & (cols[None, :] < col_start[:, None] + NA_WIN_W)
    mask = np.broadcast_to(col_ok[:, None, :], (GRID_W, kh, GRID_W)).reshape(GRID_W, kh * GRID_W)
    dr_idx = row_idx - np.arange(rows)[:, None] + NA_WIN_H - 1
    dc_idx = np.clip(cols[None, :] - cols[:, None] + NA_WIN_W - 1, 0, 2 * NA_WIN_W - 2)
    bias = rpb[:, dr_idx[:, None, :, None], dc_idx[None, :, None, :]].reshape(NA_HEADS, rows, GRID_W, kh * GRID_W)
    scale = NA_DH ** -0.5
    s_loc = jnp.einsum('bhrqd,bhrkd->bhrqk', qg, kw).astype(jnp.float32) * scale + bias.astype(jnp.float32)
    s_loc = jnp.where(mask, s_loc, NEG_INF)
    s_ctx = jnp.einsum('bhrqd,bhkd->bhrqk', qg, kc).astype(jnp.float32) * scale
    p = jax.nn.softmax(jnp.concatenate([s_loc, s_ctx], axis=-1), axis=-1).astype(vg.dtype)
    nloc = kh * GRID_W
    o = (jnp.einsum('bhrqk,bhrkd->bhrqd', p[..., :nloc], vw)
         + jnp.einsum('bhrqk,bhkd->bhrqd', p[..., nloc:], vc))
    out_lat = o.transpose(0, 2, 3, 1, 4).reshape(bn, length, NA_WIDTH)
    out_ctx = None
    if with_ctx:
        oc = dense_attend(seq_heads(q_ctx), kc, vc)
        out_ctx = oc.transpose(0, 2, 1, 3).reshape(bn, q_ctx.shape[1], NA_WIDTH)
    return out_lat, out_ctx


def merge_branches(y_ssm, y_diff, y_na, g_ssm, g_diff, g_na, w_br_ssm, w_br_diff, w_br_na, w_out):
    m = (jax.nn.sigmoid(g_ssm) * (y_ssm @ w_br_ssm)
         + jax.nn.sigmoid(g_diff) * (y_diff @ w_br_diff)
         + jax.nn.sigmoid(g_na) * (y_na @ w_br_na))
    return m @ w_out


def peer_ffn(h, wq, subkeys, u_tab, v_tab):
    bn, length, dm = h.shape
    nc = length // PEER_CHUNK
    hc = h.reshape(bn, nc, PEER_CHUNK, dm).transpose(1, 0, 2, 3)

    def chunk(hb):
        q = (hb @ wq).reshape(bn, PEER_CHUNK, PEER_HEADS, 2, PEER_DHALF)
        s = jnp.einsum('bchxd,hxnd->bchxn', q, subkeys).astype(jnp.float32)
        sv, si = lax.top_k(s, PEER_TOPK)
        cand = (sv[..., 0, :, None] + sv[..., 1, None, :]).reshape(bn, PEER_CHUNK, PEER_HEADS, PEER_TOPK * PEER_TOPK)
        cidx = (si[..., 0, :, None] * PEER_NKEYS + si[..., 1, None, :]).reshape(bn, PEER_CHUNK, PEER_HEADS, PEER_TOPK * PEER_TOPK)
        best, pos = lax.top_k(cand, PEER_TOPK)
        eidx = jnp.take_along_axis(cidx, pos, axis=-1)
        g = jax.nn.softmax(best, axis=-1).astype(hb.dtype)
        act = jax.nn.gelu(jnp.einsum('bchkd,bcd->bchk', u_tab[eidx], hb))
        return jnp.einsum('bchk,bchkd->bcd', g * act, v_tab[eidx])

    out = lax.map(chunk, hc)
    return out.transpose(1, 0, 2, 3).reshape(bn, length, dm)


def setup_inputs(seed: int = 0) -> dict:
    key = jax.random.key(seed)
    ks = jax.random.split(key, 32)
    f32 = jnp.float32

    def nrm(k, shape, scale):
        return jax.random.normal(k, shape, f32) * scale

    G, N, P = SSM_GROUPS, SSM_STATE, SSM_GROUP
    n_idx = jnp.arange(N, dtype=f32)
    return {
        'x': nrm(ks[0], (BATCH, SEQ, D_MODEL), 1.0),
        'c': nrm(ks[1], (BATCH, D_MODEL), 1.0),
        'ctx': nrm(ks[2], (BATCH, CTX_LEN, D_MODEL), 1.0),
        'c_ctx': nrm(ks[3], (D_MODEL,), 1.0),
        'ada_w': nrm(ks[4], (DEPTH, D_MODEL, 6 * D_MODEL), 0.5 * D_MODEL ** -0.5),
        'ada_b': nrm(ks[5], (DEPTH, 6 * D_MODEL), 0.01),
        'norm1_g': 1.0 + nrm(ks[6], (DEPTH, D_MODEL), 0.01),
        'norm2_g': 1.0 + nrm(ks[7], (DEPTH, D_MODEL), 0.01),
        'w_in': nrm(ks[8], (DEPTH, D_MODEL, IN_WIDTH), D_MODEL ** -0.5),
        'ssm_lambda_re': -0.5 + nrm(ks[9], (DEPTH, 2, G, N), 0.01),
        'ssm_lambda_im': math.pi * n_idx + nrm(ks[10], (DEPTH, 2, G, N), 0.01),
        'ssm_log_step': jax.random.uniform(ks[11], (DEPTH, 2, G), f32, math.log(SSM_DT_MIN), math.log(SSM_DT_MAX)),
        'ssm_b_re': nrm(ks[12], (DEPTH, 2, G, N, P), (2 * P) ** -0.5),
        'ssm_b_im': nrm(ks[13], (DEPTH, 2, G, N, P), (2 * P) ** -0.5),
        'ssm_c_re': nrm(ks[14], (DEPTH, 2, G, P, N), (2 * N) ** -0.5),
        'ssm_c_im': nrm(ks[15], (DEPTH, 2, G, P, N), (2 * N) ** -0.5),
        'ssm_d': nrm(ks[16], (DEPTH, SSM_WIDTH), 1.0),
        'ssm_glu_w': nrm(ks[17], (DEPTH, SSM_WIDTH, 2 * SSM_WIDTH), SSM_WIDTH ** -0.5),
        'diff_lambda': nrm(ks[18], (DEPTH, 4, DIFF_DH), 0.1),
        'diff_subln_g': 1.0 + nrm(ks[19], (DEPTH, 2 * DIFF_DH), 0.01),
        'na_rpb': nrm(ks[20], (DEPTH, NA_HEADS, 2 * NA_WIN_H - 1, 2 * NA_WIN_W - 1), 0.02),
        'w_br_ssm': nrm(ks[21], (DEPTH, SSM_WIDTH, D_MODEL), SSM_WIDTH ** -0.5),
        'w_br_diff': nrm(ks[22], (DEPTH, DIFF_WIDTH, D_MODEL), DIFF_WIDTH ** -0.5),
        'w_br_na': nrm(ks[23], (DEPTH, NA_WIDTH, D_MODEL), NA_WIDTH ** -0.5),
        'w_out': nrm(ks[24], (DEPTH, D_MODEL, D_MODEL), D_MODEL ** -0.5),
        'peer_wq': nrm(ks[25], (DEPTH, D_MODEL, PEER_HEADS * 2 * PEER_DHALF), D_MODEL ** -0.5),
        'peer_subkeys': nrm(ks[26], (DEPTH, PEER_HEADS, 2, PEER_NKEYS, PEER_DHALF), PEER_DHALF ** -0.5),
        'peer_u': nrm(ks[27], (DEPTH, PEER_EXPERTS, D_MODEL), D_MODEL ** -0.5),
        'peer_v': nrm(ks[28], (DEPTH, PEER_EXPERTS, D_MODEL), 0.5),
        'final_norm_g': 1.0 + nrm(ks[29], (D_MODEL,), 0.01),
    }


def reference(x, c, ctx, c_ctx, ada_w, ada_b, norm1_g, norm2_g, w_in, ssm_lambda_re, ssm_lambda_im,
              ssm_log_step, ssm_b_re, ssm_b_im, ssm_c_re, ssm_c_im, ssm_d, ssm_glu_w, diff_lambda,
              diff_subln_g, na_rpb, w_br_ssm, w_br_diff, w_br_na, w_out, peer_wq, peer_subkeys, peer_u,
              peer_v, final_norm_g):
    length = x.shape[1]
    t = jnp.arange(length)
    freqs = ROPE_THETA ** (-jnp.arange(ROPE_FREQS, dtype=jnp.float32) / ROPE_FREQS)
    ang_r = (t // GRID_W).astype(jnp.float32)[:, None] * freqs
    ang_c = (t % GRID_W).astype(jnp.float32)[:, None] * freqs
    split_idx = [int(i) for i in np.cumsum(IN_SPLITS)[:-1]]
    x_lat, x_ctx = x, ctx
    for l in range(DEPTH):
        with_ctx = l < DEPTH - 1
        lam_init = 0.8 - 0.6 * math.exp(-0.3 * l)
        m_lat = (jax.nn.silu(c) @ ada_w[l] + ada_b[l])[:, None, :]
        m_ctx = jax.nn.silu(c_ctx) @ ada_w[l] + ada_b[l]
        sh1, sc1, g1, sh2, sc2, g2 = jnp.split(m_lat, 6, axis=-1)
        csh1, csc1, cg1, csh2, csc2, cg2 = jnp.split(m_ctx, 6, axis=-1)
        h_lat = rmsnorm(x_lat, norm1_g[l]) * (1.0 + sc1) + sh1
        h_ctx = rmsnorm(x_ctx, norm1_g[l]) * (1.0 + csc1) + csh1
        pl = jnp.split(h_lat @ w_in[l], split_idx, axis=-1)
        pc = jnp.split(h_ctx @ w_in[l], split_idx, axis=-1)
        ys_l, ys_c = ssm_mixer(pl[0], pc[0], ssm_lambda_re[l], ssm_lambda_im[l], ssm_log_step[l],
                               ssm_b_re[l], ssm_b_im[l], ssm_c_re[l], ssm_c_im[l], ssm_d[l], ssm_glu_w[l], with_ctx)
        yd_l, yd_c = diff_mixer(pl[1], pl[2], pl[3], pc[1], pc[2], pc[3], diff_lambda[l], diff_subln_g[l],
                                lam_init, ang_r, ang_c, with_ctx)
        yn_l, yn_c = na_mixer(pl[4], pl[5], pl[6], pc[4], pc[5], pc[6], na_rpb[l], with_ctx)
        mix_l = merge_branches(ys_l, yd_l, yn_l, pl[7], pl[8], pl[9],
                               w_br_ssm[l], w_br_diff[l], w_br_na[l], w_out[l])
        x_lat = x_lat + g1 * mix_l
        h2 = rmsnorm(x_lat, norm2_g[l]) * (1.0 + sc2) + sh2
        x_lat = x_lat + g2 * peer_ffn(h2, peer_wq[l], peer_subkeys[l], peer_u[l], peer_v[l])
        if with_ctx:
            mix_c = merge_branches(ys_c, yd_c, yn_c, pc[7], pc[8], pc[9],
                                   w_br_ssm[l], w_br_diff[l], w_br_na[l], w_out[l])
            x_ctx = x_ctx + cg1 * mix_c
            h2c = rmsnorm(x_ctx, norm2_g[l]) * (1.0 + csc2) + csh2
            x_ctx = x_ctx + cg2 * peer_ffn(h2c, peer_wq[l], peer_subkeys[l], peer_u[l], peer_v[l])
    return rmsnorm(x_lat, final_norm_g)
```

```python
import numpy as np
from contextlib import ExitStack
import concourse.bass as bass
import concourse.mybir as mybir
from concourse.bass_utils import run_bass_kernel_spmd

F32 = mybir.dt.float32
F32R = mybir.dt.float32r
BF16 = mybir.dt.bfloat16
AV_BF16 = True
BF_TABLES = True
USE_R = True
MM = F32R if USE_R else F32
U32 = mybir.dt.uint32
I32 = mybir.dt.int32
ALU = mybir.AluOpType
AF = mybir.ActivationFunctionType
AX = mybir.AxisListType

NSLOT = 12
RELAX_SAME = set()
PAIR_SPLIT = True
BAR_LIM = 12000
DEBUG_OUT = set()


class Sched:
    def __init__(self, nc, es):
        self.nc = nc
        self.es = es
        self.eng = {'pe': nc.tensor, 'dve': nc.vector, 'act': nc.scalar, 'pool': nc.gpsimd, 'sp': nc.sync}
        self.sem = {e: es.enter_context(nc.semaphore(f"s_{e}")) for e in self.eng}
        self.cnt = {e: 0 for e in self.eng}
        self.waited = {e: {} for e in self.eng}
        self.dq = {}
        for q in ('sp', 'pool', 'act'):
            self.dq[q] = dict(sems=[es.enter_context(nc.semaphore(f"d_{q}{i}")) for i in range(NSLOT)],
                              uses=[0] * NSLOT, nxt=0)
        self.lastw = {}
        self.readers = {}
        self.scopes = []
        self.nalloc = 0
        self.sem_arrive = es.enter_context(nc.semaphore("s_arrive"))
        self.sem_epoch = es.enter_context(nc.semaphore("s_epoch"))
        self.epoch = 0

    def push(self):
        s = ExitStack()
        self.scopes.append(s)

    def pop(self):
        self.barrier()
        self.scopes.pop().close()

    def _ctx(self):
        return self.scopes[-1] if self.scopes else self.es

    def sb(self, name, shape, dtype=F32):
        self.nalloc += 1
        return self._ctx().enter_context(self.nc.sbuf_tensor(f"{name}_{self.nalloc}", list(shape), dtype))

    def ps(self, name, shape, dtype=F32):
        self.nalloc += 1
        return self._ctx().enter_context(self.nc.psum_tensor(f"{name}_{self.nalloc}", list(shape), dtype))

    def _semof(self, key):
        if isinstance(key, str):
            return self.sem[key]
        return self.dq[key[1]]['sems'][key[2]]

    def _wait(self, e, tok):
        key, val = tok
        if key == e and (e in ('pe', 'sp') or e in RELAX_SAME):
            return
        if self.waited[e].get(key, 0) >= val:
            return
        self.eng[e].wait_ge(self._semof(key), val)
        self.waited[e][key] = val

    def _deps(self, e, reads, writes):
        for r in reads:
            if r in self.lastw:
                self._wait(e, self.lastw[r])
        for w in writes:
            if w in self.lastw:
                self._wait(e, self.lastw[w])
            for k, v in self.readers.get(w, {}).items():
                self._wait(e, (k, v))

    def _commit(self, tok, reads, writes):
        for r in reads:
            d = self.readers.setdefault(r, {})
            if d.get(tok[0], 0) < tok[1]:
                d[tok[0]] = tok[1]
        for w in writes:
            self.lastw[w] = tok
            self.readers[w] = {}

    def op(self, e, fn, reads=(), writes=()):
        self._deps(e, reads, writes)
        ins = fn()
        ins.then_inc(self.sem[e], 1)
        self.cnt[e] += 1
        self._commit((e, self.cnt[e]), reads, writes)

    def dma(self, q, out, in_, reads=(), writes=(), fn=None):
        d = self.dq[q]
        self._deps(q, reads, writes)
        s = d['nxt']
        d['nxt'] = (s + 1) % NSLOT
        if d['uses'][s] > 0:
            self._wait(q, (('dma', q, s), 16 * d['uses'][s]))
        if fn is None:
            ins = self.eng[q].dma_start(out=out, in_=in_)
        else:
            ins = fn()
        d['uses'][s] += 1
        ins.then_inc(d['sems'][s], 16)
        self._commit((('dma', q, s), 16 * d['uses'][s]), reads, writes)

    def _all_tokens(self):
        toks = [(e, c) for e, c in self.cnt.items() if c > 0]
        for q, d in self.dq.items():
            for s in range(NSLOT):
                if d['uses'][s] > 0:
                    toks.append((('dma', q, s), 16 * d['uses'][s]))
        return toks

    def barrier(self, reset=True):
        toks = self._all_tokens()
        if not toks:
            return
        for e in self.eng:
            for t in toks:
                if t[0] != e:
                    self._wait(e, t)
        if not reset:
            return
        for e in self.eng:
            if self.cnt[e] > BAR_LIM:
                self.epoch += 1
                self.sem[e] = self.es.enter_context(self.nc.semaphore(f"s_{e}_{self.epoch}"))
                self.cnt[e] = 0
                for e2 in self.eng:
                    self.waited[e2].pop(e, None)
                for k in list(self.lastw.keys()):
                    if self.lastw[k][0] == e:
                        del self.lastw[k]
                for k, d in self.readers.items():
                    d.pop(e, None)

    def maybe_barrier(self, lim=None):
        if max(self.cnt.values()) > (BAR_LIM if lim is None else lim):
            self.barrier()

    def finish(self):
        self.barrier(reset=False)


D_MODEL = 1024
IN_W = 5632
CTXL = 256
CT = 2
LAM_INIT = [0.8 - 0.6 * float(np.exp(-0.3 * l)) for l in range(8)]


class Cfg:
    def __init__(self, L, depth):
        self.L = L
        self.depth = depth
        self.NT = L // 128
        self.TT = self.NT + CT
        self.R = L // 64


def declare_io(nc, cfg):
    D = {}
    dp = cfg.depth

    def din(name, shape, dt=F32):
        D[name] = nc.dram_tensor(name, list(shape), dt, kind="ExternalInput").ap()

    def dsc(name, shape, dt=F32):
        kind = "ExternalOutput" if name in DEBUG_OUT else "Internal"
        D[name] = nc.dram_tensor(name, list(shape), dt, kind=kind).ap()

    din('x', [cfg.L, 1024]); din('ctx', [CTXL, 1024]); din('cc', [2, 1024])
    din('ada_w', [dp, 1024, 6144]); din('ada_b', [dp, 6144])
    din('norm_g', [dp, 2048])
    din('w_in', [dp, 1024, IN_W])
    din('ident', [128, 128]); din('sel', [2, 256])
    D['out'] = nc.dram_tensor('out', [cfg.L // 2 if PAIR_SPLIT else cfg.L, 1024], F32, kind="ExternalOutput").ap()
    if PAIR_SPLIT:
        din('rowidx', [128, cfg.NT // 2], I32)
    dsc('xres', [cfg.TT * 128, 1024])
    dsc('pl', [cfg.TT * 128, IN_W])
    dsc('qkT', [8, 128, cfg.TT * 128]); dsc('nqkT', [4, 128, cfg.TT * 128])
    dsc('ymix', [cfg.TT * 128, 1024])
    ntypes = len(na_pair_info(cfg.R)[1])
    din('rope', [cfg.L, 64]); din('nab', [dp, 4, ntypes * 5, 128, 128])
    din('diff_lambda', [dp, 256]); din('diff_subln_g', [dp, 128])
    din('jmat', [128, 128]); din('ssm_sc', [dp, 128, 48]); din('ssm_b', [dp, 2, 16, 128, 128]); din('ssm_c', [dp, 2, 16, 128, 128])
    din('ssm_dT', [dp, 128, 2]); din('ssm_glu_w', [dp, 256, 512])
    dsc('ysT', [2, 128, cfg.TT * 128])
    dsc('bcd', [2, 6, 128, 1024])
    if BF_TABLES:
        for i in range(dp):
            dsc(f'uv{i}', [16384, 2048], BF16)
    din('w_br_ssm', [dp, 256, 1024]); din('w_br_diff', [dp, 512, 1024]); din('w_br_na', [dp, 256, 1024]); din('w_out', [dp, 1024, 1024])
    din('peer_wq', [dp, 1024, 2048]); din('peer_sk', [dp, 16, 128, 128]); [din(f'peer_u{i}', [16384, 1024]) for i in range(dp)]; [din(f'peer_v{i}', [16384, 1024]) for i in range(dp)]
    din('final_g', [1, 1024]); din('iota16', [128, 16])
    if 'ydbg' in DEBUG_OUT:
        dsc('ydbg', [2, 128, cfg.TT * 128])
    return D


def xsrc(D, cfg, l, tt):
    if l == 0:
        if tt < CT:
            return D['ctx'][tt * 128:(tt + 1) * 128, :]
        return D['x'][(tt - CT) * 128:(tt - CT + 1) * 128, :]
    return D['xres'][tt * 128:(tt + 1) * 128, :]


def stage_consts(K, nc, D):
    ident = K.sb('ident', [128, 128])
    K.dma('sp', ident[:], D['ident'], writes=['ident'])
    sel = K.sb('sel', [2, 256])
    K.dma('sp', sel[:], D['sel'], writes=['sel'])
    return dict(ident=ident, sel=sel)


def stage_mods(K, nc, D, cfg, l, C, with_ctx):
    bc = {}
    who = ['l', 'c'] if with_ctx else ['l']
    K.push()
    for w in ['l', 'c']:
        for j in ([0, 1, 2, 3, 4, 5] if w in who else [0, 1]):
            bc[(w, j)] = K.sb(f'bc{w}{j}', [128, 1024])
    ident, sel = C['ident'], C['sel']
    cc = K.sb('cc', [2, 1024])
    K.dma('sp', cc[:], D['cc'], writes=['cc'])
    K.op('act', lambda: nc.scalar.activation(cc[:], cc[:], AF.Silu), reads=['cc'], writes=['cc'])
    pT = K.ps('pT', [128, 8, 2])
    for k in range(8):
        K.op('pe', lambda k=k: nc.tensor.transpose(pT[:, k, :], cc[:, k * 128:(k + 1) * 128], ident[0:2, 0:2]),
             reads=['cc', 'ident'], writes=['pT'])
    cT = K.sb('cT', [128, 8, 2])
    K.op('dve', lambda: nc.vector.tensor_copy(cT[:], pT[:]), reads=['pT'], writes=['cT'])
    mods = K.sb('mods', [2, 6144])
    ab = K.sb('ab', [2, 6144])
    K.dma('sp', ab[0:1, :], D['ada_b'][l:l + 1, :], writes=['ab'])
    K.dma('sp', ab[1:2, :], D['ada_b'][l:l + 1, :], writes=['ab'])
    gv = K.sb('gv', [1, 2048])
    K.dma('sp', gv[:], D['norm_g'][l:l + 1, :], writes=['gv'])
    wbuf = [K.sb(f'adaw{i}', [128, 8, 512]) for i in range(2)]
    pm = [K.ps(f'pm{i}', [2, 512]) for i in range(2)]
    for cch in range(12):
        wb = wbuf[cch % 2]
        wk = f'adaw{cch % 2}'
        K.dma('sp', wb[:], D['ada_w'][l, :, cch * 512:(cch + 1) * 512].rearrange("(k p) n -> p k n", p=128), writes=[wk])
        pk = f'pm{cch % 2}'
        for k in range(8):
            K.op('pe', lambda k=k, wb=wb, p=pm[cch % 2]: nc.tensor.matmul(p[:], cT[:, k, :], wb[:, k, :], start=(k == 0), stop=(k == 7)),
                 reads=['cT', wk], writes=[pk])
        K.op('dve', lambda p=pm[cch % 2], cch=cch: nc.vector.tensor_tensor(mods[:, cch * 512:(cch + 1) * 512], p[:], ab[:, cch * 512:(cch + 1) * 512], ALU.add),
             reads=[pk, 'ab'], writes=['mods'])
    for j in (1, 4):
        K.op('dve', lambda j=j: nc.vector.tensor_scalar(mods[:, j * 1024:(j + 1) * 1024], mods[:, j * 1024:(j + 1) * 1024], 1.0, None, ALU.add),
             reads=['mods'], writes=['mods'])
    pb = [K.ps(f'pb{i}', [128, 512]) for i in range(2)]
    gbc = K.sb('gbc', [128, 2048])
    n = 0
    for q in range(4):
        K.op('pe', lambda q=q, p=pb[n % 2]: nc.tensor.matmul(p[:], sel[0:1, 0:128], gv[:, q * 512:(q + 1) * 512], start=True, stop=True),
             reads=['sel', 'gv'], writes=[f'pb{n % 2}'])
        K.op('act', lambda q=q, p=pb[n % 2]: nc.scalar.copy(gbc[:, q * 512:(q + 1) * 512], p[:]), reads=[f'pb{n % 2}'], writes=['gbc'])
        n += 1
    for wi, w in enumerate(['l', 'c']):
        for mj in range(6):
            tj = {0: 1, 1: 0, 2: 2, 3: 4, 4: 3, 5: 5}[mj]
            if (w, tj) not in bc:
                continue
            for hh in range(2):
                K.op('pe', lambda p=pb[n % 2], wi=wi, mj=mj, hh=hh: nc.tensor.matmul(
                    p[:], sel[0:2, wi * 128:(wi + 1) * 128], mods[:, mj * 1024 + hh * 512: mj * 1024 + (hh + 1) * 512], start=True, stop=True),
                    reads=['sel', 'mods'], writes=[f'pb{n % 2}'])
                dst = bc[(w, tj)][:, hh * 512:(hh + 1) * 512]
                if tj in (0, 3):
                    goff = 0 if tj == 0 else 1024
                    K.op('dve', lambda p=pb[n % 2], dst=dst, goff=goff, hh=hh: nc.vector.tensor_tensor(
                        dst, p[:], gbc[:, goff + hh * 512: goff + (hh + 1) * 512], ALU.mult),
                        reads=[f'pb{n % 2}', 'gbc'], writes=[f'bc{w}{tj}'])
                else:
                    K.op('act', lambda p=pb[n % 2], dst=dst: nc.scalar.copy(dst, p[:]), reads=[f'pb{n % 2}'], writes=[f'bc{w}{tj}'])
                n += 1
    for (w, j), t in bc.items():
        K.dma('sp', D['bcd'][0 if w == 'l' else 1, j], t[:], reads=[f'bc{w}{j}'], writes=[('bcd', w, j)])
    K.pop()
    return None


def load_bc(K, D, w, j):
    t = K.sb(f'bc{w}{j}', [128, 1024])
    K.dma('sp', t[:], D['bcd'][0 if w == 'l' else 1, j], writes=[f'bc{w}{j}'])
    return t


def norm_mod(K, nc, xt, xkey, ht, hkey, gm, gmkey, sh, shkey, scr, tag):
    ss, rs, junk = scr
    K.op('act', lambda: nc.scalar.activation(junk[:], xt[:], AF.Square, accum_out=ss[:]), reads=[xkey], writes=[f'junk{tag}', f'ss{tag}'])
    K.op('dve', lambda: nc.vector.tensor_scalar(rs[:], ss[:], 1.0 / 1024.0, 1e-6, ALU.mult, ALU.add), reads=[f'ss{tag}'], writes=[f'rs{tag}'])
    K.op('act', lambda: nc.scalar.activation(rs[:], rs[:], AF.Sqrt), reads=[f'rs{tag}'], writes=[f'rs{tag}'])
    K.op('dve', lambda: nc.vector.reciprocal(rs[:], rs[:]), reads=[f'rs{tag}'], writes=[f'rs{tag}'])
    K.op('dve', lambda: nc.vector.scalar_tensor_tensor(ht[:], xt[:], rs[:], gm[:], ALU.mult, ALU.mult),
         reads=[xkey, f'rs{tag}', gmkey], writes=[hkey])
    K.op('dve', lambda: nc.vector.tensor_tensor(ht[:], ht[:], sh[:], ALU.add), reads=[hkey, shkey], writes=[hkey])


def stage_inproj(K, nc, D, cfg, l, C, bc, with_ctx):
    K.push()
    ident = C['ident']
    bc = {(w, j): load_bc(K, D, w, j) for w in 'lc' for j in (0, 1)}
    NB = 11
    tiles = list(range(cfg.TT))
    hT = K.sb('hT', [128, NB, 8, 128], MM)
    wst = [K.sb(f'wst{i}', [128, 8, 512]) for i in range(2)]
    xt = [K.sb(f'xt{i}', [128, 1024]) for i in range(2)]
    ht = [K.sb(f'ht{i}', [128, 1024]) for i in range(2)]
    scr = [(K.sb(f'ss{i}', [128, 1]), K.sb(f'rs{i}', [128, 1]), K.sb(f'junk{i}', [128, 1024])) for i in range(2)]
    ptr = [K.ps(f'ptr{i}', [128, 1024]) for i in range(2)]
    wb = [K.sb(f'win{i}', [128, 8, 512], MM) for i in range(2)]
    po = [K.ps(f'po{i}', [128, 512]) for i in range(2)]
    ot = [K.sb(f'ot{i}', [128, 512]) for i in range(3)]
    nw = 0
    no = 0
    for b0 in range(0, len(tiles), NB):
        blk = tiles[b0:b0 + NB]
        for bi, tt in enumerate(blk):
            i = tt % 2
            K.dma('sp', xt[i][:], xsrc(D, cfg, l, tt), writes=[f'xt{i}'])
            w = 'c' if tt < CT else 'l'
            norm_mod(K, nc, xt[i], f'xt{i}', ht[i], f'ht{i}', bc[(w, 0)], f'bc{w}0', bc[(w, 1)], f'bc{w}1', scr[i], i)
            for k in range(8):
                K.op('pe', lambda k=k, i=i: nc.tensor.transpose(ptr[i][:, k * 128:(k + 1) * 128], ht[i][:, k * 128:(k + 1) * 128], ident[:]),
                     reads=[f'ht{i}', 'ident'], writes=[f'ptr{i}'])
            K.op('act', lambda i=i, bi=bi: nc.scalar.copy(hT[:, bi, :, :], ptr[i][:].rearrange("p (k t) -> p k t", k=8)),
                 reads=[f'ptr{i}'], writes=[('hT', bi)])
        for cch in range(IN_W // 512):
            wi = nw % 2
            nw += 1
            K.dma('sp', wst[wi][:], D['w_in'][l, :, cch * 512:(cch + 1) * 512].rearrange("(k p) n -> p k n", p=128), writes=[f'wst{wi}'])
            K.op('pool', lambda wi=wi: nc.gpsimd.tensor_copy(wb[wi][:], wst[wi][:]), reads=[f'wst{wi}'], writes=[f'win{wi}'])
            for bi, tt in enumerate(blk):
                pi = no % 2
                oi = no % 3
                no += 1
                for k in range(8):
                    K.op('pe', lambda k=k, bi=bi, pi=pi, wi=wi: nc.tensor.matmul(po[pi][:], hT[:, bi, k, :], wb[wi][:, k, :], start=(k == 0), stop=(k == 7)),
                         reads=[('hT', bi), f'win{wi}'], writes=[f'po{pi}'])
                K.op('act', lambda pi=pi, oi=oi: nc.scalar.copy(ot[oi][:], po[pi][:]), reads=[f'po{pi}'], writes=[f'ot{oi}'])
                K.dma('pool', D['pl'][tt * 128:(tt + 1) * 128, cch * 512:(cch + 1) * 512], ot[oi][:], reads=[f'ot{oi}'], writes=[('pl', tt, cch)])
    K.pop()


def build(L, depth, stages=('mods', 'inproj'), nlayers=None):
    cfg = Cfg(L, depth)
    nc = bass.Bass("TRN2", target_bir_lowering=False)
    D = declare_io(nc, cfg)
    with ExitStack() as es:
        K = Sched(nc, es)
        C = stage_consts(K, nc, D)
        if BF_TABLES and 'peer' in stages:
            stage_tables(K, nc, D, cfg)
        for l in range(depth if nlayers is None else nlayers):
            with_ctx = l < depth - 1
            K.push()
            bc = stage_mods(K, nc, D, cfg, l, C, with_ctx)
            if 'inproj' in stages:
                stage_inproj(K, nc, D, cfg, l, C, bc, with_ctx)
            if 'prepass' in stages:
                stage_prepass(K, nc, D, cfg, l, C, with_ctx)
            if 'diff' in stages:
                stage_diff(K, nc, D, cfg, l, C, with_ctx)
            if 'na' in stages:
                stage_na(K, nc, D, cfg, l, C, with_ctx)
            if 'ssm' in stages:
                stage_ssm(K, nc, D, cfg, l, C, with_ctx)
            if 'merge' in stages:
                stage_merge(K, nc, D, cfg, l, C, with_ctx)
            if 'peer' in stages:
                stage_peer(K, nc, D, cfg, l, C, with_ctx, final=(l == depth - 1))
            K.pop()
        K.finish()
    return nc


def na_pair_info(R):
    types = {}
    pairs = []
    for r in range(0, R, 2):
        base = int(np.clip(r - 4, 0, R - 10))
        ws0 = int(np.clip(r - 4, 0, R - 8))
        ws1 = int(np.clip(r + 1 - 4, 0, R - 8))
        key = (r - base, ws0 - base, ws1 - base)
        if key not in types:
            types[key] = len(types)
        pairs.append((r, base, types[key]))
    return pairs, list(types.keys())


def host_nab(rpb, R):
    pairs, types = na_pair_info(R)
    H = rpb.shape[0]
    cols = np.arange(64)
    col_start = np.clip(cols - 8, 0, 48)
    col_ok = (cols[None, :] >= col_start[:, None]) & (cols[None, :] < col_start[:, None] + 16)
    dc = np.clip(cols[None, :] - cols[:, None] + 15, 0, 30)
    out = np.full((H, len(types) * 5, 128, 128), -30000.0, np.float32)
    for ti, (dr0, w0, w1) in enumerate(types):
        for j in range(5):
            for kk in range(2):
                krel = 2 * j + kk
                for qq in range(2):
                    ws = (w0, w1)[qq]
                    if not (ws <= krel < ws + 8):
                        continue
                    dr = krel - (dr0 + qq) + 7
                    blk = np.where(col_ok, rpb[:, dr][:, dc], np.float32(-30000.0))
                    out[:, ti * 5 + j, kk * 64:(kk + 1) * 64, qq * 64:(qq + 1) * 64] = np.transpose(blk, (0, 2, 1))
    return out


def host_rope(L):
    t = np.arange(L)
    freqs = (10000.0 ** (-np.arange(16, dtype=np.float32) / 16)).astype(np.float32)
    ang_r = (t // 64).astype(np.float32)[:, None] * freqs
    ang_c = (t % 64).astype(np.float32)[:, None] * freqs
    ang = np.concatenate([ang_r, ang_c], 1).astype(np.float32)
    return np.concatenate([np.cos(ang), np.sin(ang)], 1).astype(np.float32)


def stage_prepass(K, nc, D, cfg, l, C, with_ctx):
    K.push()
    ident = C['ident']
    qk = [K.sb(f'qk{i}', [128, 1024]) for i in range(2)]
    qr = [K.sb(f'qr{i}', [128, 1024]) for i in range(2)]
    cs = [K.sb(f'cs{i}', [128, 64]) for i in range(2)]
    tmp = [K.sb(f'rt{i}', [128, 512]) for i in range(4)]
    nqk = [K.sb(f'nqk{i}', [128, 512]) for i in range(2)]
    pq = [K.ps(f'pq{i}', [128, 1024]) for i in range(2)]
    pn = [K.ps(f'pn{i}', [128, 512]) for i in range(2)]
    sq = [K.sb(f'sq{i}', [128, 1024]) for i in range(2)]
    sn = [K.sb(f'sn{i}', [128, 512]) for i in range(2)]
    for tt in range(cfg.TT):
        i = tt % 2
        rows = slice(tt * 128, (tt + 1) * 128)
        K.dma('sp', qk[i][:], D['pl'][rows, 256:1280], writes=[f'qk{i}'])
        K.dma('sp', nqk[i][:], D['pl'][rows, 1792:2304], writes=[f'nqk{i}'])
        src = qk[i]
        skey = f'qk{i}'
        if tt >= CT:
            K.dma('sp', cs[i][:], D['rope'][(tt - CT) * 128:(tt - CT + 1) * 128, :], writes=[f'cs{i}'])
            v = qk[i][:].rearrange("p (g rc x f) -> p g rc x f", g=16, rc=2, x=2, f=16)
            o = qr[i][:].rearrange("p (g rc x f) -> p g rc x f", g=16, rc=2, x=2, f=16)
            x1, x2 = v[:, :, :, 0, :], v[:, :, :, 1, :]
            cosb = cs[i][:, 0:32].rearrange("p (rc f) -> p rc f", rc=2).unsqueeze(1).to_broadcast([128, 16, 2, 16])
            sinb = cs[i][:, 32:64].rearrange("p (rc f) -> p rc f", rc=2).unsqueeze(1).to_broadcast([128, 16, 2, 16])
            tv = [t[:].rearrange("p (g rc f) -> p g rc f", g=16, rc=2, f=16) for t in tmp]
            rk = [skey, f'cs{i}']
            K.op('dve', lambda: nc.vector.tensor_tensor(tv[0], x1, cosb, ALU.mult), reads=rk, writes=['rt0'])
            K.op('dve', lambda: nc.vector.tensor_tensor(tv[1], x2, sinb, ALU.mult), reads=rk, writes=['rt1'])
            K.op('pool', lambda: nc.gpsimd.tensor_tensor(tv[2], x2, cosb, ALU.mult), reads=rk, writes=['rt2'])
            K.op('pool', lambda: nc.gpsimd.tensor_tensor(tv[3], x1, sinb, ALU.mult), reads=rk, writes=['rt3'])
            K.op('dve', lambda: nc.vector.tensor_tensor(o[:, :, :, 0, :], tv[0], tv[1], ALU.subtract), reads=['rt0', 'rt1'], writes=[f'qr{i}'])
            K.op('pool', lambda: nc.gpsimd.tensor_tensor(o[:, :, :, 1, :], tv[2], tv[3], ALU.add), reads=['rt2', 'rt3'], writes=[f'qr{i}'])
            src = qr[i]
            skey = f'qr{i}'
        for k in range(8):
            K.op('pe', lambda k=k, src=src: nc.tensor.transpose(pq[i][:, k * 128:(k + 1) * 128], src[:, k * 128:(k + 1) * 128], ident[:]),
                 reads=[skey, 'ident'], writes=[f'pq{i}'])
        K.op('act', lambda: nc.scalar.copy(sq[i][:], pq[i][:]), reads=[f'pq{i}'], writes=[f'sq{i}'])
        K.dma('pool', D['qkT'][:, :, rows].rearrange("b p t -> p b t"), sq[i][:].rearrange("p (b t) -> p b t", b=8),
              reads=[f'sq{i}'], writes=[('qkT', tt)])
        for k in range(4):
            K.op('pe', lambda k=k: nc.tensor.transpose(pn[i][:, k * 128:(k + 1) * 128], nqk[i][:, k * 128:(k + 1) * 128], ident[:]),
                 reads=[f'nqk{i}', 'ident'], writes=[f'pn{i}'])
        K.op('act', lambda: nc.scalar.copy(sn[i][:], pn[i][:]), reads=[f'pn{i}'], writes=[f'sn{i}'])
        K.dma('pool', D['nqkT'][:, :, rows].rearrange("b p t -> p b t"), sn[i][:].rearrange("p (b t) -> p b t", b=4),
              reads=[f'sn{i}'], writes=[('nqkT', tt)])
    K.pop()


def bcast_row(K, nc, C, row, rowkey, dst, dstkey, ncols, ps, pskey, scale=None):
    sel = C['sel']
    K.op('pe', lambda: nc.tensor.matmul(ps[:, 0:ncols], sel[0:1, 0:128], row, start=True, stop=True), reads=['sel', rowkey], writes=[pskey])
    if scale is None:
        K.op('dve', lambda: nc.vector.tensor_copy(dst, ps[:, 0:ncols]), reads=[pskey], writes=[dstkey])
    else:
        K.op('dve', lambda: nc.vector.tensor_scalar(dst, ps[:, 0:ncols], float(scale), None, ALU.mult), reads=[pskey], writes=[dstkey])


def stage_diff(K, nc, D, cfg, l, C, with_ctx):
    K.push()
    TT, NT = cfg.TT, cfg.NT
    NTOK = TT * 128
    lam_init = LAM_INIT[l]
    dl = K.sb('dl', [1, 256])
    K.dma('sp', dl[:], D['diff_lambda'][l:l + 1, :], writes=['dl'])
    pr = K.sb('pr', [1, 2, 64])
    dv = dl[:].rearrange("o (a b f) -> o a b f", a=2, b=2, f=64)
    K.op('dve', lambda: nc.vector.tensor_tensor(pr[:], dv[:, :, 0, :], dv[:, :, 1, :], ALU.mult), reads=['dl'], writes=['pr'])
    e2 = K.sb('e2', [1, 2])
    K.op('dve', lambda: nc.vector.reduce_sum(e2[:], pr[:], axis=AX.X), reads=['pr'], writes=['e2'])
    K.op('act', lambda: nc.scalar.activation(e2[:], e2[:], AF.Exp), reads=['e2'], writes=['e2'])
    nl = K.sb('nl', [1, 1])
    K.op('dve', lambda: nc.vector.tensor_tensor(nl[:], e2[:, 1:2], e2[:, 0:1], ALU.subtract), reads=['e2'], writes=['nl'])
    K.op('dve', lambda: nc.vector.tensor_scalar(nl[:], nl[:], -lam_init, None, ALU.add), reads=['nl'], writes=['nl'])
    psm = K.ps('psm', [128, 512])
    neglam = K.sb('neglam', [128, 1])
    bcast_row(K, nc, C, nl[:], 'nl', neglam[:], 'neglam', 1, psm, 'psm')
    sg = K.sb('sg', [1, 128])
    K.dma('sp', sg[:], D['diff_subln_g'][l:l + 1, :], writes=['sg'])
    gbc = K.sb('dgbc', [128, 128])
    bcast_row(K, nc, C, sg[:], 'sg', gbc[:], 'dgbc', 128, psm, 'psm', scale=1.0 - lam_init)
    qT = K.sb('dqT', [128, NTOK], MM)
    kT = K.sb('dkT', [128, NTOK], MM)
    va = K.sb('dva', [128, TT, 130], BF16 if AV_BF16 else MM)
    stg = [K.sb(f'dstg{i}', [128, 768]) for i in range(2)]
    nstg = [0]
    onez = K.sb('onez', [128, TT, 2])
    K.op('pool', lambda: nc.gpsimd.memset(onez[:, :, 0:1], 1.0), writes=['onez'])
    K.op('pool', lambda: nc.gpsimd.memset(onez[:, :, 1:2], 0.0), writes=['onez'])

    def load_round(dst_ap, src_ap, key, ncols):
        si = nstg[0] % 2
        nstg[0] += 1
        K.dma('sp', stg[si][:, 0:ncols], src_ap, writes=[f'dstg{si}'])
        eng = 'dve' if si == 0 else 'pool'
        cp = nc.vector.tensor_copy if si == 0 else nc.gpsimd.tensor_copy
        K.op(eng, lambda: cp(dst_ap, stg[si][:, 0:ncols]), reads=[f'dstg{si}'], writes=[key])
    st = [K.ps(f'dst{i}', [128, 512]) for i in range(2)]
    pt = [K.sb(f'dpt{i}', [128, 512], BF16 if AV_BF16 else MM) for i in range(2)]
    acc = [K.ps(f'dacc{i}', [128, 512]) for i in range(4)]
    om = [K.sb(f'dom{i}', [128, 4, 128]) for i in range(2)]
    rec = K.sb('drec', [128, 4])
    dd = K.sb('ddd', [128, 4, 128])
    yy = K.sb('dyy', [128, 4, 128])
    ss = K.sb('dss', [128, 4])
    junk = K.sb('djunk', [128, 128])
    n = 0
    for h in range(4):
        for c0 in range(0, NTOK, 768):
            c1 = min(NTOK, c0 + 768)
            load_round(qT[:, c0:c1], D['qkT'][h, :, c0:c1], 'dqT', c1 - c0)
            load_round(kT[:, c0:c1], D['qkT'][4 + h, :, c0:c1], 'dkT', c1 - c0)
        for t0 in range(0, TT, 6):
            t1 = min(TT, t0 + 6)
            si = nstg[0] % 2
            nstg[0] += 1
            sv_ = stg[si][:, 0:(t1 - t0) * 128].rearrange("p (t c) -> p t c", c=128)
            K.dma('sp', sv_, D['pl'][t0 * 128:t1 * 128, 1280 + h * 128:1280 + (h + 1) * 128].rearrange("(t p) c -> p t c", p=128), writes=[f'dstg{si}'])
            if si == 0:
                K.op('dve', lambda: nc.vector.tensor_copy(va[:, t0:t1, 0:128], sv_), reads=[f'dstg{si}'], writes=['dva'])
            else:
                K.op('pool', lambda: nc.gpsimd.tensor_copy(va[:, t0:t1, 0:128], sv_), reads=[f'dstg{si}'], writes=['dva'])
        K.op('dve', lambda: nc.vector.tensor_copy(va[:, :, 128:130], onez[:]), reads=['onez'], writes=['dva'])
        blocks = [(CT * 128 + qb * 512, 512, list(range(TT))) for qb in range(NT // 4)]
        if with_ctx:
            blocks.append((0, 256, [0, 1]))
        for (q0, N, kts) in blocks:
            nq = N // 128
            for m in range(2):
                ms = slice(m * 64, (m + 1) * 64)

                def S(kt, n):
                    K.op('pe', lambda: nc.tensor.matmul(st[n % 2][:, 0:N], kT[ms, kt * 128:(kt + 1) * 128], qT[ms, q0:q0 + N], start=True, stop=True),
                         reads=['dkT', 'dqT'], writes=[f'dst{n % 2}'])
                    K.op('act', lambda: nc.scalar.activation(pt[n % 2][:, 0:N], st[n % 2][:, 0:N], AF.Exp, scale=0.125),
                         reads=[f'dst{n % 2}'], writes=[f'dpt{n % 2}'])

                def AV(kt, n, first, last):
                    for qs in range(nq):
                        K.op('pe', lambda qs=qs: nc.tensor.matmul(acc[qs][:, 0:130], pt[n % 2][:, qs * 128:(qs + 1) * 128], va[:, kt, :], start=first, stop=last),
                             reads=[f'dpt{n % 2}', 'dva'], writes=[f'dacc{qs}'])
                S(kts[0], n)
                for ki, kt in enumerate(kts):
                    if ki + 1 < len(kts):
                        S(kts[ki + 1], n + 1)
                    AV(kt, n, ki == 0, ki == len(kts) - 1)
                    n += 1
                for qs in range(nq):
                    K.op('dve', lambda qs=qs: nc.vector.reciprocal(rec[:, qs:qs + 1], acc[qs][:, 128:129]), reads=[f'dacc{qs}'], writes=['drec'])
                    K.op('dve', lambda qs=qs: nc.vector.tensor_scalar(om[m][:, qs, :], acc[qs][:, 0:128], rec[:, qs:qs + 1], None, ALU.mult),
                         reads=[f'dacc{qs}', 'drec'], writes=[f'dom{m}'])
            for qs in range(nq):
                K.op('dve', lambda qs=qs: nc.vector.scalar_tensor_tensor(dd[:, qs, :], om[1][:, qs, :], neglam[:], om[0][:, qs, :], ALU.mult, ALU.add),
                     reads=['dom0', 'dom1', 'neglam'], writes=['ddd'])
                K.op('act', lambda qs=qs: nc.scalar.activation(junk[:], dd[:, qs, :], AF.Square, accum_out=ss[:, qs:qs + 1]), reads=['ddd'], writes=['djunk', 'dss'])
            K.op('dve', lambda: nc.vector.tensor_scalar(ss[:, 0:nq], ss[:, 0:nq], 1.0 / 128.0, 1e-6, ALU.mult, ALU.add), reads=['dss'], writes=['dss'])
            K.op('act', lambda: nc.scalar.activation(ss[:, 0:nq], ss[:, 0:nq], AF.Sqrt), reads=['dss'], writes=['dss'])
            K.op('dve', lambda: nc.vector.reciprocal(ss[:, 0:nq], ss[:, 0:nq]), reads=['dss'], writes=['dss'])
            for qs in range(nq):
                K.op('dve', lambda qs=qs: nc.vector.scalar_tensor_tensor(yy[:, qs, :], dd[:, qs, :], ss[:, qs:qs + 1], gbc[:], ALU.mult, ALU.mult),
                     reads=['ddd', 'dss', 'dgbc'], writes=['dyy'])
            K.dma('pool', D['ymix'][q0:q0 + N, 256 + h * 128:256 + (h + 1) * 128].rearrange("(s p) c -> p s c", p=128), yy[:, 0:nq, :],
                  reads=['dyy'], writes=[('ymix', 'd', h, q0)])
            K.maybe_barrier()
    K.pop()


def stage_na(K, nc, D, cfg, l, C, with_ctx):
    K.push()
    TT, NT, R = cfg.TT, cfg.NT, cfg.R
    NTOK = TT * 128
    pairs, types = na_pair_info(R)
    ident = C['ident']
    id8 = K.sb('id8', [128, 128])
    K.op('dve', lambda: nc.vector.tensor_scalar(id8[:], ident[:], 8.0, None, ALU.mult), reads=['ident'], writes=['id8'])
    nq = K.sb('nq', [64, NTOK])
    nk = K.sb('nk', [64, NTOK])
    nv = K.sb('nv', [128, TT, 65])
    nab = K.sb('nab', [128, len(types) * 5, 128])
    S = [K.ps(f'nS{i}', [128, 1024]) for i in range(2)]
    P = [K.sb(f'nP{i}', [128, 1024]) for i in range(2)]
    acc = [K.ps(f'nacc{i}', [128, 512]) for i in range(2)]
    rec = K.sb('nrec', [128, 1])
    ysb = [K.sb(f'nys{i}', [128, 64]) for i in range(2)]
    n = 0
    for h in range(4):
        K.dma('sp', nq[:], D['nqkT'][h // 2, (h % 2) * 64:(h % 2 + 1) * 64, :], writes=['nq'])
        K.dma('sp', nk[:], D['nqkT'][2 + h // 2, (h % 2) * 64:(h % 2 + 1) * 64, :], writes=['nk'])
        for t0 in range(0, TT, 6):
            t1 = min(TT, t0 + 6)
            K.dma('sp', nv[:, t0:t1, 0:64], D['pl'][t0 * 128:t1 * 128, 2304 + h * 64:2304 + (h + 1) * 64].rearrange("(t p) c -> p t c", p=128), writes=['nv'])
        K.op('pool', lambda: nc.gpsimd.memset(nv[:, :, 64:65], 1.0), writes=['nv'])
        K.dma('sp', nab[:], D['nab'][l, h].rearrange("j k q -> k j q"), writes=['nab'])
        jobs = []
        for (r, base, ty) in pairs:
            tl = [(CT + (base + 2 * j) // 2, ty * 5 + j) for j in range(5)] + [(0, None), (1, None)]
            jobs.append((CT * 128 + r * 64, tl))
        if with_ctx:
            for qt in range(2):
                jobs.append((qt * 128, [(0, None), (1, None)]))
        for (q0, tl) in jobs:
            i = n % 2
            n += 1
            nt = len(tl)
            for j, (kt, bj) in enumerate(tl):
                K.op('pe', lambda j=j, kt=kt, bj=bj: nc.tensor.matmul(S[i][:, j * 128:(j + 1) * 128], nk[:, kt * 128:(kt + 1) * 128], nq[:, q0:q0 + 128],
                                                                  start=True, stop=(bj is None)),
                     reads=['nk', 'nq'], writes=[f'nS{i}'])
                if bj is not None:
                    K.op('pe', lambda j=j, bj=bj: nc.tensor.matmul(S[i][:, j * 128:(j + 1) * 128], id8[:], nab[:, bj, :], start=False, stop=True),
                         reads=['id8', 'nab'], writes=[f'nS{i}'])
            for c0 in range(0, nt * 128, 512):
                c1 = min(nt * 128, c0 + 512)
                K.op('act', lambda c0=c0, c1=c1: nc.scalar.activation(P[i][:, c0:c1], S[i][:, c0:c1], AF.Exp, scale=0.125),
                     reads=[f'nS{i}'], writes=[f'nP{i}'])
            for j, (kt, bj) in enumerate(tl):
                K.op('pe', lambda j=j, kt=kt: nc.tensor.matmul(acc[i][:, 0:65], P[i][:, j * 128:(j + 1) * 128], nv[:, kt, :], start=(j == 0), stop=(j == nt - 1)),
                     reads=[f'nP{i}', 'nv'], writes=[f'nacc{i}'])
            K.op('dve', lambda: nc.vector.reciprocal(rec[:], acc[i][:, 64:65]), reads=[f'nacc{i}'], writes=['nrec'])
            K.op('dve', lambda: nc.vector.tensor_scalar(ysb[i][:], acc[i][:, 0:64], rec[:], None, ALU.mult), reads=[f'nacc{i}', 'nrec'], writes=[f'nys{i}'])
            K.dma('pool', D['ymix'][q0:q0 + 128, 768 + h * 64:768 + (h + 1) * 64], ysb[i][:], reads=[f'nys{i}'], writes=[('ymix', 'n', h, q0)])
    K.pop()


def core_inputs(inp, b, L, depth, half=0):
    R = L // 64
    sel = np.zeros((2, 256), np.float32)
    sel[0, :128] = 1
    sel[1, 128:] = 1
    f = lambda a: np.ascontiguousarray(a, dtype=np.float32)
    hs = [host_ssm(inp, l) for l in range(depth)]
    im = dict(
        x=inp['x'][b, :L], ctx=inp['ctx'][b], cc=np.stack([inp['c'][b], inp['c_ctx']]),
        ada_w=inp['ada_w'][:depth], ada_b=inp['ada_b'][:depth],
        norm_g=np.concatenate([inp['norm1_g'], inp['norm2_g']], 1)[:depth],
        w_in=inp['w_in'][:depth], ident=np.eye(128, dtype=np.float32), sel=sel,
        rope=host_rope(L), nab=np.stack([host_nab(inp['na_rpb'][l], R) for l in range(depth)]),
        diff_lambda=inp['diff_lambda'][:depth].reshape(depth, 256), diff_subln_g=inp['diff_subln_g'][:depth],
        jmat=np.eye(128, dtype=np.float32)[::-1],
        ssm_sc=np.stack([hs[l][0] for l in range(depth)]), ssm_b=np.stack([hs[l][1] for l in range(depth)]),
        ssm_c=np.stack([hs[l][2] for l in range(depth)]),
        ssm_dT=np.stack([inp['ssm_d'][l].reshape(2, 128).T for l in range(depth)]), ssm_glu_w=inp['ssm_glu_w'][:depth],
        w_br_ssm=inp['w_br_ssm'][:depth], w_br_diff=inp['w_br_diff'][:depth], w_br_na=inp['w_br_na'][:depth], w_out=inp['w_out'][:depth],
        peer_wq=inp['peer_wq'][:depth], peer_sk=inp['peer_subkeys'][:depth].reshape(depth, 16, 128, 128),
        final_g=inp['final_norm_g'].reshape(1, 1024),
        iota16=np.tile(np.arange(16, dtype=np.float32), (128, 1)),
    )
    out = {k: f(v) for k, v in im.items()}
    if PAIR_SPLIT:
        nt2 = (L // 128) // 2
        out['rowidx'] = np.ascontiguousarray((CT * 128 + half * (L // 2) + np.arange(nt2)[None, :] * 128 + np.arange(128)[:, None]).astype(np.int32))
    im = {}
    for i in range(depth):
        im[f'peer_u{i}'] = inp['peer_u'][i]
        im[f'peer_v{i}'] = inp['peer_v'][i]
    out.update({k: f(v) for k, v in im.items()})
    return out


TWO_PI = float(2 * np.pi)


def host_ssm(inp, l):
    lre, lim, ls = inp['ssm_lambda_re'][l], inp['ssm_lambda_im'][l], inp['ssm_log_step'][l]
    sc = np.zeros((128, 48), np.float32)
    bb = np.zeros((2, 16, 128, 128), np.float32)
    cm = np.zeros((2, 16, 128, 128), np.float32)
    for d in range(2):
        for st in range(8):
            col = (d * 8 + st) * 3
            for gl in range(2):
                g = 2 * st + gl
                rows = slice(gl * 64, (gl + 1) * 64)
                sc[rows, col + 0] = lre[d, g]
                sc[rows, col + 1] = lim[d, g]
                sc[rows, col + 2] = ls[d, g]
                c0 = (g - 8 * (st // 4)) * 16
                bb[0, d * 8 + st, rows, c0:c0 + 16] = inp['ssm_b_re'][l, d, g]
                bb[1, d * 8 + st, rows, c0:c0 + 16] = inp['ssm_b_im'][l, d, g]
                cm[0, d * 8 + st, rows, c0:c0 + 16] = inp['ssm_c_re'][l, d, g].T
                cm[1, d * 8 + st, rows, c0:c0 + 16] = inp['ssm_c_im'][l, d, g].T
    return sc, bb, cm


def gelu_tanh(K, nc, eng_a, out, x, xkey, outkey, t1, t1key):
    K.op('dve', lambda: nc.vector.tensor_tensor(t1, x, x, ALU.mult), reads=[xkey], writes=[t1key])
    K.op('dve', lambda: nc.vector.tensor_scalar(t1, t1, 0.044715, 1.0, ALU.mult, ALU.add), reads=[t1key], writes=[t1key])
    K.op('dve', lambda: nc.vector.tensor_tensor(t1, t1, x, ALU.mult), reads=[t1key, xkey], writes=[t1key])
    K.op('act', lambda: nc.scalar.activation(t1, t1, AF.Sigmoid, scale=1.5957691216057308), reads=[t1key], writes=[t1key])
    K.op('dve', lambda: nc.vector.tensor_tensor(out, x, t1, ALU.mult), reads=[xkey, t1key], writes=[outkey])


def stage_ssm(K, nc, D, cfg, l, C, with_ctx):
    K.push()
    TT, NT = cfg.TT, cfg.NT
    ident = C['ident']
    jm = K.sb('jm', [128, 128])
    K.dma('sp', jm[:], D['jmat'], writes=['jm'])
    T = 512
    sc = K.sb('ssc', [128, 16, 3])
    K.dma('sp', sc[:], D['ssm_sc'][l].rearrange("p (c j) -> p c j", j=3), writes=['ssc'])
    names = ['dtv', 'lre', 'a', 'th', 'rho', 'cos', 'sin', 'x', 'den', 'cfr', 'cfi', 'k', 'tmp', 'tmp2', 'nsin', 'ncfi']
    V = {nm: K.sb('sv_' + nm, [128, 16]) for nm in names}
    ki = K.sb('sv_ki', [128, 16], I32)
    lim = sc[:, :, 1]

    def dv(fn, reads, writes):
        K.op('dve', fn, reads=['sv_' + r if r != 'ssc' else r for r in reads], writes=['sv_' + w for w in writes])

    SV = ['svall', 'ssc']

    def d1(fn):
        K.op('dve', fn, reads=SV, writes=['svall'])

    def tt(o, a_, b_, op):
        d1(lambda: nc.vector.tensor_tensor(o, a_, b_, op))

    def ts(o, a_, s1, s2, op0, op1=None):
        if op1 is None:
            d1(lambda: nc.vector.tensor_scalar(o, a_, s1, None, op0))
        else:
            d1(lambda: nc.vector.tensor_scalar(o, a_, s1, s2, op0, op1))

    def to_int_float(dst, src):
        d1(lambda: nc.vector.tensor_copy(ki[:], src))
        d1(lambda: nc.vector.tensor_copy(dst, ki[:]))

    def horner(dst, z, coefs):
        ts(dst, z, float(coefs[-1]), 1.0, ALU.mult, ALU.add)
        for cf in reversed(coefs[:-1]):
            tt(dst, dst, z, ALU.mult)
            ts(dst, dst, float(cf), 1.0, ALU.mult, ALU.add)

    x_, k_, t_, t2_ = V['x'][:], V['k'][:], V['tmp'][:], V['tmp2'][:]
    ts(x_, sc[:, :, 2], 1.0 / 0.6931471805599453, None, ALU.mult)
    to_int_float(k_, x_)
    ts(t_, k_, -0.693359375, None, ALU.mult)
    tt(x_, sc[:, :, 2], t_, ALU.add)
    ts(t_, k_, 2.12194440e-4, None, ALU.mult)
    tt(x_, x_, t_, ALU.add)
    horner(V['dtv'][:], x_, [1.0 / i for i in range(1, 14)])
    for j in range(1, 17):
        ts(t_, k_, float(-j), -0.5, ALU.is_le, ALU.mult)
        ts(t_, t_, 1.0, None, ALU.add)
        tt(V['dtv'][:], V['dtv'][:], t_, ALU.mult)
    ts(V['lre'][:], sc[:, :, 0], -1e-4, None, ALU.min)
    tt(V['a'][:], V['lre'][:], V['dtv'][:], ALU.mult)
    tt(V['th'][:], lim, V['dtv'][:], ALU.mult)
    horner(V['rho'][:], V['a'][:], [1.0 / i for i in range(1, 10)])
    ts(x_, V['th'][:], 1.0 / TWO_PI, None, ALU.mult)
    to_int_float(k_, x_)
    ts(t_, k_, -6.28125, None, ALU.mult)
    tt(x_, V['th'][:], t_, ALU.add)
    ts(t_, k_, -1.9353071795864769e-3, None, ALU.mult)
    tt(x_, x_, t_, ALU.add)
    for sgn, cmpop, thr in ((-1.0, ALU.is_gt, float(np.pi)), (1.0, ALU.is_lt, -float(np.pi))):
        ts(t_, x_, thr, None, cmpop)
        ts(t2_, t_, sgn * 6.28125, None, ALU.mult)
        tt(x_, x_, t2_, ALU.add)
        ts(t2_, t_, sgn * 1.9353071795864769e-3, None, ALU.mult)
        tt(x_, x_, t2_, ALU.add)
    ts(x_, x_, 0.25, None, ALU.mult)
    tt(k_, x_, x_, ALU.mult)
    horner(V['cos'][:], k_, [-1.0 / ((2 * i) * (2 * i - 1)) for i in range(1, 9)])
    horner(V['sin'][:], k_, [-1.0 / ((2 * i) * (2 * i + 1)) for i in range(1, 9)])
    tt(V['sin'][:], V['sin'][:], x_, ALU.mult)
    for _ in range(2):
        tt(t_, V['cos'][:], V['cos'][:], ALU.mult)
        tt(t2_, V['sin'][:], V['sin'][:], ALU.mult)
        tt(V['sin'][:], V['sin'][:], V['cos'][:], ALU.mult)
        ts(V['sin'][:], V['sin'][:], 2.0, None, ALU.mult)
        tt(V['cos'][:], t_, t2_, ALU.subtract)
    tt(t_, V['cos'][:], V['cos'][:], ALU.mult)
    tt(t2_, V['sin'][:], V['sin'][:], ALU.mult)
    tt(t_, t_, t2_, ALU.add)
    ts(t_, t_, -0.5, 1.5, ALU.mult, ALU.add)
    tt(V['cos'][:], V['cos'][:], t_, ALU.mult)
    tt(V['sin'][:], V['sin'][:], t_, ALU.mult)

    def dv(fn, reads, writes):
        K.op('dve', fn, reads=SV, writes=['svall'])

    dv(lambda: nc.vector.tensor_tensor(V['tmp'][:], V['rho'][:], V['cos'][:], ALU.mult), ['rho', 'cos'], ['tmp'])
    dv(lambda: nc.vector.tensor_scalar(V['tmp'][:], V['tmp'][:], -1.0, None, ALU.add), ['tmp'], ['tmp'])
    dv(lambda: nc.vector.tensor_tensor(V['tmp2'][:], V['rho'][:], V['sin'][:], ALU.mult), ['rho', 'sin'], ['tmp2'])
    dv(lambda: nc.vector.tensor_tensor(V['den'][:], V['lre'][:], V['lre'][:], ALU.mult), ['lre'], ['den'])
    dv(lambda: nc.vector.tensor_tensor(V['k'][:], lim, lim, ALU.mult), ['ssc'], ['k'])
    dv(lambda: nc.vector.tensor_tensor(V['den'][:], V['den'][:], V['k'][:], ALU.add), ['den', 'k'], ['den'])
    dv(lambda: nc.vector.reciprocal(V['den'][:], V['den'][:]), ['den'], ['den'])
    dv(lambda: nc.vector.tensor_tensor(V['cfr'][:], V['tmp'][:], V['lre'][:], ALU.mult), ['tmp', 'lre'], ['cfr'])
    dv(lambda: nc.vector.tensor_tensor(V['k'][:], V['tmp2'][:], lim, ALU.mult), ['tmp2', 'ssc'], ['k'])
    dv(lambda: nc.vector.tensor_tensor(V['cfr'][:], V['cfr'][:], V['k'][:], ALU.add), ['cfr', 'k'], ['cfr'])
    dv(lambda: nc.vector.tensor_tensor(V['cfr'][:], V['cfr'][:], V['den'][:], ALU.mult), ['cfr', 'den'], ['cfr'])
    dv(lambda: nc.vector.tensor_tensor(V['cfi'][:], V['tmp2'][:], V['lre'][:], ALU.mult), ['tmp2', 'lre'], ['cfi'])
    dv(lambda: nc.vector.tensor_tensor(V['k'][:], V['tmp'][:], lim, ALU.mult), ['tmp', 'ssc'], ['k'])
    dv(lambda: nc.vector.tensor_tensor(V['cfi'][:], V['cfi'][:], V['k'][:], ALU.subtract), ['cfi', 'k'], ['cfi'])
    dv(lambda: nc.vector.tensor_tensor(V['cfi'][:], V['cfi'][:], V['den'][:], ALU.mult), ['cfi', 'den'], ['cfi'])
    dv(lambda: nc.vector.tensor_scalar(V['ncfi'][:], V['cfi'][:], -1.0, None, ALU.mult), ['cfi'], ['ncfi'])
    dv(lambda: nc.vector.tensor_scalar(V['nsin'][:], V['sin'][:], -1.0, None, ALU.mult), ['sin'], ['nsin'])
    Bt = K.sb('sBt', [128, 16, 2, 128])
    Cm = K.sb('sCm', [128, 16, 2, 128])
    K.dma('sp', Cm[:, :, 0, :], D['ssm_c'][l, 0].rearrange("c p f -> p c f"), writes=['sCm'])
    K.dma('sp', Cm[:, :, 1, :], D['ssm_c'][l, 1].rearrange("c p f -> p c f"), writes=['sCm'])
    K.op('pool', lambda: nc.gpsimd.tensor_scalar(Cm[:, :, 1, :], Cm[:, :, 1, :], -1.0, None, ALU.mult), reads=['sCm'], writes=['sCm'])
    K.push()
    braw = K.sb('sbraw', [128, 16, 2, 128])
    K.dma('sp', braw[:, :, 0, :], D['ssm_b'][l, 0].rearrange("c p f -> p c f"), writes=['sbraw'])
    K.dma('sp', braw[:, :, 1, :], D['ssm_b'][l, 1].rearrange("c p f -> p c f"), writes=['sbraw'])
    bbar = [K.sb(f'sbbar{i}', [128, 2, 128]) for i in range(2)]
    t4 = [K.sb(f'sbt{i}', [128, 128]) for i in range(2)]
    pbt = [K.ps(f'spbt{i}', [128, 256]) for i in range(2)]
    for c in range(16):
        i = c % 2
        cr, ci, nci = V['cfr'][:, c:c + 1], V['cfi'][:, c:c + 1], V['ncfi'][:, c:c + 1]
        K.op('dve', lambda: nc.vector.tensor_scalar(t4[0][:], braw[:, c, 1, :], nci, None, ALU.mult), reads=['sbraw', 'svall'], writes=['sbt0'])
        K.op('dve', lambda: nc.vector.scalar_tensor_tensor(bbar[i][:, 0, :], braw[:, c, 0, :], cr, t4[0][:], ALU.mult, ALU.add),
             reads=['sbraw', 'svall', 'sbt0'], writes=[f'sbbar{i}'])
        K.op('dve', lambda: nc.vector.tensor_scalar(t4[1][:], braw[:, c, 0, :], ci, None, ALU.mult), reads=['sbraw', 'svall'], writes=['sbt1'])
        K.op('dve', lambda: nc.vector.scalar_tensor_tensor(bbar[i][:, 1, :], braw[:, c, 1, :], cr, t4[1][:], ALU.mult, ALU.add),
             reads=['sbraw', 'svall', 'sbt1'], writes=[f'sbbar{i}'])
        for ri in range(2):
            K.op('pe', lambda ri=ri: nc.tensor.transpose(pbt[i][:, ri * 128:(ri + 1) * 128], bbar[i][:, ri, :], ident[:]),
                 reads=[f'sbbar{i}', 'ident'], writes=[f'spbt{i}'])
        K.op('act', lambda: nc.scalar.copy(Bt[:, c, :, :], pbt[i][:].rearrange("p (r f) -> p r f", r=2)), reads=[f'spbt{i}'], writes=['sBt'])
    K.pop()
    E = K.sb('sE', [128, 16, 2, T])
    et = [K.sb(f'set{i}', [128, T // 2]) for i in range(2)]
    for c in range(16):
        K.op('dve', lambda: nc.vector.tensor_copy(E[:, c, 0, 0:1], V['cos'][:, c:c + 1]), reads=['svall'], writes=[('sE', c)])
        K.op('dve', lambda: nc.vector.tensor_copy(E[:, c, 1, 0:1], V['sin'][:, c:c + 1]), reads=['svall'], writes=[('sE', c)])
        m = 1
        while m < T:
            wr, wi = E[:, c, 0, m - 1:m], E[:, c, 1, m - 1:m]
            er, ei = E[:, c, 0, 0:m], E[:, c, 1, 0:m]
            K.op('dve', lambda: nc.vector.tensor_scalar(et[0][:, 0:m], ei, wi, None, ALU.mult), reads=[('sE', c)], writes=['set0'])
            K.op('dve', lambda: nc.vector.tensor_scalar(et[1][:, 0:m], ei, wr, None, ALU.mult), reads=[('sE', c)], writes=['set1'])
            K.op('dve', lambda: nc.vector.scalar_tensor_tensor(E[:, c, 0, m:2 * m], er, wr, et[0][:, 0:m], ALU.mult, ALU.subtract),
                 reads=[('sE', c), 'set0'], writes=[('sE', c)])
            K.op('dve', lambda: nc.vector.scalar_tensor_tensor(E[:, c, 1, m:2 * m], er, wi, et[1][:, 0:m], ALU.mult, ALU.add),
                 reads=[('sE', c), 'set1'], writes=[('sE', c)])
            m *= 2
    dT = K.sb('sdT', [128, 2])
    K.dma('sp', dT[:], D['ssm_dT'][l], writes=['sdT'])
    gw = K.sb('sgw', [128, 2, 512])
    K.dma('sp', gw[:], D['ssm_glu_w'][l].rearrange("(k p) n -> p k n", p=128), writes=['sgw'])
    carry = K.sb('scarry', [128, 8, 2])
    ut = [K.sb(f'sut{i}', [128, 4, 256]) for i in range(1)]
    uT = [K.sb(f'suT{i}', [128, 2, T]) for i in range(2)]
    puT = K.ps('spuT', [128, 2, T])
    pbu = [K.ps(f'spbu{i}', [128, 2, T]) for i in range(1)]
    bu = [K.sb(f'sbu{i}', [128, 2, T]) for i in range(1)]
    X = [K.sb(f'sX{i}', [128, 2, T]) for i in range(1)]
    W = [K.sb(f'sW{i}', [128, 2, T]) for i in range(1)]
    S = [K.sb(f'sS{i}', [128, 2, T]) for i in range(1)]
    tq = [K.sb(f'stq{i}', [128, T]) for i in range(4)]
    py = K.ps('spy', [128, 2, T])
    yTf = K.sb('syTf', [128, 2, T])
    ytok = K.sb('sytok', [128, 4, 256])
    yb = K.sb('syb', [128, 2, T])
    g1 = K.sb('sg1', [128, 2, T])
    pz = K.ps('spz', [128, T])
    zs = K.sb('szs', [128, 512])
    zo = K.sb('szo', [128, 256])
    nu = 0
    for d in range(2):
        chunks = [([0, 1], True)] + [([CT + 4 * c + i for i in range(4)], False) for c in range(NT // 4)]
        if d == 1:
            chunks = [([1, 0], True)] + [([CT + NT - 1 - (4 * c + i) for i in range(4)], False) for c in range(NT // 4)]
        for ci, (tl, is_ctx) in enumerate(chunks):
            Tc = len(tl) * 128
            ui = ci % 2
            for j, tt in enumerate(tl):
                K.dma('sp', ut[0][:, j, :], D['pl'][tt * 128:(tt + 1) * 128, 0:256], writes=['sut0'])
            for j in range(len(tl)):
                for kc in range(2):
                    K.op('pe', lambda j=j, kc=kc: nc.tensor.matmul(puT[:, kc, j * 128:(j + 1) * 128], ut[0][:, j, kc * 128:(kc + 1) * 128],
                                                                 (ident if d == 0 else jm)[:], start=True, stop=True),
                         reads=['sut0', 'ident', 'jm'], writes=['spuT'])
            K.op('act', lambda: nc.scalar.copy(uT[ui][:, :, 0:Tc], puT[:, :, 0:Tc]), reads=['spuT'], writes=[f'suT{ui}'])
            need_out = (not is_ctx) or with_ctx
            for st in range(8):
                c = d * 8 + st
                kc = st // 4
                b = 0
                nu += 1
                for ri in range(2):
                    K.op('pe', lambda ri=ri: nc.tensor.matmul(pbu[0][:, ri, 0:Tc], Bt[:, c, ri, :], uT[ui][:, kc, 0:Tc], start=True, stop=True),
                         reads=['sBt', f'suT{ui}'], writes=['spbu0'])
                K.op('act', lambda: nc.scalar.copy(bu[b][:, :, 0:Tc], pbu[0][:, :, 0:Tc]), reads=['spbu0'], writes=[f'sbu{b}'])
                Er, Ei = E[:, c, 0, 0:Tc], E[:, c, 1, 0:Tc]
                br, bi = bu[b][:, 0, 0:Tc], bu[b][:, 1, 0:Tc]
                ek = ('sE', c)
                K.op('dve', lambda: nc.vector.tensor_tensor(tq[0][:, 0:Tc], Er, br, ALU.mult), reads=[ek, f'sbu{b}'], writes=['stq0'])
                K.op('dve', lambda: nc.vector.tensor_tensor(tq[1][:, 0:Tc], Ei, bi, ALU.mult), reads=[ek, f'sbu{b}'], writes=['stq1'])
                K.op('dve', lambda: nc.vector.tensor_tensor(X[b][:, 0, 0:Tc], tq[0][:, 0:Tc], tq[1][:, 0:Tc], ALU.add), reads=['stq0', 'stq1'], writes=[f'sX{b}'])
                K.op('pool', lambda: nc.gpsimd.tensor_tensor(tq[2][:, 0:Tc], Er, bi, ALU.mult), reads=[ek, f'sbu{b}'], writes=['stq2'])
                K.op('pool', lambda: nc.gpsimd.tensor_tensor(tq[3][:, 0:Tc], Ei, br, ALU.mult), reads=[ek, f'sbu{b}'], writes=['stq3'])
                K.op('pool', lambda: nc.gpsimd.tensor_tensor(X[b][:, 1, 0:Tc], tq[2][:, 0:Tc], tq[3][:, 0:Tc], ALU.subtract), reads=['stq2', 'stq3'], writes=[f'sX{b}'])
                rho_b = V['rho'][:, c:c + 1].to_broadcast([128, Tc])
                for ri in range(2):
                    init = 0.0 if ci == 0 else carry[:, st, ri:ri + 1]
                    K.op('dve', lambda ri=ri, init=init: nc.vector.tensor_tensor_scan(W[b][:, ri, 0:Tc], rho_b, X[b][:, ri, 0:Tc], init, ALU.mult, ALU.add),
                         reads=[f'sX{b}', 'svall', ('scarry', st)], writes=[f'sW{b}'])
                wr_, wi_ = W[b][:, 0, 0:Tc], W[b][:, 1, 0:Tc]
                K.op('dve', lambda: nc.vector.tensor_tensor(tq[0][:, 0:Tc], Er, wr_, ALU.mult), reads=[ek, f'sW{b}'], writes=['stq0'])
                K.op('dve', lambda: nc.vector.tensor_tensor(tq[1][:, 0:Tc], Ei, wi_, ALU.mult), reads=[ek, f'sW{b}'], writes=['stq1'])
                K.op('dve', lambda: nc.vector.tensor_tensor(S[b][:, 0, 0:Tc], tq[0][:, 0:Tc], tq[1][:, 0:Tc], ALU.subtract), reads=['stq0', 'stq1'], writes=[f'sS{b}'])
                K.op('pool', lambda: nc.gpsimd.tensor_tensor(tq[2][:, 0:Tc], Er, wi_, ALU.mult), reads=[ek, f'sW{b}'], writes=['stq2'])
                K.op('pool', lambda: nc.gpsimd.tensor_tensor(tq[3][:, 0:Tc], Ei, wr_, ALU.mult), reads=[ek, f'sW{b}'], writes=['stq3'])
                K.op('pool', lambda: nc.gpsimd.tensor_tensor(S[b][:, 1, 0:Tc], tq[2][:, 0:Tc], tq[3][:, 0:Tc], ALU.add), reads=['stq2', 'stq3'], writes=[f'sS{b}'])
                K.op('act', lambda: nc.scalar.copy(carry[:, st, :], S[b][:, :, Tc - 1]), reads=[f'sS{b}'], writes=[('scarry', st)])
                if not need_out:
                    continue
                first = (st % 4 == 0)
                last = (st % 4 == 3)
                for ri in range(2):
                    K.op('pe', lambda ri=ri: nc.tensor.matmul(py[:, kc, 0:Tc], Cm[:, c, ri, :], S[b][:, ri, 0:Tc], start=(first and ri == 0), stop=(last and ri == 1)),
                         reads=['sCm', f'sS{b}'], writes=['spy'])
            if not need_out:
                continue
            if d == 0:
                col0 = tl[0] * 128
                for ft in range(2):
                    K.op('dve', lambda ft=ft: nc.vector.scalar_tensor_tensor(yTf[:, ft, 0:Tc], uT[ui][:, ft, 0:Tc], dT[:, ft:ft + 1], py[:, ft, 0:Tc], ALU.mult, ALU.add),
                         reads=[f'suT{ui}', 'sdT', 'spy'], writes=['syTf'])
                K.dma('pool', D['ysT'][:, :, col0:col0 + Tc].rearrange("f p t -> p f t"), yTf[:, :, 0:Tc], reads=['syTf'], writes=[('ysT', col0)])
            else:
                nt_ = len(tl)
                K.op('act', lambda: nc.scalar.copy(yTf[:, :, 0:Tc], py[:, :, 0:Tc]), reads=['spy'], writes=['syTf'])
                pyt = puT[:].rearrange("p a t -> p (a t)").rearrange("p (s f) -> p s f", f=256)
                for j in range(nt_):
                    for ft in range(2):
                        K.op('pe', lambda j=j, ft=ft: nc.tensor.transpose(pyt[:, j, ft * 128:(ft + 1) * 128], yTf[:, ft, j * 128:(j + 1) * 128], ident[:]),
                             reads=['syTf', 'ident'], writes=['spuT'])
                K.op('act', lambda: nc.scalar.copy(ytok[:, 0:nt_, :], pyt[:, 0:nt_, :]), reads=['spuT'], writes=['sytok'])
                col0 = tl[-1] * 128
                K.dma('sp', yb[:, :, 0:Tc], D['ysT'][:, :, col0:col0 + Tc].rearrange("f p t -> p f t"), writes=['syb'])
                for j in range(nt_):
                    nj = nt_ - 1 - j
                    for ft in range(2):
                        K.op('pe', lambda j=j, nj=nj, ft=ft: nc.tensor.matmul(py[:, ft, nj * 128:(nj + 1) * 128], ytok[:, j, ft * 128:(ft + 1) * 128], jm[:], start=True, stop=True),
                             reads=['sytok', 'jm'], writes=['spy'])
                K.op('dve', lambda: nc.vector.tensor_tensor(yb[:, :, 0:Tc], yb[:, :, 0:Tc], py[:, :, 0:Tc], ALU.add), reads=['syb', 'spy'], writes=['syb'])
                if 'ydbg' in DEBUG_OUT:
                    K.dma('sp', D['ydbg'][:, :, col0:col0 + Tc].rearrange("f p t -> p f t"), yb[:, :, 0:Tc], reads=['syb'], writes=[('ydbg', col0)])
                gelu_tanh(K, nc, None, yb[:, :, 0:Tc], yb[:, :, 0:Tc], 'syb', 'syb', g1[:, :, 0:Tc], 'sg1')
                for j in range(nt_):
                    for ft in range(2):
                        K.op('pe', lambda j=j, ft=ft: nc.tensor.matmul(pz[:], yb[:, ft, j * 128:(j + 1) * 128], gw[:, ft, :], start=(ft == 0), stop=(ft == 1)),
                             reads=['syb', 'sgw'], writes=['spz'])
                    K.op('act', lambda: nc.scalar.activation(zs[:, 256:512], pz[:, 256:512], AF.Sigmoid), reads=['spz'], writes=['szs'])
                    K.op('dve', lambda: nc.vector.tensor_tensor(zo[:], pz[:, 0:256], zs[:, 256:512], ALU.mult), reads=['spz', 'szs'], writes=['szo'])
                    r0 = col0 + j * 128
                    K.dma('pool', D['ymix'][r0:r0 + 128, 0:256], zo[:], reads=['szo'], writes=[('ymix', 's', r0)])
        K.barrier()
    K.pop()


def stage_merge(K, nc, D, cfg, l, C, with_ctx):
    K.push()
    ident = C['ident']
    g1 = {'l': load_bc(K, D, 'l', 2)}
    if with_ctx:
        g1['c'] = load_bc(K, D, 'c', 2)
    wbr = K.sb('wbr', [128, 8, 1024], MM)
    wout = K.sb('wout', [128, 8, 1024], MM)
    K.push()
    mst = [K.sb(f'mst{i}', [128, 2, 1024]) for i in range(2)]
    srcs = [(wbr, 0, D['w_br_ssm'][l]), (wbr, 2, D['w_br_diff'][l][0:256]), (wbr, 4, D['w_br_diff'][l][256:512]), (wbr, 6, D['w_br_na'][l])] + \
           [(wout, 2 * j, D['w_out'][l][256 * j:256 * (j + 1)]) for j in range(4)]
    for si, (dst, k0, src) in enumerate(srcs):
        b_ = si % 2
        K.dma('sp', mst[b_][:], src.rearrange("(k p) n -> p k n", p=128), writes=[f'mst{b_}'])
        K.op('pool', lambda: nc.gpsimd.tensor_copy(dst[:, k0:k0 + 2, :], mst[b_][:]), reads=[f'mst{b_}'], writes=['wbr', 'wout'])
    K.pop()
    yt = [K.sb(f'myt{i}', [128, 1024]) for i in range(2)]
    gt = [K.sb(f'mgt{i}', [128, 3072]) for i in range(2)]
    xt = [K.sb(f'mxt{i}', [128, 1024]) for i in range(2)]
    yT = K.sb('myT', [128, 8, 128], MM)
    mm = K.sb('mmm', [128, 1024])
    mT = K.sb('mmT', [128, 8, 128], MM)
    tmp = K.sb('mtmp', [128, 512])
    xo = [K.sb(f'mxo{i}', [128, 1024]) for i in range(2)]
    ptr = K.ps('mptr', [128, 1024])
    pb = [K.ps(f'mpb{i}', [128, 512]) for i in range(2)]
    npb = 0
    tiles = list(range(cfg.TT)) if with_ctx else list(range(CT, cfg.TT))
    branches = [(0, [0, 1]), (1, [2, 3, 4, 5]), (2, [6, 7])]
    for tt in tiles:
        i = tt % 2
        w = 'c' if tt < CT else 'l'
        rows = slice(tt * 128, (tt + 1) * 128)
        K.dma('sp', yt[i][:], D['ymix'][rows, :], writes=[f'myt{i}'])
        K.dma('sp', gt[i][:], D['pl'][rows, 2560:5632], writes=[f'mgt{i}'])
        K.dma('sp', xt[i][:], xsrc(D, cfg, l, tt), writes=[f'mxt{i}'])
        K.op('act', lambda: nc.scalar.activation(gt[i][:], gt[i][:], AF.Sigmoid), reads=[f'mgt{i}'], writes=[f'mgt{i}'])
        for k in range(8):
            K.op('pe', lambda k=k: nc.tensor.transpose(ptr[:, k * 128:(k + 1) * 128], yt[i][:, k * 128:(k + 1) * 128], ident[:]),
                 reads=[f'myt{i}', 'ident'], writes=['mptr'])
        K.op('act', lambda: nc.scalar.copy(yT[:], ptr[:].rearrange("p (k t) -> p k t", k=8)), reads=['mptr'], writes=['myT'])
        for (br, ks) in branches:
            for hh in range(2):
                p = pb[npb % 2]
                pk = f'mpb{npb % 2}'
                npb += 1
                for ki, k in enumerate(ks):
                    K.op('pe', lambda k=k, ki=ki: nc.tensor.matmul(p[:], yT[:, k, :], wbr[:, k, hh * 512:(hh + 1) * 512], start=(ki == 0), stop=(ki == len(ks) - 1)),
                         reads=['myT', 'wbr'], writes=[pk])
                gsl = gt[i][:, br * 1024 + hh * 512: br * 1024 + (hh + 1) * 512]
                if br == 0:
                    K.op('dve', lambda: nc.vector.tensor_tensor(mm[:, hh * 512:(hh + 1) * 512], p[:], gsl, ALU.mult), reads=[pk, f'mgt{i}'], writes=[('mmm', hh)])
                else:
                    K.op('dve', lambda: nc.vector.tensor_tensor(tmp[:], p[:], gsl, ALU.mult), reads=[pk, f'mgt{i}'], writes=['mtmp'])
                    K.op('pool', lambda: nc.gpsimd.tensor_tensor(mm[:, hh * 512:(hh + 1) * 512], mm[:, hh * 512:(hh + 1) * 512], tmp[:], ALU.add),
                         reads=['mtmp', ('mmm', hh)], writes=[('mmm', hh)])
        for k in range(8):
            K.op('pe', lambda k=k: nc.tensor.transpose(ptr[:, k * 128:(k + 1) * 128], mm[:, k * 128:(k + 1) * 128], ident[:]),
                 reads=[('mmm', k // 4), 'ident'], writes=['mptr'])
        K.op('act', lambda: nc.scalar.copy(mT[:], ptr[:].rearrange("p (k t) -> p k t", k=8)), reads=['mptr'], writes=['mmT'])
        for hh in range(2):
            p = pb[npb % 2]
            pk = f'mpb{npb % 2}'
            npb += 1
            for k in range(8):
                K.op('pe', lambda k=k: nc.tensor.matmul(p[:], mT[:, k, :], wout[:, k, hh * 512:(hh + 1) * 512], start=(k == 0), stop=(k == 7)),
                     reads=['mmT', 'wout'], writes=[pk])
            K.op('dve', lambda: nc.vector.tensor_tensor(tmp[:], p[:], g1[w][:, hh * 512:(hh + 1) * 512], ALU.mult), reads=[pk, f'bc{w}2'], writes=['mtmp'])
            K.op('pool', lambda: nc.gpsimd.tensor_tensor(xo[i][:, hh * 512:(hh + 1) * 512], xt[i][:, hh * 512:(hh + 1) * 512], tmp[:], ALU.add),
                 reads=['mtmp', f'mxt{i}'], writes=[f'mxo{i}'])
        K.dma('pool', D['xres'][rows, :], xo[i][:], reads=[f'mxo{i}'], writes=[('xres', tt)])
    K.pop()


def stage_peer(K, nc, D, cfg, l, C, with_ctx, final):
    K.push()
    ident = C['ident']
    bc = {}
    for w in (['l', 'c'] if with_ctx else ['l']):
        for j in (3, 4, 5):
            bc[(w, j)] = load_bc(K, D, w, j)
    if final:
        fgr = K.sb('fgr', [1, 1024])
        K.dma('sp', fgr[:], D['final_g'], writes=['fgr'])
        fg = K.sb('fg', [128, 1024])
        K.push()
        with_ps = K.ps('fgps', [128, 512])
        for hh in range(2):
            bcast_row(K, nc, C, fgr[:, hh * 512:(hh + 1) * 512], 'fgr', fg[:, hh * 512:(hh + 1) * 512], 'fg', 512, with_ps, 'fgps')
        K.pop()
    wq = K.sb('wq', [128, 8, 2048])
    K.dma('sp', wq[:, :, 0:1024], D['peer_wq'][l, :, 0:1024].rearrange("(k p) n -> p k n", p=128), writes=['wq'])
    K.dma('sp', wq[:, :, 1024:2048], D['peer_wq'][l, :, 1024:2048].rearrange("(k p) n -> p k n", p=128), writes=['wq'])
    skT = K.sb('skT', [128, 16, 128])
    identR = K.sb('identR', [128, 128], MM)
    K.push()
    skr = K.sb('skr', [128, 16, 128])
    K.dma('sp', skr[:], D['peer_sk'][l].rearrange("c n d -> n c d"), writes=['skr'])
    pst = K.ps('pst', [128, 4, 128])
    K.op('dve', lambda: nc.vector.tensor_copy(identR[:], ident[:]), reads=['ident'], writes=['identR'])
    for c4 in range(4):
        for j in range(4):
            K.op('pe', lambda j=j: nc.tensor.transpose(pst[:, j, :], skr[:, c4 * 4 + j, :], ident[:]), reads=['skr', 'ident'], writes=['pst'])
        K.op('act', lambda: nc.scalar.copy(skT[:, c4 * 4:(c4 + 1) * 4, :], pst[:]), reads=['pst'], writes=['skT'])
    K.pop()
    iota16 = K.sb('iota16', [128, 16])
    K.dma('sp', iota16[:], D['iota16'], writes=['iota16'])
    xt = [K.sb(f'pxt{i}', [128, 1024]) for i in range(1)]
    h2 = K.sb('ph2', [128, 1024])
    tmp = K.sb('junkP', [128, 1024])
    scr = (K.sb('pss', [128, 1]), K.sb('prs', [128, 1]), tmp)
    ptr = K.ps('pptr', [128, 1024])
    h2T = K.sb('ph2T', [128, 8, 128])
    pq = [K.ps(f'ppq{i}', [128, 4, 128]) for i in range(2)]
    qT = K.sb('pqT', [128, 16, 128])
    psc = [K.ps(f'ppsc{i}', [128, 4, 128]) for i in range(1)]
    pacc = [K.ps(f'ppacc{i}', [128, 512]) for i in range(2)]
    vsc = [K.sb(f'pvsc{i}', [128, 1024], MM) for i in range(2)]
    s = K.sb('ps_s', [128, 16, 128])
    s2 = K.sb('ps_s2', [128, 16, 128])
    sv = K.sb('ps_sv', [128, 16, 16])
    si = K.sb('ps_si', [128, 16, 16], U32)
    sif = K.sb('ps_sif', [128, 16, 16])
    cand = s[:].rearrange("p a n -> p (a n)").rearrange("p (h c) -> p h c", c=256)
    cand2 = s2[:].rearrange("p a n -> p (a n)").rearrange("p (h c) -> p h c", c=256)
    best = K.sb('ps_best', [128, 8, 16])
    pos = K.sb('ps_pos', [128, 8, 16], U32)
    pa = K.sb('ps_pa', [128, 8, 16], U32)
    pbb = K.sb('ps_pb', [128, 8, 16], U32)
    paf = K.sb('ps_paf', [128, 8, 16])
    pbf = K.sb('ps_pbf', [128, 8, 16])
    oh = K.sb('ps_oh', [128, 8, 16, 16])
    ii = K.sb('ps_ii', [128, 8, 16])
    jj = K.sb('ps_jj', [128, 8, 16])
    eidx = K.sb('ps_eidx', [128, 128], I32)
    gg = K.sb('ps_g', [128, 8, 16])
    gs = K.sb('ps_gs', [128, 8])
    act = K.sb('ps_act', [128, 128])
    wgt = K.sb('ps_wgt', [128, 128])
    gtmp = K.sb('ps_gtmp', [128, 128])
    NG = 10
    gb = [K.sb(f'pgb{i}', [128, 2048], BF16) for i in range(NG)]
    xo = K.sb('pxo', [128, 1024])
    ng = 0
    nq_ = 0
    tiles = list(range(cfg.TT)) if with_ctx else list(range(CT, cfg.TT))
    split = final and PAIR_SPLIT
    if split:
        tiles = list(range(CT, CT + cfg.NT // 2))
        ridx = K.sb('ridx', [128, cfg.NT // 2], I32)
        K.dma('sp', ridx[:], D['rowidx'], writes=['ridx'])
    T1 = ['pk_all']
    for tt in tiles:
        i = 0
        w = 'c' if tt < CT else 'l'
        rows = slice(tt * 128, (tt + 1) * 128)
        if split:
            j_ = tt - CT
            K.dma('pool', None, None, reads=['ridx'], writes=[f'pxt{i}'], fn=lambda: nc.gpsimd.indirect_dma_start(
                out=xt[i][:], out_offset=None, in_=D['xres'], in_offset=bass.IndirectOffsetOnAxis(ap=ridx[:, j_:j_ + 1], axis=0)))
        else:
            K.dma('sp', xt[i][:], D['xres'][rows, :], writes=[f'pxt{i}'])
        norm_mod(K, nc, xt[i], f'pxt{i}', h2, 'ph2', bc[(w, 3)], f'bc{w}3', bc[(w, 4)], f'bc{w}4', scr, 'P')
        for k in range(8):
            K.op('pe', lambda k=k: nc.tensor.transpose(ptr[:, k * 128:(k + 1) * 128], h2[:, k * 128:(k + 1) * 128], ident[:]),
                 reads=['ph2', 'ident'], writes=['pptr'])
        K.op('act', lambda: nc.scalar.copy(h2T[:], ptr[:].rearrange("p (k t) -> p k t", k=8)), reads=['pptr'], writes=['ph2T'])
        for c4 in range(4):
            p = pq[nq_ % 2]
            pk = f'ppq{nq_ % 2}'
            nq_ += 1
            for j in range(4):
                hx = c4 * 4 + j
                for k in range(8):
                    K.op('pe', lambda j=j, k=k, hx=hx: nc.tensor.matmul(p[:, j, :], wq[:, k, hx * 128:(hx + 1) * 128], h2T[:, k, :], start=(k == 0), stop=(k == 7)),
                         reads=['wq', 'ph2T'], writes=[pk])
            K.op('act', lambda: nc.scalar.copy(qT[:, c4 * 4:(c4 + 1) * 4, :], p[:]), reads=[pk], writes=[('pqT', c4)])
        for c4 in range(4):
            p = psc[0]
            pk = 'ppsc0'
            for j in range(4):
                hx = c4 * 4 + j
                K.op('pe', lambda j=j, hx=hx: nc.tensor.matmul(p[:, j, :], qT[:, hx, :], skT[:, hx, :], start=True, stop=True),
                     reads=[('pqT', c4), 'skT'], writes=[pk])
            K.op('act', lambda: nc.scalar.copy(s[:, c4 * 4:(c4 + 1) * 4, :], p[:]), reads=[pk], writes=[('ps_s', c4)])
        for hx in range(16):
            sk_ = ('ps_s', hx // 4)
            K.op('dve', lambda: nc.vector.max(out=sv[:, hx, 0:8], in_=s[:, hx, :]), reads=[sk_], writes=T1)
            K.op('dve', lambda: nc.vector.max_index(out=si[:, hx, 0:8], in_max=sv[:, hx, 0:8], in_values=s[:, hx, :]), reads=[sk_] + T1, writes=T1)
            K.op('dve', lambda: nc.vector.match_replace(out=s2[:, hx, :], in_to_replace=sv[:, hx, 0:8], in_values=s[:, hx, :], imm_value=-1e30), reads=[sk_] + T1, writes=T1)
            K.op('dve', lambda: nc.vector.max(out=sv[:, hx, 8:16], in_=s2[:, hx, :]), reads=T1, writes=T1)
            K.op('dve', lambda: nc.vector.max_index(out=si[:, hx, 8:16], in_max=sv[:, hx, 8:16], in_values=s2[:, hx, :]), reads=T1, writes=T1)

        T2 = T1 + [('ps_s', c) for c in range(4)]

        def dv(fn):
            K.op('dve', fn, reads=T2, writes=T2)
        svv = sv[:].rearrange("p (h x) a -> p h x a", x=2)
        cv = cand.rearrange("p h (a b) -> p h a b", b=16)
        dv(lambda: nc.vector.tensor_tensor(cv, svv[:, :, 0, :].unsqueeze(3).to_broadcast([128, 8, 16, 16]),
                                           svv[:, :, 1, :].unsqueeze(2).to_broadcast([128, 8, 16, 16]), ALU.add))
        for h in range(8):
            dv(lambda: nc.vector.max(out=best[:, h, 0:8], in_=cand[:, h, :]))
            dv(lambda: nc.vector.max_index(out=pos[:, h, 0:8], in_max=best[:, h, 0:8], in_values=cand[:, h, :]))
            dv(lambda: nc.vector.match_replace(out=cand2[:, h, :], in_to_replace=best[:, h, 0:8], in_values=cand[:, h, :], imm_value=-1e30))
            dv(lambda: nc.vector.max(out=best[:, h, 8:16], in_=cand2[:, h, :]))
            dv(lambda: nc.vector.max_index(out=pos[:, h, 8:16], in_max=best[:, h, 8:16], in_values=cand2[:, h, :]))
        dv(lambda: nc.vector.tensor_tensor(gg[:], best[:], best[:, :, 0:1].to_broadcast([128, 8, 16]), ALU.subtract))
        K.op('act', lambda: nc.scalar.activation(gg[:], gg[:], AF.Exp), reads=T1, writes=T1)
        dv(lambda: nc.vector.reduce_sum(gs[:], gg[:], axis=AX.X))
        dv(lambda: nc.vector.reciprocal(gs[:], gs[:]))
        dv(lambda: nc.vector.tensor_tensor(gg[:], gg[:], gs[:].unsqueeze(2).to_broadcast([128, 8, 16]), ALU.mult))
        dv(lambda: nc.vector.tensor_scalar(pa[:], pos[:], 4, None, ALU.logical_shift_right))
        dv(lambda: nc.vector.tensor_scalar(pbb[:], pos[:], 15, None, ALU.bitwise_and))
        dv(lambda: nc.vector.tensor_copy(paf[:], pa[:]))
        dv(lambda: nc.vector.tensor_copy(pbf[:], pbb[:]))
        dv(lambda: nc.vector.tensor_copy(sif[:], si[:]))
        sfv = sif[:].rearrange("p (h x) a -> p h x a", x=2)
        io_b = iota16[:].unsqueeze(1).unsqueeze(1).to_broadcast([128, 8, 16, 16])
        for (pf, xsel, dst) in ((paf, 0, ii), (pbf, 1, jj)):
            dv(lambda: nc.vector.tensor_tensor(oh[:], pf[:].unsqueeze(3).to_broadcast([128, 8, 16, 16]), io_b, ALU.is_equal))
            dv(lambda: nc.vector.tensor_tensor(oh[:], oh[:], sfv[:, :, xsel, :].unsqueeze(2).to_broadcast([128, 8, 16, 16]), ALU.mult))
            dv(lambda: nc.vector.reduce_sum(dst[:], oh[:], axis=AX.X))
        dv(lambda: nc.vector.scalar_tensor_tensor(ii[:], ii[:], 128.0, jj[:], ALU.mult, ALU.add))
        dv(lambda: nc.vector.tensor_copy(eidx[:], ii[:].rearrange("p h k -> p (h k)")))
        GRP = 4
        ggf = gg[:].rearrange("p h k -> p (h k)")
        for g0 in range(0, 128, GRP):
            held = []
            for slot in range(g0, g0 + GRP):
                b_ = gb[ng % NG]
                bk = f'pgb{ng % NG}'
                ng += 1
                held.append((b_, bk))
                K.dma('pool', None, None, reads=T1, writes=[bk], fn=lambda: nc.gpsimd.indirect_dma_start(
                    out=b_[:], out_offset=None, in_=D[f'uv{l}'], in_offset=bass.IndirectOffsetOnAxis(ap=eidx[:, slot:slot + 1], axis=0)))
                K.op('dve', lambda: nc.vector.scalar_tensor_tensor(tmp[:], b_[:, 0:1024], 1.0, h2[:], ALU.mult, ALU.mult, accum_out=act[:, slot:slot + 1]),
                     reads=[bk, 'ph2'], writes=['junkP', 'ps_act'])
            sl = slice(g0, g0 + GRP)
            gelu_tanh(K, nc, None, wgt[:, sl], act[:, sl], 'ps_act', 'ps_wgt', gtmp[:, sl], 'ps_gtmp')
            K.op('dve', lambda: nc.vector.tensor_tensor(wgt[:, sl], wgt[:, sl], ggf[:, sl], ALU.mult), reads=['ps_wgt'] + T1, writes=['ps_wgt'])
            for si_, slot in enumerate(range(g0, g0 + GRP)):
                b_, bk = held[si_]
                vi = slot % 2
                K.op('act', lambda: nc.scalar.activation(vsc[vi][:], b_[:, 1024:2048], AF.Copy, scale=wgt[:, slot:slot + 1]), reads=[bk, 'ps_wgt'], writes=[f'pvsc{vi}'])
                for hh in range(2):
                    K.op('pe', lambda hh=hh: nc.tensor.matmul(pacc[hh][:], identR[:], vsc[vi][:, hh * 512:(hh + 1) * 512], start=(slot == 0), stop=(slot == 127)),
                         reads=['identR', f'pvsc{vi}'], writes=[f'ppacc{hh}'])
        for hh in range(2):
            K.op('dve', lambda hh=hh: nc.vector.tensor_tensor(tmp[:, hh * 512:(hh + 1) * 512], pacc[hh][:], bc[(w, 5)][:, hh * 512:(hh + 1) * 512], ALU.mult),
                 reads=[f'ppacc{hh}', f'bc{w}5'], writes=['junkP'])
        K.op('dve', lambda: nc.vector.tensor_tensor(xo[:], xt[i][:], tmp[:], ALU.add), reads=['junkP', f'pxt{i}'], writes=['pxo'])
        if not final:
            K.dma('sp', D['xres'][rows, :], xo[:], reads=['pxo'], writes=[('xres', tt)])
        else:
            ss, rs, junk = scr
            K.op('act', lambda: nc.scalar.activation(junk[:], xo[:], AF.Square, accum_out=ss[:]), reads=['pxo'], writes=['junkP', 'ssP'])
            K.op('dve', lambda: nc.vector.tensor_scalar(rs[:], ss[:], 1.0 / 1024.0, 1e-6, ALU.mult, ALU.add), reads=['ssP'], writes=['rsP'])
            K.op('act', lambda: nc.scalar.activation(rs[:], rs[:], AF.Sqrt), reads=['rsP'], writes=['rsP'])
            K.op('dve', lambda: nc.vector.reciprocal(rs[:], rs[:]), reads=['rsP'], writes=['rsP'])
            K.op('dve', lambda: nc.vector.scalar_tensor_tensor(tmp[:], xo[:], rs[:], fg[:], ALU.mult, ALU.mult), reads=['pxo', 'rsP', 'fg'], writes=['junkP'])
            K.dma('sp', D['out'][(tt - CT) * 128:(tt - CT + 1) * 128, :], tmp[:], reads=['junkP'], writes=[('out', tt)])
        K.maybe_barrier()
    K.pop()


def stage_tables(K, nc, D, cfg):
    K.push()
    src = [K.sb(f'tbs{i}', [128, 4, 1024]) for i in range(3)]
    dst = [K.sb(f'tbd{i}', [128, 4, 1024], BF16) for i in range(3)]
    n = 0
    for l in range(cfg.depth):
        for nm_s, nm_d, c0 in ((f'peer_u{l}', f'uv{l}', 0), (f'peer_v{l}', f'uv{l}', 1024)):
            for r0 in range(0, 16384, 512):
                i = n % 3
                n += 1
                K.dma('sp', src[i][:], D[nm_s][r0:r0 + 512, :].rearrange("(p j) c -> p j c", j=4), writes=[f'tbs{i}'])
                if i == 0:
                    K.op('dve', lambda: nc.vector.tensor_copy(dst[i][:], src[i][:]), reads=[f'tbs{i}'], writes=[f'tbd{i}'])
                elif i == 1:
                    K.op('pool', lambda: nc.gpsimd.tensor_copy(dst[i][:], src[i][:]), reads=[f'tbs{i}'], writes=[f'tbd{i}'])
                else:
                    K.op('act', lambda: nc.scalar.copy(dst[i][:], src[i][:]), reads=[f'tbs{i}'], writes=[f'tbd{i}'])
                K.dma('act', D[nm_d][r0:r0 + 512, c0:c0 + 1024].rearrange("(p j) c -> p j c", j=4), dst[i][:], reads=[f'tbd{i}'], writes=[(nm_d, r0, c0)])
    K.pop()


ALL_STAGES = ('inproj', 'prepass', 'diff', 'na', 'ssm', 'merge', 'peer')
_NC_CACHE = {}


def kernel(**inputs):
    inp = {k: np.asarray(v) for k, v in inputs.items()}
    B, L, _ = inp['x'].shape
    depth = inp['ada_w'].shape[0]
    key = (L, depth)
    if key not in _NC_CACHE:
        _NC_CACHE[key] = build(L, depth, stages=ALL_STAGES)
    nc = _NC_CACHE[key]
    n_cores = 8
    if PAIR_SPLIT:
        in_maps = []
        for b in range(B):
            m0 = core_inputs(inp, b, L, depth, 0)
            m1 = dict(m0)
            m1['rowidx'] = core_inputs_rowidx(L, 1)
            in_maps += [m0, m1]
        res = run_bass_kernel_spmd(nc, in_maps, core_ids=list(range(n_cores)))
        out = np.stack([np.concatenate([res.results[2 * b]['out'], res.results[2 * b + 1]['out']], 0) for b in range(B)], 0)
    else:
        maps = [core_inputs(inp, b, L, depth) for b in range(B)]
        in_maps = [maps[i % B] for i in range(n_cores)]
        res = run_bass_kernel_spmd(nc, in_maps, core_ids=list(range(n_cores)))
        out = np.stack([res.results[b]['out'] for b in range(B)], 0)
    return out.astype(np.float32)


def core_inputs_rowidx(L, half):
    nt2 = (L // 128) // 2
    return np.ascontiguousarray((CT * 128 + half * (L // 2) + np.arange(nt2)[None, :] * 128 + np.arange(128)[:, None]).astype(np.int32))
```

```python
import numpy as np
from contextlib import ExitStack
import concourse.bass as bass
import concourse.mybir as mybir
from concourse.bass_utils import run_bass_kernel_spmd

F32 = mybir.dt.float32
F32R = mybir.dt.float32r
BF16 = mybir.dt.bfloat16
QK_BF16 = True
AV_BF16 = True
BF_TABLES = True
USE_R = True
MM = F32R if USE_R else F32
U32 = mybir.dt.uint32
I32 = mybir.dt.int32
ALU = mybir.AluOpType
AF = mybir.ActivationFunctionType
AX = mybir.AxisListType

NSLOT = 12
RELAX_SAME = set()
PAIR_SPLIT = True
BAR_LIM = 12000
DEBUG_OUT = set()


class Sched:
    def __init__(self, nc, es):
        self.nc = nc
        self.es = es
        self.eng = {'pe': nc.tensor, 'dve': nc.vector, 'act': nc.scalar, 'pool': nc.gpsimd, 'sp': nc.sync}
        self.sem = {e: es.enter_context(nc.semaphore(f"s_{e}")) for e in self.eng}
        self.cnt = {e: 0 for e in self.eng}
        self.waited = {e: {} for e in self.eng}
        self.dq = {}
        for q in ('sp', 'pool', 'act'):
            self.dq[q] = dict(sems=[es.enter_context(nc.semaphore(f"d_{q}{i}")) for i in range(NSLOT)],
                              uses=[0] * NSLOT, nxt=0)
        self.lastw = {}
        self.readers = {}
        self.scopes = []
        self.nalloc = 0
        self.sem_arrive = es.enter_context(nc.semaphore("s_arrive"))
        self.sem_epoch = es.enter_context(nc.semaphore("s_epoch"))
        self.epoch = 0

    def push(self):
        s = ExitStack()
        self.scopes.append(s)

    def pop(self):
        self.barrier()
        self.scopes.pop().close()

    def _ctx(self):
        return self.scopes[-1] if self.scopes else self.es

    def sb(self, name, shape, dtype=F32):
        self.nalloc += 1
        return self._ctx().enter_context(self.nc.sbuf_tensor(f"{name}_{self.nalloc}", list(shape), dtype))

    def ps(self, name, shape, dtype=F32):
        self.nalloc += 1
        return self._ctx().enter_context(self.nc.psum_tensor(f"{name}_{self.nalloc}", list(shape), dtype))

    def _semof(self, key):
        if isinstance(key, str):
            return self.sem[key]
        return self.dq[key[1]]['sems'][key[2]]

    def _wait(self, e, tok):
        key, val = tok
        if key == e and (e in ('pe', 'sp') or e in RELAX_SAME):
            return
        if self.waited[e].get(key, 0) >= val:
            return
        self.eng[e].wait_ge(self._semof(key), val)
        self.waited[e][key] = val

    def _deps(self, e, reads, writes):
        for r in reads:
            if r in self.lastw:
                self._wait(e, self.lastw[r])
        for w in writes:
            if w in self.lastw:
                self._wait(e, self.lastw[w])
            for k, v in self.readers.get(w, {}).items():
                self._wait(e, (k, v))

    def _commit(self, tok, reads, writes):
        for r in reads:
            d = self.readers.setdefault(r, {})
            if d.get(tok[0], 0) < tok[1]:
                d[tok[0]] = tok[1]
        for w in writes:
            self.lastw[w] = tok
            self.readers[w] = {}

    def op(self, e, fn, reads=(), writes=()):
        self._deps(e, reads, writes)
        ins = fn()
        ins.then_inc(self.sem[e], 1)
        self.cnt[e] += 1
        self._commit((e, self.cnt[e]), reads, writes)

    def dma(self, q, out, in_, reads=(), writes=(), fn=None):
        d = self.dq[q]
        self._deps(q, reads, writes)
        s = d['nxt']
        d['nxt'] = (s + 1) % NSLOT
        if d['uses'][s] > 0:
            self._wait(q, (('dma', q, s), 16 * d['uses'][s]))
        if fn is None:
            ins = self.eng[q].dma_start(out=out, in_=in_)
        else:
            ins = fn()
        d['uses'][s] += 1
        ins.then_inc(d['sems'][s], 16)
        self._commit((('dma', q, s), 16 * d['uses'][s]), reads, writes)

    def _all_tokens(self):
        toks = [(e, c) for e, c in self.cnt.items() if c > 0]
        for q, d in self.dq.items():
            for s in range(NSLOT):
                if d['uses'][s] > 0:
                    toks.append((('dma', q, s), 16 * d['uses'][s]))
        return toks

    def barrier(self, reset=True):
        toks = self._all_tokens()
        if not toks:
            return
        for e in self.eng:
            for t in toks:
                if t[0] != e:
                    self._wait(e, t)
        if not reset:
            return
        for e in self.eng:
            if self.cnt[e] > BAR_LIM:
                self.epoch += 1
                self.sem[e] = self.es.enter_context(self.nc.semaphore(f"s_{e}_{self.epoch}"))
                self.cnt[e] = 0
                for e2 in self.eng:
                    self.waited[e2].pop(e, None)
                for k in list(self.lastw.keys()):
                    if self.lastw[k][0] == e:
                        del self.lastw[k]
                for k, d in self.readers.items():
                    d.pop(e, None)

    def maybe_barrier(self, lim=None):
        if max(self.cnt.values()) > (BAR_LIM if lim is None else lim):
            self.barrier()

    def finish(self):
        self.barrier(reset=False)


D_MODEL = 1024
IN_W = 5632
CTXL = 256
CT = 2
LAM_INIT = [0.8 - 0.6 * float(np.exp(-0.3 * l)) for l in range(8)]


class Cfg:
    def __init__(self, L, depth):
        self.L = L
        self.depth = depth
        self.NT = L // 128
        self.TT = self.NT + CT
        self.R = L // 64


def declare_io(nc, cfg):
    D = {}
    dp = cfg.depth

    def din(name, shape, dt=F32):
        D[name] = nc.dram_tensor(name, list(shape), dt, kind="ExternalInput").ap()

    def dsc(name, shape, dt=F32):
        kind = "ExternalOutput" if name in DEBUG_OUT else "Internal"
        D[name] = nc.dram_tensor(name, list(shape), dt, kind=kind).ap()

    din('x', [cfg.L, 1024]); din('ctx', [CTXL, 1024]); din('cc', [2, 1024])
    din('ada_w', [dp, 1024, 6144]); din('ada_b', [dp, 6144])
    din('norm_g', [dp, 2048])
    din('w_in', [dp, 1024, IN_W])
    din('ident', [128, 128]); din('sel', [2, 256])
    D['out'] = nc.dram_tensor('out', [cfg.L // 2 if PAIR_SPLIT else cfg.L, 1024], F32, kind="ExternalOutput").ap()
    if PAIR_SPLIT:
        din('rowidx', [128, cfg.NT // 2], I32)
    dsc('xres', [cfg.TT * 128, 1024])
    dsc('pl', [cfg.TT * 128, IN_W])
    dsc('qkT', [8, 128, cfg.TT * 128]); dsc('nqkT', [4, 128, cfg.TT * 128])
    dsc('ymix', [cfg.TT * 128, 1024])
    ntypes = len(na_pair_info(cfg.R)[1])
    din('rope', [cfg.L, 64]); din('nab', [dp, 4, ntypes * 5, 128, 128])
    din('diff_lambda', [dp, 256]); din('diff_subln_g', [dp, 128])
    din('jmat', [128, 128]); din('ssm_sc', [dp, 128, 48]); din('ssm_b', [dp, 2, 16, 128, 128]); din('ssm_c', [dp, 2, 16, 128, 128])
    din('ssm_dT', [dp, 128, 2]); din('ssm_glu_w', [dp, 256, 512])
    dsc('ysT', [2, 128, cfg.TT * 128])
    dsc('bcd', [2, 6, 128, 1024])
    if BF_TABLES:
        for i in range(dp):
            dsc(f'uv{i}', [16384, 2048], BF16)
    din('w_br_ssm', [dp, 256, 1024]); din('w_br_diff', [dp, 512, 1024]); din('w_br_na', [dp, 256, 1024]); din('w_out', [dp, 1024, 1024])
    din('peer_wq', [dp, 1024, 2048]); din('peer_sk', [dp, 16, 128, 128]); [din(f'peer_u{i}', [16384, 1024]) for i in range(dp)]; [din(f'peer_v{i}', [16384, 1024]) for i in range(dp)]
    din('final_g', [1, 1024]); din('iota16', [128, 16])
    if 'ydbg' in DEBUG_OUT:
        dsc('ydbg', [2, 128, cfg.TT * 128])
    return D


def xsrc(D, cfg, l, tt):
    if l == 0:
        if tt < CT:
            return D['ctx'][tt * 128:(tt + 1) * 128, :]
        return D['x'][(tt - CT) * 128:(tt - CT + 1) * 128, :]
    return D['xres'][tt * 128:(tt + 1) * 128, :]


def stage_consts(K, nc, D):
    ident = K.sb('ident', [128, 128])
    K.dma('sp', ident[:], D['ident'], writes=['ident'])
    sel = K.sb('sel', [2, 256])
    K.dma('sp', sel[:], D['sel'], writes=['sel'])
    return dict(ident=ident, sel=sel)


def stage_mods(K, nc, D, cfg, l, C, with_ctx):
    bc = {}
    who = ['l', 'c'] if with_ctx else ['l']
    K.push()
    for w in ['l', 'c']:
        for j in ([0, 1, 2, 3, 4, 5] if w in who else [0, 1]):
            bc[(w, j)] = K.sb(f'bc{w}{j}', [128, 1024])
    ident, sel = C['ident'], C['sel']
    cc = K.sb('cc', [2, 1024])
    K.dma('sp', cc[:], D['cc'], writes=['cc'])
    K.op('act', lambda: nc.scalar.activation(cc[:], cc[:], AF.Silu), reads=['cc'], writes=['cc'])
    pT = K.ps('pT', [128, 8, 2])
    for k in range(8):
        K.op('pe', lambda k=k: nc.tensor.transpose(pT[:, k, :], cc[:, k * 128:(k + 1) * 128], ident[0:2, 0:2]),
             reads=['cc', 'ident'], writes=['pT'])
    cT = K.sb('cT', [128, 8, 2])
    K.op('dve', lambda: nc.vector.tensor_copy(cT[:], pT[:]), reads=['pT'], writes=['cT'])
    mods = K.sb('mods', [2, 6144])
    ab = K.sb('ab', [2, 6144])
    K.dma('sp', ab[0:1, :], D['ada_b'][l:l + 1, :], writes=['ab'])
    K.dma('sp', ab[1:2, :], D['ada_b'][l:l + 1, :], writes=['ab'])
    gv = K.sb('gv', [1, 2048])
    K.dma('sp', gv[:], D['norm_g'][l:l + 1, :], writes=['gv'])
    wbuf = [K.sb(f'adaw{i}', [128, 8, 512]) for i in range(2)]
    pm = [K.ps(f'pm{i}', [2, 512]) for i in range(2)]
    for cch in range(12):
        wb = wbuf[cch % 2]
        wk = f'adaw{cch % 2}'
        K.dma('sp', wb[:], D['ada_w'][l, :, cch * 512:(cch + 1) * 512].rearrange("(k p) n -> p k n", p=128), writes=[wk])
        pk = f'pm{cch % 2}'
        for k in range(8):
            K.op('pe', lambda k=k, wb=wb, p=pm[cch % 2]: nc.tensor.matmul(p[:], cT[:, k, :], wb[:, k, :], start=(k == 0), stop=(k == 7)),
                 reads=['cT', wk], writes=[pk])
        K.op('dve', lambda p=pm[cch % 2], cch=cch: nc.vector.tensor_tensor(mods[:, cch * 512:(cch + 1) * 512], p[:], ab[:, cch * 512:(cch + 1) * 512], ALU.add),
             reads=[pk, 'ab'], writes=['mods'])
    for j in (1, 4):
        K.op('dve', lambda j=j: nc.vector.tensor_scalar(mods[:, j * 1024:(j + 1) * 1024], mods[:, j * 1024:(j + 1) * 1024], 1.0, None, ALU.add),
             reads=['mods'], writes=['mods'])
    pb = [K.ps(f'pb{i}', [128, 512]) for i in range(2)]
    gbc = K.sb('gbc', [128, 2048])
    n = 0
    for q in range(4):
        K.op('pe', lambda q=q, p=pb[n % 2]: nc.tensor.matmul(p[:], sel[0:1, 0:128], gv[:, q * 512:(q + 1) * 512], start=True, stop=True),
             reads=['sel', 'gv'], writes=[f'pb{n % 2}'])
        K.op('act', lambda q=q, p=pb[n % 2]: nc.scalar.copy(gbc[:, q * 512:(q + 1) * 512], p[:]), reads=[f'pb{n % 2}'], writes=['gbc'])
        n += 1
    for wi, w in enumerate(['l', 'c']):
        for mj in range(6):
            tj = {0: 1, 1: 0, 2: 2, 3: 4, 4: 3, 5: 5}[mj]
            if (w, tj) not in bc:
                continue
            for hh in range(2):
                K.op('pe', lambda p=pb[n % 2], wi=wi, mj=mj, hh=hh: nc.tensor.matmul(
                    p[:], sel[0:2, wi * 128:(wi + 1) * 128], mods[:, mj * 1024 + hh * 512: mj * 1024 + (hh + 1) * 512], start=True, stop=True),
                    reads=['sel', 'mods'], writes=[f'pb{n % 2}'])
                dst = bc[(w, tj)][:, hh * 512:(hh + 1) * 512]
                if tj in (0, 3):
                    goff = 0 if tj == 0 else 1024
                    K.op('dve', lambda p=pb[n % 2], dst=dst, goff=goff, hh=hh: nc.vector.tensor_tensor(
                        dst, p[:], gbc[:, goff + hh * 512: goff + (hh + 1) * 512], ALU.mult),
                        reads=[f'pb{n % 2}', 'gbc'], writes=[f'bc{w}{tj}'])
                else:
                    K.op('act', lambda p=pb[n % 2], dst=dst: nc.scalar.copy(dst, p[:]), reads=[f'pb{n % 2}'], writes=[f'bc{w}{tj}'])
                n += 1
    for (w, j), t in bc.items():
        K.dma('sp', D['bcd'][0 if w == 'l' else 1, j], t[:], reads=[f'bc{w}{j}'], writes=[('bcd', w, j)])
    K.pop()
    return None


def load_bc(K, D, w, j):
    t = K.sb(f'bc{w}{j}', [128, 1024])
    K.dma('sp', t[:], D['bcd'][0 if w == 'l' else 1, j], writes=[f'bc{w}{j}'])
    return t


def norm_mod(K, nc, xt, xkey, ht, hkey, gm, gmkey, sh, shkey, scr, tag):
    ss, rs, junk = scr
    K.op('act', lambda: nc.scalar.activation(junk[:], xt[:], AF.Square, accum_out=ss[:]), reads=[xkey], writes=[f'junk{tag}', f'ss{tag}'])
    K.op('dve', lambda: nc.vector.tensor_scalar(rs[:], ss[:], 1.0 / 1024.0, 1e-6, ALU.mult, ALU.add), reads=[f'ss{tag}'], writes=[f'rs{tag}'])
    K.op('act', lambda: nc.scalar.activation(rs[:], rs[:], AF.Sqrt), reads=[f'rs{tag}'], writes=[f'rs{tag}'])
    K.op('dve', lambda: nc.vector.reciprocal(rs[:], rs[:]), reads=[f'rs{tag}'], writes=[f'rs{tag}'])
    K.op('dve', lambda: nc.vector.scalar_tensor_tensor(ht[:], xt[:], rs[:], gm[:], ALU.mult, ALU.mult),
         reads=[xkey, f'rs{tag}', gmkey], writes=[hkey])
    K.op('dve', lambda: nc.vector.tensor_tensor(ht[:], ht[:], sh[:], ALU.add), reads=[hkey, shkey], writes=[hkey])


def stage_inproj(K, nc, D, cfg, l, C, bc, with_ctx):
    K.push()
    ident = C['ident']
    bc = {(w, j): load_bc(K, D, w, j) for w in 'lc' for j in (0, 1)}
    NB = 11
    tiles = list(range(cfg.TT))
    hT = K.sb('hT', [128, NB, 8, 128], MM)
    wst = [K.sb(f'wst{i}', [128, 8, 512]) for i in range(2)]
    xt = [K.sb(f'xt{i}', [128, 1024]) for i in range(2)]
    ht = [K.sb(f'ht{i}', [128, 1024]) for i in range(2)]
    scr = [(K.sb(f'ss{i}', [128, 1]), K.sb(f'rs{i}', [128, 1]), K.sb(f'junk{i}', [128, 1024])) for i in range(2)]
    ptr = [K.ps(f'ptr{i}', [128, 1024]) for i in range(2)]
    wb = [K.sb(f'win{i}', [128, 8, 512], MM) for i in range(2)]
    po = [K.ps(f'po{i}', [128, 512]) for i in range(2)]
    ot = [K.sb(f'ot{i}', [128, 512]) for i in range(3)]
    nw = 0
    no = 0
    for b0 in range(0, len(tiles), NB):
        blk = tiles[b0:b0 + NB]
        for bi, tt in enumerate(blk):
            i = tt % 2
            K.dma('sp', xt[i][:], xsrc(D, cfg, l, tt), writes=[f'xt{i}'])
            w = 'c' if tt < CT else 'l'
            norm_mod(K, nc, xt[i], f'xt{i}', ht[i], f'ht{i}', bc[(w, 0)], f'bc{w}0', bc[(w, 1)], f'bc{w}1', scr[i], i)
            for k in range(8):
                K.op('pe', lambda k=k, i=i: nc.tensor.transpose(ptr[i][:, k * 128:(k + 1) * 128], ht[i][:, k * 128:(k + 1) * 128], ident[:]),
                     reads=[f'ht{i}', 'ident'], writes=[f'ptr{i}'])
            K.op('act', lambda i=i, bi=bi: nc.scalar.copy(hT[:, bi, :, :], ptr[i][:].rearrange("p (k t) -> p k t", k=8)),
                 reads=[f'ptr{i}'], writes=[('hT', bi)])
        for cch in range(IN_W // 512):
            wi = nw % 2
            nw += 1
            K.dma('sp', wst[wi][:], D['w_in'][l, :, cch * 512:(cch + 1) * 512].rearrange("(k p) n -> p k n", p=128), writes=[f'wst{wi}'])
            K.op('pool', lambda wi=wi: nc.gpsimd.tensor_copy(wb[wi][:], wst[wi][:]), reads=[f'wst{wi}'], writes=[f'win{wi}'])
            for bi, tt in enumerate(blk):
                pi = no % 2
                oi = no % 3
                no += 1
                for k in range(8):
                    K.op('pe', lambda k=k, bi=bi, pi=pi, wi=wi: nc.tensor.matmul(po[pi][:], hT[:, bi, k, :], wb[wi][:, k, :], start=(k == 0), stop=(k == 7)),
                         reads=[('hT', bi), f'win{wi}'], writes=[f'po{pi}'])
                K.op('act', lambda pi=pi, oi=oi: nc.scalar.copy(ot[oi][:], po[pi][:]), reads=[f'po{pi}'], writes=[f'ot{oi}'])
                K.dma('pool', D['pl'][tt * 128:(tt + 1) * 128, cch * 512:(cch + 1) * 512], ot[oi][:], reads=[f'ot{oi}'], writes=[('pl', tt, cch)])
    K.pop()


def build(L, depth, stages=('mods', 'inproj'), nlayers=None):
    cfg = Cfg(L, depth)
    nc = bass.Bass("TRN2", target_bir_lowering=False)
    D = declare_io(nc, cfg)
    with ExitStack() as es:
        K = Sched(nc, es)
        C = stage_consts(K, nc, D)
        if BF_TABLES and 'peer' in stages:
            stage_tables(K, nc, D, cfg)
        for l in range(depth if nlayers is None else nlayers):
            with_ctx = l < depth - 1
            K.push()
            bc = stage_mods(K, nc, D, cfg, l, C, with_ctx)
            if 'inproj' in stages:
                stage_inproj(K, nc, D, cfg, l, C, bc, with_ctx)
            if 'prepass' in stages:
                stage_prepass(K, nc, D, cfg, l, C, with_ctx)
            if 'diff' in stages:
                stage_diff(K, nc, D, cfg, l, C, with_ctx)
            if 'na' in stages:
                stage_na(K, nc, D, cfg, l, C, with_ctx)
            if 'ssm' in stages:
                stage_ssm(K, nc, D, cfg, l, C, with_ctx)
            if 'merge' in stages:
                stage_merge(K, nc, D, cfg, l, C, with_ctx)
            if 'peer' in stages:
                stage_peer(K, nc, D, cfg, l, C, with_ctx, final=(l == depth - 1))
            K.pop()
        K.finish()
    return nc


def na_pair_info(R):
    types = {}
    pairs = []
    for r in range(0, R, 2):
        base = int(np.clip(r - 4, 0, R - 10))
        ws0 = int(np.clip(r - 4, 0, R - 8))
        ws1 = int(np.clip(r + 1 - 4, 0, R - 8))
        key = (r - base, ws0 - base, ws1 - base)
        if key not in types:
            types[key] = len(types)
        pairs.append((r, base, types[key]))
    return pairs, list(types.keys())


def host_nab(rpb, R):
    pairs, types = na_pair_info(R)
    H = rpb.shape[0]
    cols = np.arange(64)
    col_start = np.clip(cols - 8, 0, 48)
    col_ok = (cols[None, :] >= col_start[:, None]) & (cols[None, :] < col_start[:, None] + 16)
    dc = np.clip(cols[None, :] - cols[:, None] + 15, 0, 30)
    out = np.full((H, len(types) * 5, 128, 128), -30000.0, np.float32)
    for ti, (dr0, w0, w1) in enumerate(types):
        for j in range(5):
            for kk in range(2):
                krel = 2 * j + kk
                for qq in range(2):
                    ws = (w0, w1)[qq]
                    if not (ws <= krel < ws + 8):
                        continue
                    dr = krel - (dr0 + qq) + 7
                    blk = np.where(col_ok, rpb[:, dr][:, dc], np.float32(-30000.0))
                    out[:, ti * 5 + j, kk * 64:(kk + 1) * 64, qq * 64:(qq + 1) * 64] = np.transpose(blk, (0, 2, 1))
    return out


def host_rope(L):
    t = np.arange(L)
    freqs = (10000.0 ** (-np.arange(16, dtype=np.float32) / 16)).astype(np.float32)
    ang_r = (t // 64).astype(np.float32)[:, None] * freqs
    ang_c = (t % 64).astype(np.float32)[:, None] * freqs
    ang = np.concatenate([ang_r, ang_c], 1).astype(np.float32)
    return np.concatenate([np.cos(ang), np.sin(ang)], 1).astype(np.float32)


def stage_prepass(K, nc, D, cfg, l, C, with_ctx):
    K.push()
    ident = C['ident']
    qk = [K.sb(f'qk{i}', [128, 1024]) for i in range(2)]
    qr = [K.sb(f'qr{i}', [128, 1024]) for i in range(2)]
    cs = [K.sb(f'cs{i}', [128, 64]) for i in range(2)]
    tmp = [K.sb(f'rt{i}', [128, 512]) for i in range(4)]
    nqk = [K.sb(f'nqk{i}', [128, 512]) for i in range(2)]
    pq = [K.ps(f'pq{i}', [128, 1024]) for i in range(2)]
    pn = [K.ps(f'pn{i}', [128, 512]) for i in range(2)]
    sq = [K.sb(f'sq{i}', [128, 1024]) for i in range(2)]
    sn = [K.sb(f'sn{i}', [128, 512]) for i in range(2)]
    for tt in range(cfg.TT):
        i = tt % 2
        rows = slice(tt * 128, (tt + 1) * 128)
        K.dma('sp', qk[i][:], D['pl'][rows, 256:1280], writes=[f'qk{i}'])
        K.dma('sp', nqk[i][:], D['pl'][rows, 1792:2304], writes=[f'nqk{i}'])
        src = qk[i]
        skey = f'qk{i}'
        if tt >= CT:
            K.dma('sp', cs[i][:], D['rope'][(tt - CT) * 128:(tt - CT + 1) * 128, :], writes=[f'cs{i}'])
            v = qk[i][:].rearrange("p (g rc x f) -> p g rc x f", g=16, rc=2, x=2, f=16)
            o = qr[i][:].rearrange("p (g rc x f) -> p g rc x f", g=16, rc=2, x=2, f=16)
            x1, x2 = v[:, :, :, 0, :], v[:, :, :, 1, :]
            cosb = cs[i][:, 0:32].rearrange("p (rc f) -> p rc f", rc=2).unsqueeze(1).to_broadcast([128, 16, 2, 16])
            sinb = cs[i][:, 32:64].rearrange("p (rc f) -> p rc f", rc=2).unsqueeze(1).to_broadcast([128, 16, 2, 16])
            tv = [t[:].rearrange("p (g rc f) -> p g rc f", g=16, rc=2, f=16) for t in tmp]
            rk = [skey, f'cs{i}']
            K.op('dve', lambda: nc.vector.tensor_tensor(tv[0], x1, cosb, ALU.mult), reads=rk, writes=['rt0'])
            K.op('dve', lambda: nc.vector.tensor_tensor(tv[1], x2, sinb, ALU.mult), reads=rk, writes=['rt1'])
            K.op('pool', lambda: nc.gpsimd.tensor_tensor(tv[2], x2, cosb, ALU.mult), reads=rk, writes=['rt2'])
            K.op('pool', lambda: nc.gpsimd.tensor_tensor(tv[3], x1, sinb, ALU.mult), reads=rk, writes=['rt3'])
            K.op('dve', lambda: nc.vector.tensor_tensor(o[:, :, :, 0, :], tv[0], tv[1], ALU.subtract), reads=['rt0', 'rt1'], writes=[f'qr{i}'])
            K.op('pool', lambda: nc.gpsimd.tensor_tensor(o[:, :, :, 1, :], tv[2], tv[3], ALU.add), reads=['rt2', 'rt3'], writes=[f'qr{i}'])
            src = qr[i]
            skey = f'qr{i}'
        for k in range(8):
            K.op('pe', lambda k=k, src=src: nc.tensor.transpose(pq[i][:, k * 128:(k + 1) * 128], src[:, k * 128:(k + 1) * 128], ident[:]),
                 reads=[skey, 'ident'], writes=[f'pq{i}'])
        K.op('act', lambda: nc.scalar.copy(sq[i][:], pq[i][:]), reads=[f'pq{i}'], writes=[f'sq{i}'])
        K.dma('pool', D['qkT'][:, :, rows].rearrange("b p t -> p b t"), sq[i][:].rearrange("p (b t) -> p b t", b=8),
              reads=[f'sq{i}'], writes=[('qkT', tt)])
        for k in range(4):
            K.op('pe', lambda k=k: nc.tensor.transpose(pn[i][:, k * 128:(k + 1) * 128], nqk[i][:, k * 128:(k + 1) * 128], ident[:]),
                 reads=[f'nqk{i}', 'ident'], writes=[f'pn{i}'])
        K.op('act', lambda: nc.scalar.copy(sn[i][:], pn[i][:]), reads=[f'pn{i}'], writes=[f'sn{i}'])
        K.dma('pool', D['nqkT'][:, :, rows].rearrange("b p t -> p b t"), sn[i][:].rearrange("p (b t) -> p b t", b=4),
              reads=[f'sn{i}'], writes=[('nqkT', tt)])
    K.pop()


def bcast_row(K, nc, C, row, rowkey, dst, dstkey, ncols, ps, pskey, scale=None):
    sel = C['sel']
    K.op('pe', lambda: nc.tensor.matmul(ps[:, 0:ncols], sel[0:1, 0:128], row, start=True, stop=True), reads=['sel', rowkey], writes=[pskey])
    if scale is None:
        K.op('dve', lambda: nc.vector.tensor_copy(dst, ps[:, 0:ncols]), reads=[pskey], writes=[dstkey])
    else:
        K.op('dve', lambda: nc.vector.tensor_scalar(dst, ps[:, 0:ncols], float(scale), None, ALU.mult), reads=[pskey], writes=[dstkey])


def stage_diff(K, nc, D, cfg, l, C, with_ctx):
    K.push()
    TT, NT = cfg.TT, cfg.NT
    NTOK = TT * 128
    lam_init = LAM_INIT[l]
    dl = K.sb('dl', [1, 256])
    K.dma('sp', dl[:], D['diff_lambda'][l:l + 1, :], writes=['dl'])
    pr = K.sb('pr', [1, 2, 64])
    dv = dl[:].rearrange("o (a b f) -> o a b f", a=2, b=2, f=64)
    K.op('dve', lambda: nc.vector.tensor_tensor(pr[:], dv[:, :, 0, :], dv[:, :, 1, :], ALU.mult), reads=['dl'], writes=['pr'])
    e2 = K.sb('e2', [1, 2])
    K.op('dve', lambda: nc.vector.reduce_sum(e2[:], pr[:], axis=AX.X), reads=['pr'], writes=['e2'])
    K.op('act', lambda: nc.scalar.activation(e2[:], e2[:], AF.Exp), reads=['e2'], writes=['e2'])
    nl = K.sb('nl', [1, 1])
    K.op('dve', lambda: nc.vector.tensor_tensor(nl[:], e2[:, 1:2], e2[:, 0:1], ALU.subtract), reads=['e2'], writes=['nl'])
    K.op('dve', lambda: nc.vector.tensor_scalar(nl[:], nl[:], -lam_init, None, ALU.add), reads=['nl'], writes=['nl'])
    psm = K.ps('psm', [128, 512])
    neglam = K.sb('neglam', [128, 1])
    bcast_row(K, nc, C, nl[:], 'nl', neglam[:], 'neglam', 1, psm, 'psm')
    sg = K.sb('sg', [1, 128])
    K.dma('sp', sg[:], D['diff_subln_g'][l:l + 1, :], writes=['sg'])
    gbc = K.sb('dgbc', [128, 128])
    bcast_row(K, nc, C, sg[:], 'sg', gbc[:], 'dgbc', 128, psm, 'psm', scale=1.0 - lam_init)
    qT = K.sb('dqT', [128, NTOK], BF16 if QK_BF16 else MM)
    kT = K.sb('dkT', [128, NTOK], BF16 if QK_BF16 else MM)
    va = K.sb('dva', [128, TT, 130], BF16 if AV_BF16 else MM)
    stg = [K.sb(f'dstg{i}', [128, 768]) for i in range(2)]
    nstg = [0]
    onez = K.sb('onez', [128, TT, 2])
    K.op('pool', lambda: nc.gpsimd.memset(onez[:, :, 0:1], 1.0), writes=['onez'])
    K.op('pool', lambda: nc.gpsimd.memset(onez[:, :, 1:2], 0.0), writes=['onez'])

    def load_round(dst_ap, src_ap, key, ncols):
        si = nstg[0] % 2
        nstg[0] += 1
        K.dma('sp', stg[si][:, 0:ncols], src_ap, writes=[f'dstg{si}'])
        eng = 'dve' if si == 0 else 'pool'
        cp = nc.vector.tensor_copy if si == 0 else nc.gpsimd.tensor_copy
        K.op(eng, lambda: cp(dst_ap, stg[si][:, 0:ncols]), reads=[f'dstg{si}'], writes=[key])
    st = [K.ps(f'dst{i}', [128, 512]) for i in range(2)]
    pt = [K.sb(f'dpt{i}', [128, 512], BF16 if AV_BF16 else MM) for i in range(2)]
    acc = [K.ps(f'dacc{i}', [128, 512]) for i in range(4)]
    om = [K.sb(f'dom{i}', [128, 4, 128]) for i in range(2)]
    rec = K.sb('drec', [128, 4])
    dd = K.sb('ddd', [128, 4, 128])
    yy = K.sb('dyy', [128, 4, 128])
    ss = K.sb('dss', [128, 4])
    junk = K.sb('djunk', [128, 128])
    n = 0
    for h in range(4):
        for c0 in range(0, NTOK, 768):
            c1 = min(NTOK, c0 + 768)
            load_round(qT[:, c0:c1], D['qkT'][h, :, c0:c1], 'dqT', c1 - c0)
            load_round(kT[:, c0:c1], D['qkT'][4 + h, :, c0:c1], 'dkT', c1 - c0)
        for t0 in range(0, TT, 6):
            t1 = min(TT, t0 + 6)
            si = nstg[0] % 2
            nstg[0] += 1
            sv_ = stg[si][:, 0:(t1 - t0) * 128].rearrange("p (t c) -> p t c", c=128)
            K.dma('sp', sv_, D['pl'][t0 * 128:t1 * 128, 1280 + h * 128:1280 + (h + 1) * 128].rearrange("(t p) c -> p t c", p=128), writes=[f'dstg{si}'])
            if si == 0:
                K.op('dve', lambda: nc.vector.tensor_copy(va[:, t0:t1, 0:128], sv_), reads=[f'dstg{si}'], writes=['dva'])
            else:
                K.op('pool', lambda: nc.gpsimd.tensor_copy(va[:, t0:t1, 0:128], sv_), reads=[f'dstg{si}'], writes=['dva'])
        K.op('dve', lambda: nc.vector.tensor_copy(va[:, :, 128:130], onez[:]), reads=['onez'], writes=['dva'])
        blocks = [(CT * 128 + qb * 512, 512, list(range(TT))) for qb in range(NT // 4)]
        if with_ctx:
            blocks.append((0, 256, [0, 1]))
        for (q0, N, kts) in blocks:
            nq = N // 128
            for m in range(2):
                ms = slice(m * 64, (m + 1) * 64)

                def S(kt, n):
                    K.op('pe', lambda: nc.tensor.matmul(st[n % 2][:, 0:N], kT[ms, kt * 128:(kt + 1) * 128], qT[ms, q0:q0 + N], start=True, stop=True),
                         reads=['dkT', 'dqT'], writes=[f'dst{n % 2}'])
                    K.op('act', lambda: nc.scalar.activation(pt[n % 2][:, 0:N], st[n % 2][:, 0:N], AF.Exp, scale=0.125),
                         reads=[f'dst{n % 2}'], writes=[f'dpt{n % 2}'])

                def AV(kt, n, first, last):
                    for qs in range(nq):
                        K.op('pe', lambda qs=qs: nc.tensor.matmul(acc[qs][:, 0:130], pt[n % 2][:, qs * 128:(qs + 1) * 128], va[:, kt, :], start=first, stop=last),
                             reads=[f'dpt{n % 2}', 'dva'], writes=[f'dacc{qs}'])
                S(kts[0], n)
                for ki, kt in enumerate(kts):
                    if ki + 1 < len(kts):
                        S(kts[ki + 1], n + 1)
                    AV(kt, n, ki == 0, ki == len(kts) - 1)
                    n += 1
                for qs in range(nq):
                    K.op('dve', lambda qs=qs: nc.vector.reciprocal(rec[:, qs:qs + 1], acc[qs][:, 128:129]), reads=[f'dacc{qs}'], writes=['drec'])
                    K.op('dve', lambda qs=qs: nc.vector.tensor_scalar(om[m][:, qs, :], acc[qs][:, 0:128], rec[:, qs:qs + 1], None, ALU.mult),
                         reads=[f'dacc{qs}', 'drec'], writes=[f'dom{m}'])
            for qs in range(nq):
                K.op('dve', lambda qs=qs: nc.vector.scalar_tensor_tensor(dd[:, qs, :], om[1][:, qs, :], neglam[:], om[0][:, qs, :], ALU.mult, ALU.add),
                     reads=['dom0', 'dom1', 'neglam'], writes=['ddd'])
                K.op('act', lambda qs=qs: nc.scalar.activation(junk[:], dd[:, qs, :], AF.Square, accum_out=ss[:, qs:qs + 1]), reads=['ddd'], writes=['djunk', 'dss'])
            K.op('dve', lambda: nc.vector.tensor_scalar(ss[:, 0:nq], ss[:, 0:nq], 1.0 / 128.0, 1e-6, ALU.mult, ALU.add), reads=['dss'], writes=['dss'])
            K.op('act', lambda: nc.scalar.activation(ss[:, 0:nq], ss[:, 0:nq], AF.Sqrt), reads=['dss'], writes=['dss'])
            K.op('dve', lambda: nc.vector.reciprocal(ss[:, 0:nq], ss[:, 0:nq]), reads=['dss'], writes=['dss'])
            for qs in range(nq):
                K.op('dve', lambda qs=qs: nc.vector.scalar_tensor_tensor(yy[:, qs, :], dd[:, qs, :], ss[:, qs:qs + 1], gbc[:], ALU.mult, ALU.mult),
                     reads=['ddd', 'dss', 'dgbc'], writes=['dyy'])
            K.dma('pool', D['ymix'][q0:q0 + N, 256 + h * 128:256 + (h + 1) * 128].rearrange("(s p) c -> p s c", p=128), yy[:, 0:nq, :],
                  reads=['dyy'], writes=[('ymix', 'd', h, q0)])
            K.maybe_barrier()
    K.pop()


def stage_na(K, nc, D, cfg, l, C, with_ctx):
    K.push()
    TT, NT, R = cfg.TT, cfg.NT, cfg.R
    NTOK = TT * 128
    pairs, types = na_pair_info(R)
    ident = C['ident']
    id8 = K.sb('id8', [128, 128])
    K.op('dve', lambda: nc.vector.tensor_scalar(id8[:], ident[:], 8.0, None, ALU.mult), reads=['ident'], writes=['id8'])
    nq = K.sb('nq', [64, NTOK])
    nk = K.sb('nk', [64, NTOK])
    nv = K.sb('nv', [128, TT, 65])
    nab = K.sb('nab', [128, len(types) * 5, 128])
    S = [K.ps(f'nS{i}', [128, 1024]) for i in range(2)]
    P = [K.sb(f'nP{i}', [128, 1024]) for i in range(2)]
    acc = [K.ps(f'nacc{i}', [128, 512]) for i in range(2)]
    rec = K.sb('nrec', [128, 1])
    ysb = [K.sb(f'nys{i}', [128, 64]) for i in range(2)]
    n = 0
    for h in range(4):
        K.dma('sp', nq[:], D['nqkT'][h // 2, (h % 2) * 64:(h % 2 + 1) * 64, :], writes=['nq'])
        K.dma('sp', nk[:], D['nqkT'][2 + h // 2, (h % 2) * 64:(h % 2 + 1) * 64, :], writes=['nk'])
        for t0 in range(0, TT, 6):
            t1 = min(TT, t0 + 6)
            K.dma('sp', nv[:, t0:t1, 0:64], D['pl'][t0 * 128:t1 * 128, 2304 + h * 64:2304 + (h + 1) * 64].rearrange("(t p) c -> p t c", p=128), writes=['nv'])
        K.op('pool', lambda: nc.gpsimd.memset(nv[:, :, 64:65], 1.0), writes=['nv'])
        K.dma('sp', nab[:], D['nab'][l, h].rearrange("j k q -> k j q"), writes=['nab'])
        jobs = []
        for (r, base, ty) in pairs:
            tl = [(CT + (base + 2 * j) // 2, ty * 5 + j) for j in range(5)] + [(0, None), (1, None)]
            jobs.append((CT * 128 + r * 64, tl))
        if with_ctx:
            for qt in range(2):
                jobs.append((qt * 128, [(0, None), (1, None)]))
        for (q0, tl) in jobs:
            i = n % 2
            n += 1
            nt = len(tl)
            for j, (kt, bj) in enumerate(tl):
                K.op('pe', lambda j=j, kt=kt, bj=bj: nc.tensor.matmul(S[i][:, j * 128:(j + 1) * 128], nk[:, kt * 128:(kt + 1) * 128], nq[:, q0:q0 + 128],
                                                                  start=True, stop=(bj is None)),
                     reads=['nk', 'nq'], writes=[f'nS{i}'])
                if bj is not None:
                    K.op('pe', lambda j=j, bj=bj: nc.tensor.matmul(S[i][:, j * 128:(j + 1) * 128], id8[:], nab[:, bj, :], start=False, stop=True),
                         reads=['id8', 'nab'], writes=[f'nS{i}'])
            for c0 in range(0, nt * 128, 512):
                c1 = min(nt * 128, c0 + 512)
                K.op('act', lambda c0=c0, c1=c1: nc.scalar.activation(P[i][:, c0:c1], S[i][:, c0:c1], AF.Exp, scale=0.125),
                     reads=[f'nS{i}'], writes=[f'nP{i}'])
            for j, (kt, bj) in enumerate(tl):
                K.op('pe', lambda j=j, kt=kt: nc.tensor.matmul(acc[i][:, 0:65], P[i][:, j * 128:(j + 1) * 128], nv[:, kt, :], start=(j == 0), stop=(j == nt - 1)),
                     reads=[f'nP{i}', 'nv'], writes=[f'nacc{i}'])
            K.op('dve', lambda: nc.vector.reciprocal(rec[:], acc[i][:, 64:65]), reads=[f'nacc{i}'], writes=['nrec'])
            K.op('dve', lambda: nc.vector.tensor_scalar(ysb[i][:], acc[i][:, 0:64], rec[:], None, ALU.mult), reads=[f'nacc{i}', 'nrec'], writes=[f'nys{i}'])
            K.dma('pool', D['ymix'][q0:q0 + 128, 768 + h * 64:768 + (h + 1) * 64], ysb[i][:], reads=[f'nys{i}'], writes=[('ymix', 'n', h, q0)])
    K.pop()


def core_inputs(inp, b, L, depth, half=0):
    R = L // 64
    sel = np.zeros((2, 256), np.float32)
    sel[0, :128] = 1
    sel[1, 128:] = 1
    f = lambda a: np.ascontiguousarray(a, dtype=np.float32)
    hs = [host_ssm(inp, l) for l in range(depth)]
    im = dict(
        x=inp['x'][b, :L], ctx=inp['ctx'][b], cc=np.stack([inp['c'][b], inp['c_ctx']]),
        ada_w=inp['ada_w'][:depth], ada_b=inp['ada_b'][:depth],
        norm_g=np.concatenate([inp['norm1_g'], inp['norm2_g']], 1)[:depth],
        w_in=inp['w_in'][:depth], ident=np.eye(128, dtype=np.float32), sel=sel,
        rope=host_rope(L), nab=np.stack([host_nab(inp['na_rpb'][l], R) for l in range(depth)]),
        diff_lambda=inp['diff_lambda'][:depth].reshape(depth, 256), diff_subln_g=inp['diff_subln_g'][:depth],
        jmat=np.eye(128, dtype=np.float32)[::-1],
        ssm_sc=np.stack([hs[l][0] for l in range(depth)]), ssm_b=np.stack([hs[l][1] for l in range(depth)]),
        ssm_c=np.stack([hs[l][2] for l in range(depth)]),
        ssm_dT=np.stack([inp['ssm_d'][l].reshape(2, 128).T for l in range(depth)]), ssm_glu_w=inp['ssm_glu_w'][:depth],
        w_br_ssm=inp['w_br_ssm'][:depth], w_br_diff=inp['w_br_diff'][:depth], w_br_na=inp['w_br_na'][:depth], w_out=inp['w_out'][:depth],
        peer_wq=inp['peer_wq'][:depth], peer_sk=inp['peer_subkeys'][:depth].reshape(depth, 16, 128, 128),
        final_g=inp['final_norm_g'].reshape(1, 1024),
        iota16=np.tile(np.arange(16, dtype=np.float32), (128, 1)),
    )
    out = {k: f(v) for k, v in im.items()}
    if PAIR_SPLIT:
        nt2 = (L // 128) // 2
        out['rowidx'] = np.ascontiguousarray((CT * 128 + half * (L // 2) + np.arange(nt2)[None, :] * 128 + np.arange(128)[:, None]).astype(np.int32))
    im = {}
    for i in range(depth):
        im[f'peer_u{i}'] = inp['peer_u'][i]
        im[f'peer_v{i}'] = inp['peer_v'][i]
    out.update({k: f(v) for k, v in im.items()})
    return out


TWO_PI = float(2 * np.pi)


def host_ssm(inp, l):
    lre, lim, ls = inp['ssm_lambda_re'][l], inp['ssm_lambda_im'][l], inp['ssm_log_step'][l]
    sc = np.zeros((128, 48), np.float32)
    bb = np.zeros((2, 16, 128, 128), np.float32)
    cm = np.zeros((2, 16, 128, 128), np.float32)
    for d in range(2):
        for st in range(8):
            col = (d * 8 + st) * 3
            for gl in range(2):
                g = 2 * st + gl
                rows = slice(gl * 64, (gl + 1) * 64)
                sc[rows, col + 0] = lre[d, g]
                sc[rows, col + 1] = lim[d, g]
                sc[rows, col + 2] = ls[d, g]
                c0 = (g - 8 * (st // 4)) * 16
                bb[0, d * 8 + st, rows, c0:c0 + 16] = inp['ssm_b_re'][l, d, g]
                bb[1, d * 8 + st, rows, c0:c0 + 16] = inp['ssm_b_im'][l, d, g]
                cm[0, d * 8 + st, rows, c0:c0 + 16] = inp['ssm_c_re'][l, d, g].T
                cm[1, d * 8 + st, rows, c0:c0 + 16] = inp['ssm_c_im'][l, d, g].T
    return sc, bb, cm


def gelu_tanh(K, nc, eng_a, out, x, xkey, outkey, t1, t1key):
    K.op('dve', lambda: nc.vector.tensor_tensor(t1, x, x, ALU.mult), reads=[xkey], writes=[t1key])
    K.op('dve', lambda: nc.vector.tensor_scalar(t1, t1, 0.044715, 1.0, ALU.mult, ALU.add), reads=[t1key], writes=[t1key])
    K.op('dve', lambda: nc.vector.tensor_tensor(t1, t1, x, ALU.mult), reads=[t1key, xkey], writes=[t1key])
    K.op('act', lambda: nc.scalar.activation(t1, t1, AF.Sigmoid, scale=1.5957691216057308), reads=[t1key], writes=[t1key])
    K.op('dve', lambda: nc.vector.tensor_tensor(out, x, t1, ALU.mult), reads=[xkey, t1key], writes=[outkey])


def stage_ssm(K, nc, D, cfg, l, C, with_ctx):
    K.push()
    TT, NT = cfg.TT, cfg.NT
    ident = C['ident']
    jm = K.sb('jm', [128, 128])
    K.dma('sp', jm[:], D['jmat'], writes=['jm'])
    T = 512
    sc = K.sb('ssc', [128, 16, 3])
    K.dma('sp', sc[:], D['ssm_sc'][l].rearrange("p (c j) -> p c j", j=3), writes=['ssc'])
    names = ['dtv', 'lre', 'a', 'th', 'rho', 'cos', 'sin', 'x', 'den', 'cfr', 'cfi', 'k', 'tmp', 'tmp2', 'nsin', 'ncfi']
    V = {nm: K.sb('sv_' + nm, [128, 16]) for nm in names}
    ki = K.sb('sv_ki', [128, 16], I32)
    lim = sc[:, :, 1]

    def dv(fn, reads, writes):
        K.op('dve', fn, reads=['sv_' + r if r != 'ssc' else r for r in reads], writes=['sv_' + w for w in writes])

    SV = ['svall', 'ssc']

    def d1(fn):
        K.op('dve', fn, reads=SV, writes=['svall'])

    def tt(o, a_, b_, op):
        d1(lambda: nc.vector.tensor_tensor(o, a_, b_, op))

    def ts(o, a_, s1, s2, op0, op1=None):
        if op1 is None:
            d1(lambda: nc.vector.tensor_scalar(o, a_, s1, None, op0))
        else:
            d1(lambda: nc.vector.tensor_scalar(o, a_, s1, s2, op0, op1))

    def to_int_float(dst, src):
        d1(lambda: nc.vector.tensor_copy(ki[:], src))
        d1(lambda: nc.vector.tensor_copy(dst, ki[:]))

    def horner(dst, z, coefs):
        ts(dst, z, float(coefs[-1]), 1.0, ALU.mult, ALU.add)
        for cf in reversed(coefs[:-1]):
            tt(dst, dst, z, ALU.mult)
            ts(dst, dst, float(cf), 1.0, ALU.mult, ALU.add)

    x_, k_, t_, t2_ = V['x'][:], V['k'][:], V['tmp'][:], V['tmp2'][:]
    ts(x_, sc[:, :, 2], 1.0 / 0.6931471805599453, None, ALU.mult)
    to_int_float(k_, x_)
    ts(t_, k_, -0.693359375, None, ALU.mult)
    tt(x_, sc[:, :, 2], t_, ALU.add)
    ts(t_, k_, 2.12194440e-4, None, ALU.mult)
    tt(x_, x_, t_, ALU.add)
    horner(V['dtv'][:], x_, [1.0 / i for i in range(1, 14)])
    for j in range(1, 17):
        ts(t_, k_, float(-j), -0.5, ALU.is_le, ALU.mult)
        ts(t_, t_, 1.0, None, ALU.add)
        tt(V['dtv'][:], V['dtv'][:], t_, ALU.mult)
    ts(V['lre'][:], sc[:, :, 0], -1e-4, None, ALU.min)
    tt(V['a'][:], V['lre'][:], V['dtv'][:], ALU.mult)
    tt(V['th'][:], lim, V['dtv'][:], ALU.mult)
    horner(V['rho'][:], V['a'][:], [1.0 / i for i in range(1, 10)])
    ts(x_, V['th'][:], 1.0 / TWO_PI, None, ALU.mult)
    to_int_float(k_, x_)
    ts(t_, k_, -6.28125, None, ALU.mult)
    tt(x_, V['th'][:], t_, ALU.add)
    ts(t_, k_, -1.9353071795864769e-3, None, ALU.mult)
    tt(x_, x_, t_, ALU.add)
    for sgn, cmpop, thr in ((-1.0, ALU.is_gt, float(np.pi)), (1.0, ALU.is_lt, -float(np.pi))):
        ts(t_, x_, thr, None, cmpop)
        ts(t2_, t_, sgn * 6.28125, None, ALU.mult)
        tt(x_, x_, t2_, ALU.add)
        ts(t2_, t_, sgn * 1.9353071795864769e-3, None, ALU.mult)
        tt(x_, x_, t2_, ALU.add)
    ts(x_, x_, 0.25, None, ALU.mult)
    tt(k_, x_, x_, ALU.mult)
    horner(V['cos'][:], k_, [-1.0 / ((2 * i) * (2 * i - 1)) for i in range(1, 9)])
    horner(V['sin'][:], k_, [-1.0 / ((2 * i) * (2 * i + 1)) for i in range(1, 9)])
    tt(V['sin'][:], V['sin'][:], x_, ALU.mult)
    for _ in range(2):
        tt(t_, V['cos'][:], V['cos'][:], ALU.mult)
        tt(t2_, V['sin'][:], V['sin'][:], ALU.mult)
        tt(V['sin'][:], V['sin'][:], V['cos'][:], ALU.mult)
        ts(V['sin'][:], V['sin'][:], 2.0, None, ALU.mult)
        tt(V['cos'][:], t_, t2_, ALU.subtract)
    tt(t_, V['cos'][:], V['cos'][:], ALU.mult)
    tt(t2_, V['sin'][:], V['sin'][:], ALU.mult)
    tt(t_, t_, t2_, ALU.add)
    ts(t_, t_, -0.5, 1.5, ALU.mult, ALU.add)
    tt(V['cos'][:], V['cos'][:], t_, ALU.mult)
    tt(V['sin'][:], V['sin'][:], t_, ALU.mult)

    def dv(fn, reads, writes):
        K.op('dve', fn, reads=SV, writes=['svall'])

    dv(lambda: nc.vector.tensor_tensor(V['tmp'][:], V['rho'][:], V['cos'][:], ALU.mult), ['rho', 'cos'], ['tmp'])
    dv(lambda: nc.vector.tensor_scalar(V['tmp'][:], V['tmp'][:], -1.0, None, ALU.add), ['tmp'], ['tmp'])
    dv(lambda: nc.vector.tensor_tensor(V['tmp2'][:], V['rho'][:], V['sin'][:], ALU.mult), ['rho', 'sin'], ['tmp2'])
    dv(lambda: nc.vector.tensor_tensor(V['den'][:], V['lre'][:], V['lre'][:], ALU.mult), ['lre'], ['den'])
    dv(lambda: nc.vector.tensor_tensor(V['k'][:], lim, lim, ALU.mult), ['ssc'], ['k'])
    dv(lambda: nc.vector.tensor_tensor(V['den'][:], V['den'][:], V['k'][:], ALU.add), ['den', 'k'], ['den'])
    dv(lambda: nc.vector.reciprocal(V['den'][:], V['den'][:]), ['den'], ['den'])
    dv(lambda: nc.vector.tensor_tensor(V['cfr'][:], V['tmp'][:], V['lre'][:], ALU.mult), ['tmp', 'lre'], ['cfr'])
    dv(lambda: nc.vector.tensor_tensor(V['k'][:], V['tmp2'][:], lim, ALU.mult), ['tmp2', 'ssc'], ['k'])
    dv(lambda: nc.vector.tensor_tensor(V['cfr'][:], V['cfr'][:], V['k'][:], ALU.add), ['cfr', 'k'], ['cfr'])
    dv(lambda: nc.vector.tensor_tensor(V['cfr'][:], V['cfr'][:], V['den'][:], ALU.mult), ['cfr', 'den'], ['cfr'])
    dv(lambda: nc.vector.tensor_tensor(V['cfi'][:], V['tmp2'][:], V['lre'][:], ALU.mult), ['tmp2', 'lre'], ['cfi'])
    dv(lambda: nc.vector.tensor_tensor(V['k'][:], V['tmp'][:], lim, ALU.mult), ['tmp', 'ssc'], ['k'])
    dv(lambda: nc.vector.tensor_tensor(V['cfi'][:], V['cfi'][:], V['k'][:], ALU.subtract), ['cfi', 'k'], ['cfi'])
    dv(lambda: nc.vector.tensor_tensor(V['cfi'][:], V['cfi'][:], V['den'][:], ALU.mult), ['cfi', 'den'], ['cfi'])
    dv(lambda: nc.vector.tensor_scalar(V['ncfi'][:], V['cfi'][:], -1.0, None, ALU.mult), ['cfi'], ['ncfi'])
    dv(lambda: nc.vector.tensor_scalar(V['nsin'][:], V['sin'][:], -1.0, None, ALU.mult), ['sin'], ['nsin'])
    Bt = K.sb('sBt', [128, 16, 2, 128])
    Cm = K.sb('sCm', [128, 16, 2, 128])
    K.dma('sp', Cm[:, :, 0, :], D['ssm_c'][l, 0].rearrange("c p f -> p c f"), writes=['sCm'])
    K.dma('sp', Cm[:, :, 1, :], D['ssm_c'][l, 1].rearrange("c p f -> p c f"), writes=['sCm'])
    K.op('pool', lambda: nc.gpsimd.tensor_scalar(Cm[:, :, 1, :], Cm[:, :, 1, :], -1.0, None, ALU.mult), reads=['sCm'], writes=['sCm'])
    K.push()
    braw = K.sb('sbraw', [128, 16, 2, 128])
    K.dma('sp', braw[:, :, 0, :], D['ssm_b'][l, 0].rearrange("c p f -> p c f"), writes=['sbraw'])
    K.dma('sp', braw[:, :, 1, :], D['ssm_b'][l, 1].rearrange("c p f -> p c f"), writes=['sbraw'])
    bbar = [K.sb(f'sbbar{i}', [128, 2, 128]) for i in range(2)]
    t4 = [K.sb(f'sbt{i}', [128, 128]) for i in range(2)]
    pbt = [K.ps(f'spbt{i}', [128, 256]) for i in range(2)]
    for c in range(16):
        i = c % 2
        cr, ci, nci = V['cfr'][:, c:c + 1], V['cfi'][:, c:c + 1], V['ncfi'][:, c:c + 1]
        K.op('dve', lambda: nc.vector.tensor_scalar(t4[0][:], braw[:, c, 1, :], nci, None, ALU.mult), reads=['sbraw', 'svall'], writes=['sbt0'])
        K.op('dve', lambda: nc.vector.scalar_tensor_tensor(bbar[i][:, 0, :], braw[:, c, 0, :], cr, t4[0][:], ALU.mult, ALU.add),
             reads=['sbraw', 'svall', 'sbt0'], writes=[f'sbbar{i}'])
        K.op('dve', lambda: nc.vector.tensor_scalar(t4[1][:], braw[:, c, 0, :], ci, None, ALU.mult), reads=['sbraw', 'svall'], writes=['sbt1'])
        K.op('dve', lambda: nc.vector.scalar_tensor_tensor(bbar[i][:, 1, :], braw[:, c, 1, :], cr, t4[1][:], ALU.mult, ALU.add),
             reads=['sbraw', 'svall', 'sbt1'], writes=[f'sbbar{i}'])
        for ri in range(2):
            K.op('pe', lambda ri=ri: nc.tensor.transpose(pbt[i][:, ri * 128:(ri + 1) * 128], bbar[i][:, ri, :], ident[:]),
                 reads=[f'sbbar{i}', 'ident'], writes=[f'spbt{i}'])
        K.op('act', lambda: nc.scalar.copy(Bt[:, c, :, :], pbt[i][:].rearrange("p (r f) -> p r f", r=2)), reads=[f'spbt{i}'], writes=['sBt'])
    K.pop()
    E = K.sb('sE', [128, 16, 2, T])
    et = [K.sb(f'set{i}', [128, T // 2]) for i in range(2)]
    for c in range(16):
        K.op('dve', lambda: nc.vector.tensor_copy(E[:, c, 0, 0:1], V['cos'][:, c:c + 1]), reads=['svall'], writes=[('sE', c)])
        K.op('dve', lambda: nc.vector.tensor_copy(E[:, c, 1, 0:1], V['sin'][:, c:c + 1]), reads=['svall'], writes=[('sE', c)])
        m = 1
        while m < T:
            wr, wi = E[:, c, 0, m - 1:m], E[:, c, 1, m - 1:m]
            er, ei = E[:, c, 0, 0:m], E[:, c, 1, 0:m]
            K.op('dve', lambda: nc.vector.tensor_scalar(et[0][:, 0:m], ei, wi, None, ALU.mult), reads=[('sE', c)], writes=['set0'])
            K.op('dve', lambda: nc.vector.tensor_scalar(et[1][:, 0:m], ei, wr, None, ALU.mult), reads=[('sE', c)], writes=['set1'])
            K.op('dve', lambda: nc.vector.scalar_tensor_tensor(E[:, c, 0, m:2 * m], er, wr, et[0][:, 0:m], ALU.mult, ALU.subtract),
                 reads=[('sE', c), 'set0'], writes=[('sE', c)])
            K.op('dve', lambda: nc.vector.scalar_tensor_tensor(E[:, c, 1, m:2 * m], er, wi, et[1][:, 0:m], ALU.mult, ALU.add),
                 reads=[('sE', c), 'set1'], writes=[('sE', c)])
            m *= 2
    dT = K.sb('sdT', [128, 2])
    K.dma('sp', dT[:], D['ssm_dT'][l], writes=['sdT'])
    gw = K.sb('sgw', [128, 2, 512])
    K.dma('sp', gw[:], D['ssm_glu_w'][l].rearrange("(k p) n -> p k n", p=128), writes=['sgw'])
    carry = K.sb('scarry', [128, 8, 2])
    ut = [K.sb(f'sut{i}', [128, 4, 256]) for i in range(1)]
    uT = [K.sb(f'suT{i}', [128, 2, T]) for i in range(2)]
    puT = K.ps('spuT', [128, 2, T])
    pbu = [K.ps(f'spbu{i}', [128, 2, T]) for i in range(1)]
    bu = [K.sb(f'sbu{i}', [128, 2, T]) for i in range(2)]
    X = [K.sb(f'sX{i}', [128, 2, T]) for i in range(2)]
    W = [K.sb(f'sW{i}', [128, 2, T]) for i in range(2)]
    S = [K.sb(f'sS{i}', [128, 2, T]) for i in range(2)]
    tq_all = [K.sb(f'stq{i}', [128, T]) for i in range(8)]
    py = K.ps('spy', [128, 2, T])
    yTf = K.sb('syTf', [128, 2, T])
    ytok = K.sb('sytok', [128, 4, 256])
    yb = K.sb('syb', [128, 2, T])
    g1 = K.sb('sg1', [128, 2, T])
    pz = K.ps('spz', [128, T])
    zs = K.sb('szs', [128, 512])
    zo = K.sb('szo', [128, 256])
    nu = 0
    for d in range(2):
        chunks = [([0, 1], True)] + [([CT + 4 * c + i for i in range(4)], False) for c in range(NT // 4)]
        if d == 1:
            chunks = [([1, 0], True)] + [([CT + NT - 1 - (4 * c + i) for i in range(4)], False) for c in range(NT // 4)]
        for ci, (tl, is_ctx) in enumerate(chunks):
            Tc = len(tl) * 128
            ui = ci % 2
            for j, tt in enumerate(tl):
                K.dma('sp', ut[0][:, j, :], D['pl'][tt * 128:(tt + 1) * 128, 0:256], writes=['sut0'])
            for j in range(len(tl)):
                for kc in range(2):
                    K.op('pe', lambda j=j, kc=kc: nc.tensor.matmul(puT[:, kc, j * 128:(j + 1) * 128], ut[0][:, j, kc * 128:(kc + 1) * 128],
                                                                 (ident if d == 0 else jm)[:], start=True, stop=True),
                         reads=['sut0', 'ident', 'jm'], writes=['spuT'])
            K.op('act', lambda: nc.scalar.copy(uT[ui][:, :, 0:Tc], puT[:, :, 0:Tc]), reads=['spuT'], writes=[f'suT{ui}'])
            need_out = (not is_ctx) or with_ctx
            for st in range(8):
                c = d * 8 + st
                kc = st // 4
                b = nu % 2
                nu += 1
                tq = tq_all[4 * b:4 * b + 4]
                tqk = [f'stq{4 * b + i}' for i in range(4)]
                for ri in range(2):
                    K.op('pe', lambda ri=ri: nc.tensor.matmul(pbu[0][:, ri, 0:Tc], Bt[:, c, ri, :], uT[ui][:, kc, 0:Tc], start=True, stop=True),
                         reads=['sBt', f'suT{ui}'], writes=['spbu0'])
                K.op('act', lambda: nc.scalar.copy(bu[b][:, :, 0:Tc], pbu[0][:, :, 0:Tc]), reads=['spbu0'], writes=[f'sbu{b}'])
                Er, Ei = E[:, c, 0, 0:Tc], E[:, c, 1, 0:Tc]
                br, bi = bu[b][:, 0, 0:Tc], bu[b][:, 1, 0:Tc]
                ek = ('sE', c)
                K.op('dve', lambda: nc.vector.tensor_tensor(tq[0][:, 0:Tc], Er, br, ALU.mult), reads=[ek, f'sbu{b}'], writes=[tqk[0]])
                K.op('dve', lambda: nc.vector.tensor_tensor(tq[1][:, 0:Tc], Ei, bi, ALU.mult), reads=[ek, f'sbu{b}'], writes=[tqk[1]])
                K.op('dve', lambda: nc.vector.tensor_tensor(X[b][:, 0, 0:Tc], tq[0][:, 0:Tc], tq[1][:, 0:Tc], ALU.add), reads=[tqk[0], tqk[1]], writes=[f'sX{b}'])
                K.op('pool', lambda: nc.gpsimd.tensor_tensor(tq[2][:, 0:Tc], Er, bi, ALU.mult), reads=[ek, f'sbu{b}'], writes=[tqk[2]])
                K.op('pool', lambda: nc.gpsimd.tensor_tensor(tq[3][:, 0:Tc], Ei, br, ALU.mult), reads=[ek, f'sbu{b}'], writes=[tqk[3]])
                K.op('pool', lambda: nc.gpsimd.tensor_tensor(X[b][:, 1, 0:Tc], tq[2][:, 0:Tc], tq[3][:, 0:Tc], ALU.subtract), reads=[tqk[2], tqk[3]], writes=[f'sX{b}'])
                rho_b = V['rho'][:, c:c + 1].to_broadcast([128, Tc])
                for ri in range(2):
                    init = 0.0 if ci == 0 else carry[:, st, ri:ri + 1]
                    K.op('dve', lambda ri=ri, init=init: nc.vector.tensor_tensor_scan(W[b][:, ri, 0:Tc], rho_b, X[b][:, ri, 0:Tc], init, ALU.mult, ALU.add),
                         reads=[f'sX{b}', 'svall', ('scarry', st)], writes=[f'sW{b}'])
                wr_, wi_ = W[b][:, 0, 0:Tc], W[b][:, 1, 0:Tc]
                K.op('dve', lambda: nc.vector.tensor_tensor(tq[0][:, 0:Tc], Er, wr_, ALU.mult), reads=[ek, f'sW{b}'], writes=[tqk[0]])
                K.op('dve', lambda: nc.vector.tensor_tensor(tq[1][:, 0:Tc], Ei, wi_, ALU.mult), reads=[ek, f'sW{b}'], writes=[tqk[1]])
                K.op('dve', lambda: nc.vector.tensor_tensor(S[b][:, 0, 0:Tc], tq[0][:, 0:Tc], tq[1][:, 0:Tc], ALU.subtract), reads=[tqk[0], tqk[1]], writes=[f'sS{b}'])
                K.op('pool', lambda: nc.gpsimd.tensor_tensor(tq[2][:, 0:Tc], Er, wi_, ALU.mult), reads=[ek, f'sW{b}'], writes=[tqk[2]])
                K.op('pool', lambda: nc.gpsimd.tensor_tensor(tq[3][:, 0:Tc], Ei, wr_, ALU.mult), reads=[ek, f'sW{b}'], writes=[tqk[3]])
                K.op('pool', lambda: nc.gpsimd.tensor_tensor(S[b][:, 1, 0:Tc], tq[2][:, 0:Tc], tq[3][:, 0:Tc], ALU.add), reads=[tqk[2], tqk[3]], writes=[f'sS{b}'])
                K.op('act', lambda: nc.scalar.copy(carry[:, st, :], S[b][:, :, Tc - 1]), reads=[f'sS{b}'], writes=[('scarry', st)])
                if not need_out:
                    continue
                first = (st % 4 == 0)
                last = (st % 4 == 3)
                for ri in range(2):
                    K.op('pe', lambda ri=ri: nc.tensor.matmul(py[:, kc, 0:Tc], Cm[:, c, ri, :], S[b][:, ri, 0:Tc], start=(first and ri == 0), stop=(last and ri == 1)),
                         reads=['sCm', f'sS{b}'], writes=['spy'])
            if not need_out:
                continue
            if d == 0:
                col0 = tl[0] * 128
                for ft in range(2):
                    K.op('dve', lambda ft=ft: nc.vector.scalar_tensor_tensor(yTf[:, ft, 0:Tc], uT[ui][:, ft, 0:Tc], dT[:, ft:ft + 1], py[:, ft, 0:Tc], ALU.mult, ALU.add),
                         reads=[f'suT{ui}', 'sdT', 'spy'], writes=['syTf'])
                K.dma('pool', D['ysT'][:, :, col0:col0 + Tc].rearrange("f p t -> p f t"), yTf[:, :, 0:Tc], reads=['syTf'], writes=[('ysT', col0)])
            else:
                nt_ = len(tl)
                K.op('act', lambda: nc.scalar.copy(yTf[:, :, 0:Tc], py[:, :, 0:Tc]), reads=['spy'], writes=['syTf'])
                pyt = puT[:].rearrange("p a t -> p (a t)").rearrange("p (s f) -> p s f", f=256)
                for j in range(nt_):
                    for ft in range(2):
                        K.op('pe', lambda j=j, ft=ft: nc.tensor.transpose(pyt[:, j, ft * 128:(ft + 1) * 128], yTf[:, ft, j * 128:(j + 1) * 128], ident[:]),
                             reads=['syTf', 'ident'], writes=['spuT'])
                K.op('act', lambda: nc.scalar.copy(ytok[:, 0:nt_, :], pyt[:, 0:nt_, :]), reads=['spuT'], writes=['sytok'])
                col0 = tl[-1] * 128
                K.dma('sp', yb[:, :, 0:Tc], D['ysT'][:, :, col0:col0 + Tc].rearrange("f p t -> p f t"), writes=['syb'])
                for j in range(nt_):
                    nj = nt_ - 1 - j
                    for ft in range(2):
                        K.op('pe', lambda j=j, nj=nj, ft=ft: nc.tensor.matmul(py[:, ft, nj * 128:(nj + 1) * 128], ytok[:, j, ft * 128:(ft + 1) * 128], jm[:], start=True, stop=True),
                             reads=['sytok', 'jm'], writes=['spy'])
                K.op('dve', lambda: nc.vector.tensor_tensor(yb[:, :, 0:Tc], yb[:, :, 0:Tc], py[:, :, 0:Tc], ALU.add), reads=['syb', 'spy'], writes=['syb'])
                if 'ydbg' in DEBUG_OUT:
                    K.dma('sp', D['ydbg'][:, :, col0:col0 + Tc].rearrange("f p t -> p f t"), yb[:, :, 0:Tc], reads=['syb'], writes=[('ydbg', col0)])
                gelu_tanh(K, nc, None, yb[:, :, 0:Tc], yb[:, :, 0:Tc], 'syb', 'syb', g1[:, :, 0:Tc], 'sg1')
                for j in range(nt_):
                    for ft in range(2):
                        K.op('pe', lambda j=j, ft=ft: nc.tensor.matmul(pz[:], yb[:, ft, j * 128:(j + 1) * 128], gw[:, ft, :], start=(ft == 0), stop=(ft == 1)),
                             reads=['syb', 'sgw'], writes=['spz'])
                    K.op('act', lambda: nc.scalar.activation(zs[:, 256:512], pz[:, 256:512], AF.Sigmoid), reads=['spz'], writes=['szs'])
                    K.op('dve', lambda: nc.vector.tensor_tensor(zo[:], pz[:, 0:256], zs[:, 256:512], ALU.mult), reads=['spz', 'szs'], writes=['szo'])
                    r0 = col0 + j * 128
                    K.dma('pool', D['ymix'][r0:r0 + 128, 0:256], zo[:], reads=['szo'], writes=[('ymix', 's', r0)])
        K.barrier()
    K.pop()


def stage_merge(K, nc, D, cfg, l, C, with_ctx):
    K.push()
    ident = C['ident']
    g1 = {'l': load_bc(K, D, 'l', 2)}
    if with_ctx:
        g1['c'] = load_bc(K, D, 'c', 2)
    wbr = K.sb('wbr', [128, 8, 1024], MM)
    wout = K.sb('wout', [128, 8, 1024], MM)
    K.push()
    mst = [K.sb(f'mst{i}', [128, 2, 1024]) for i in range(2)]
    srcs = [(wbr, 0, D['w_br_ssm'][l]), (wbr, 2, D['w_br_diff'][l][0:256]), (wbr, 4, D['w_br_diff'][l][256:512]), (wbr, 6, D['w_br_na'][l])] + \
           [(wout, 2 * j, D['w_out'][l][256 * j:256 * (j + 1)]) for j in range(4)]
    for si, (dst, k0, src) in enumerate(srcs):
        b_ = si % 2
        K.dma('sp', mst[b_][:], src.rearrange("(k p) n -> p k n", p=128), writes=[f'mst{b_}'])
        K.op('pool', lambda: nc.gpsimd.tensor_copy(dst[:, k0:k0 + 2, :], mst[b_][:]), reads=[f'mst{b_}'], writes=['wbr', 'wout'])
    K.pop()
    yt = [K.sb(f'myt{i}', [128, 1024]) for i in range(2)]
    gt = [K.sb(f'mgt{i}', [128, 3072]) for i in range(2)]
    xt = [K.sb(f'mxt{i}', [128, 1024]) for i in range(2)]
    yT = K.sb('myT', [128, 8, 128], MM)
    mm = K.sb('mmm', [128, 1024])
    mT = K.sb('mmT', [128, 8, 128], MM)
    tmp = K.sb('mtmp', [128, 512])
    xo = [K.sb(f'mxo{i}', [128, 1024]) for i in range(2)]
    ptr = K.ps('mptr', [128, 1024])
    pb = [K.ps(f'mpb{i}', [128, 512]) for i in range(2)]
    npb = 0
    tiles = list(range(cfg.TT)) if with_ctx else list(range(CT, cfg.TT))
    branches = [(0, [0, 1]), (1, [2, 3, 4, 5]), (2, [6, 7])]
    for tt in tiles:
        i = tt % 2
        w = 'c' if tt < CT else 'l'
        rows = slice(tt * 128, (tt + 1) * 128)
        K.dma('sp', yt[i][:], D['ymix'][rows, :], writes=[f'myt{i}'])
        K.dma('sp', gt[i][:], D['pl'][rows, 2560:5632], writes=[f'mgt{i}'])
        K.dma('sp', xt[i][:], xsrc(D, cfg, l, tt), writes=[f'mxt{i}'])
        K.op('act', lambda: nc.scalar.activation(gt[i][:], gt[i][:], AF.Sigmoid), reads=[f'mgt{i}'], writes=[f'mgt{i}'])
        for k in range(8):
            K.op('pe', lambda k=k: nc.tensor.transpose(ptr[:, k * 128:(k + 1) * 128], yt[i][:, k * 128:(k + 1) * 128], ident[:]),
                 reads=[f'myt{i}', 'ident'], writes=['mptr'])
        K.op('act', lambda: nc.scalar.copy(yT[:], ptr[:].rearrange("p (k t) -> p k t", k=8)), reads=['mptr'], writes=['myT'])
        for (br, ks) in branches:
            for hh in range(2):
                p = pb[npb % 2]
                pk = f'mpb{npb % 2}'
                npb += 1
                for ki, k in enumerate(ks):
                    K.op('pe', lambda k=k, ki=ki: nc.tensor.matmul(p[:], yT[:, k, :], wbr[:, k, hh * 512:(hh + 1) * 512], start=(ki == 0), stop=(ki == len(ks) - 1)),
                         reads=['myT', 'wbr'], writes=[pk])
                gsl = gt[i][:, br * 1024 + hh * 512: br * 1024 + (hh + 1) * 512]
                if br == 0:
                    K.op('dve', lambda: nc.vector.tensor_tensor(mm[:, hh * 512:(hh + 1) * 512], p[:], gsl, ALU.mult), reads=[pk, f'mgt{i}'], writes=[('mmm', hh)])
                else:
                    K.op('dve', lambda: nc.vector.tensor_tensor(tmp[:], p[:], gsl, ALU.mult), reads=[pk, f'mgt{i}'], writes=['mtmp'])
                    K.op('pool', lambda: nc.gpsimd.tensor_tensor(mm[:, hh * 512:(hh + 1) * 512], mm[:, hh * 512:(hh + 1) * 512], tmp[:], ALU.add),
                         reads=['mtmp', ('mmm', hh)], writes=[('mmm', hh)])
        for k in range(8):
            K.op('pe', lambda k=k: nc.tensor.transpose(ptr[:, k * 128:(k + 1) * 128], mm[:, k * 128:(k + 1) * 128], ident[:]),
                 reads=[('mmm', k // 4), 'ident'], writes=['mptr'])
        K.op('act', lambda: nc.scalar.copy(mT[:], ptr[:].rearrange("p (k t) -> p k t", k=8)), reads=['mptr'], writes=['mmT'])
        for hh in range(2):
            p = pb[npb % 2]
            pk = f'mpb{npb % 2}'
            npb += 1
            for k in range(8):
                K.op('pe', lambda k=k: nc.tensor.matmul(p[:], mT[:, k, :], wout[:, k, hh * 512:(hh + 1) * 512], start=(k == 0), stop=(k == 7)),
                     reads=['mmT', 'wout'], writes=[pk])
            K.op('dve', lambda: nc.vector.tensor_tensor(tmp[:], p[:], g1[w][:, hh * 512:(hh + 1) * 512], ALU.mult), reads=[pk, f'bc{w}2'], writes=['mtmp'])
            K.op('pool', lambda: nc.gpsimd.tensor_tensor(xo[i][:, hh * 512:(hh + 1) * 512], xt[i][:, hh * 512:(hh + 1) * 512], tmp[:], ALU.add),
                 reads=['mtmp', f'mxt{i}'], writes=[f'mxo{i}'])
        K.dma('pool', D['xres'][rows, :], xo[i][:], reads=[f'mxo{i}'], writes=[('xres', tt)])
    K.pop()


def stage_peer(K, nc, D, cfg, l, C, with_ctx, final):
    K.push()
    ident = C['ident']
    bc = {}
    for w in (['l', 'c'] if with_ctx else ['l']):
        for j in (3, 4, 5):
            bc[(w, j)] = load_bc(K, D, w, j)
    if final:
        fgr = K.sb('fgr', [1, 1024])
        K.dma('sp', fgr[:], D['final_g'], writes=['fgr'])
        fg = K.sb('fg', [128, 1024])
        K.push()
        with_ps = K.ps('fgps', [128, 512])
        for hh in range(2):
            bcast_row(K, nc, C, fgr[:, hh * 512:(hh + 1) * 512], 'fgr', fg[:, hh * 512:(hh + 1) * 512], 'fg', 512, with_ps, 'fgps')
        K.pop()
    wq = K.sb('wq', [128, 8, 2048])
    K.dma('sp', wq[:, :, 0:1024], D['peer_wq'][l, :, 0:1024].rearrange("(k p) n -> p k n", p=128), writes=['wq'])
    K.dma('sp', wq[:, :, 1024:2048], D['peer_wq'][l, :, 1024:2048].rearrange("(k p) n -> p k n", p=128), writes=['wq'])
    skT = K.sb('skT', [128, 16, 128])
    identR = K.sb('identR', [128, 128], MM)
    K.push()
    skr = K.sb('skr', [128, 16, 128])
    K.dma('sp', skr[:], D['peer_sk'][l].rearrange("c n d -> n c d"), writes=['skr'])
    pst = K.ps('pst', [128, 4, 128])
    K.op('dve', lambda: nc.vector.tensor_copy(identR[:], ident[:]), reads=['ident'], writes=['identR'])
    for c4 in range(4):
        for j in range(4):
            K.op('pe', lambda j=j: nc.tensor.transpose(pst[:, j, :], skr[:, c4 * 4 + j, :], ident[:]), reads=['skr', 'ident'], writes=['pst'])
        K.op('act', lambda: nc.scalar.copy(skT[:, c4 * 4:(c4 + 1) * 4, :], pst[:]), reads=['pst'], writes=['skT'])
    K.pop()
    iota16 = K.sb('iota16', [128, 16])
    K.dma('sp', iota16[:], D['iota16'], writes=['iota16'])
    xt = [K.sb(f'pxt{i}', [128, 1024]) for i in range(1)]
    h2 = K.sb('ph2', [128, 1024])
    tmp = K.sb('junkP', [128, 1024])
    scr = (K.sb('pss', [128, 1]), K.sb('prs', [128, 1]), tmp)
    ptr = K.ps('pptr', [128, 1024])
    h2T = K.sb('ph2T', [128, 8, 128])
    pq = [K.ps(f'ppq{i}', [128, 4, 128]) for i in range(2)]
    qT = K.sb('pqT', [128, 16, 128])
    psc = [K.ps(f'ppsc{i}', [128, 4, 128]) for i in range(1)]
    pacc = [K.ps(f'ppacc{i}', [128, 512]) for i in range(2)]
    vsc = [K.sb(f'pvsc{i}', [128, 1024], MM) for i in range(2)]
    s = K.sb('ps_s', [128, 16, 128])
    s2 = K.sb('ps_s2', [128, 16, 128])
    sv = K.sb('ps_sv', [128, 16, 16])
    si = K.sb('ps_si', [128, 16, 16], U32)
    sif = K.sb('ps_sif', [128, 16, 16])
    cand = s[:].rearrange("p a n -> p (a n)").rearrange("p (h c) -> p h c", c=256)
    cand2 = s2[:].rearrange("p a n -> p (a n)").rearrange("p (h c) -> p h c", c=256)
    best = K.sb('ps_best', [128, 8, 16])
    pos = K.sb('ps_pos', [128, 8, 16], U32)
    pa = K.sb('ps_pa', [128, 8, 16], U32)
    pbb = K.sb('ps_pb', [128, 8, 16], U32)
    paf = K.sb('ps_paf', [128, 8, 16])
    pbf = K.sb('ps_pbf', [128, 8, 16])
    oh = K.sb('ps_oh', [128, 8, 16, 16])
    ii = K.sb('ps_ii', [128, 8, 16])
    jj = K.sb('ps_jj', [128, 8, 16])
    eidx = K.sb('ps_eidx', [128, 128], I32)
    gg = K.sb('ps_g', [128, 8, 16])
    gs = K.sb('ps_gs', [128, 8])
    act = K.sb('ps_act', [128, 128])
    wgt = K.sb('ps_wgt', [128, 128])
    gtmp = K.sb('ps_gtmp', [128, 128])
    NG = 10
    gb = [K.sb(f'pgb{i}', [128, 2048], BF16) for i in range(NG)]
    xo = K.sb('pxo', [128, 1024])
    ng = 0
    nq_ = 0
    tiles = list(range(cfg.TT)) if with_ctx else list(range(CT, cfg.TT))
    split = final and PAIR_SPLIT
    if split:
        tiles = list(range(CT, CT + cfg.NT // 2))
        ridx = K.sb('ridx', [128, cfg.NT // 2], I32)
        K.dma('sp', ridx[:], D['rowidx'], writes=['ridx'])
    T1 = ['pk_all']
    for tt in tiles:
        i = 0
        w = 'c' if tt < CT else 'l'
        rows = slice(tt * 128, (tt + 1) * 128)
        if split:
            j_ = tt - CT
            K.dma('pool', None, None, reads=['ridx'], writes=[f'pxt{i}'], fn=lambda: nc.gpsimd.indirect_dma_start(
                out=xt[i][:], out_offset=None, in_=D['xres'], in_offset=bass.IndirectOffsetOnAxis(ap=ridx[:, j_:j_ + 1], axis=0)))
        else:
            K.dma('sp', xt[i][:], D['xres'][rows, :], writes=[f'pxt{i}'])
        norm_mod(K, nc, xt[i], f'pxt{i}', h2, 'ph2', bc[(w, 3)], f'bc{w}3', bc[(w, 4)], f'bc{w}4', scr, 'P')
        for k in range(8):
            K.op('pe', lambda k=k: nc.tensor.transpose(ptr[:, k * 128:(k + 1) * 128], h2[:, k * 128:(k + 1) * 128], ident[:]),
                 reads=['ph2', 'ident'], writes=['pptr'])
        K.op('act', lambda: nc.scalar.copy(h2T[:], ptr[:].rearrange("p (k t) -> p k t", k=8)), reads=['pptr'], writes=['ph2T'])
        for c4 in range(4):
            p = pq[nq_ % 2]
            pk = f'ppq{nq_ % 2}'
            nq_ += 1
            for j in range(4):
                hx = c4 * 4 + j
                for k in range(8):
                    K.op('pe', lambda j=j, k=k, hx=hx: nc.tensor.matmul(p[:, j, :], wq[:, k, hx * 128:(hx + 1) * 128], h2T[:, k, :], start=(k == 0), stop=(k == 7)),
                         reads=['wq', 'ph2T'], writes=[pk])
            K.op('act', lambda: nc.scalar.copy(qT[:, c4 * 4:(c4 + 1) * 4, :], p[:]), reads=[pk], writes=[('pqT', c4)])
        for c4 in range(4):
            p = psc[0]
            pk = 'ppsc0'
            for j in range(4):
                hx = c4 * 4 + j
                K.op('pe', lambda j=j, hx=hx: nc.tensor.matmul(p[:, j, :], qT[:, hx, :], skT[:, hx, :], start=True, stop=True),
                     reads=[('pqT', c4), 'skT'], writes=[pk])
            K.op('act', lambda: nc.scalar.copy(s[:, c4 * 4:(c4 + 1) * 4, :], p[:]), reads=[pk], writes=[('ps_s', c4)])
        for hx in range(16):
            sk_ = ('ps_s', hx // 4)
            K.op('dve', lambda: nc.vector.max(out=sv[:, hx, 0:8], in_=s[:, hx, :]), reads=[sk_], writes=T1)
            K.op('dve', lambda: nc.vector.max_index(out=si[:, hx, 0:8], in_max=sv[:, hx, 0:8], in_values=s[:, hx, :]), reads=[sk_] + T1, writes=T1)
            K.op('dve', lambda: nc.vector.match_replace(out=s2[:, hx, :], in_to_replace=sv[:, hx, 0:8], in_values=s[:, hx, :], imm_value=-1e30), reads=[sk_] + T1, writes=T1)
            K.op('dve', lambda: nc.vector.max(out=sv[:, hx, 8:16], in_=s2[:, hx, :]), reads=T1, writes=T1)
            K.op('dve', lambda: nc.vector.max_index(out=si[:, hx, 8:16], in_max=sv[:, hx, 8:16], in_values=s2[:, hx, :]), reads=T1, writes=T1)

        T2 = T1 + [('ps_s', c) for c in range(4)]

        def dv(fn):
            K.op('dve', fn, reads=T2, writes=T2)
        svv = sv[:].rearrange("p (h x) a -> p h x a", x=2)
        cv = cand.rearrange("p h (a b) -> p h a b", b=16)
        dv(lambda: nc.vector.tensor_tensor(cv, svv[:, :, 0, :].unsqueeze(3).to_broadcast([128, 8, 16, 16]),
                                           svv[:, :, 1, :].unsqueeze(2).to_broadcast([128, 8, 16, 16]), ALU.add))
        for h in range(8):
            dv(lambda: nc.vector.max(out=best[:, h, 0:8], in_=cand[:, h, :]))
            dv(lambda: nc.vector.max_index(out=pos[:, h, 0:8], in_max=best[:, h, 0:8], in_values=cand[:, h, :]))
            dv(lambda: nc.vector.match_replace(out=cand2[:, h, :], in_to_replace=best[:, h, 0:8], in_values=cand[:, h, :], imm_value=-1e30))
            dv(lambda: nc.vector.max(out=best[:, h, 8:16], in_=cand2[:, h, :]))
            dv(lambda: nc.vector.max_index(out=pos[:, h, 8:16], in_max=best[:, h, 8:16], in_values=cand2[:, h, :]))
        dv(lambda: nc.vector.tensor_tensor(gg[:], best[:], best[:, :, 0:1].to_broadcast([128, 8, 16]), ALU.subtract))
        K.op('act', lambda: nc.scalar.activation(gg[:], gg[:], AF.Exp), reads=T1, writes=T1)
        dv(lambda: nc.vector.reduce_sum(gs[:], gg[:], axis=AX.X))
        dv(lambda: nc.vector.reciprocal(gs[:], gs[:]))
        dv(lambda: nc.vector.tensor_tensor(gg[:], gg[:], gs[:].unsqueeze(2).to_broadcast([128, 8, 16]), ALU.mult))
        dv(lambda: nc.vector.tensor_scalar(pa[:], pos[:], 4, None, ALU.logical_shift_right))
        dv(lambda: nc.vector.tensor_scalar(pbb[:], pos[:], 15, None, ALU.bitwise_and))
        dv(lambda: nc.vector.tensor_copy(paf[:], pa[:]))
        dv(lambda: nc.vector.tensor_copy(pbf[:], pbb[:]))
        dv(lambda: nc.vector.tensor_copy(sif[:], si[:]))
        sfv = sif[:].rearrange("p (h x) a -> p h x a", x=2)
        io_b = iota16[:].unsqueeze(1).unsqueeze(1).to_broadcast([128, 8, 16, 16])
        for (pf, xsel, dst) in ((paf, 0, ii), (pbf, 1, jj)):
            dv(lambda: nc.vector.tensor_tensor(oh[:], pf[:].unsqueeze(3).to_broadcast([128, 8, 16, 16]), io_b, ALU.is_equal))
            dv(lambda: nc.vector.tensor_tensor(oh[:], oh[:], sfv[:, :, xsel, :].unsqueeze(2).to_broadcast([128, 8, 16, 16]), ALU.mult))
            dv(lambda: nc.vector.reduce_sum(dst[:], oh[:], axis=AX.X))
        dv(lambda: nc.vector.scalar_tensor_tensor(ii[:], ii[:], 128.0, jj[:], ALU.mult, ALU.add))
        dv(lambda: nc.vector.tensor_copy(eidx[:], ii[:].rearrange("p h k -> p (h k)")))
        GRP = 4
        ggf = gg[:].rearrange("p h k -> p (h k)")
        for g0 in range(0, 128, GRP):
            held = []
            for slot in range(g0, g0 + GRP):
                b_ = gb[ng % NG]
                bk = f'pgb{ng % NG}'
                ng += 1
                held.append((b_, bk))
                K.dma('pool', None, None, reads=T1, writes=[bk], fn=lambda: nc.gpsimd.indirect_dma_start(
                    out=b_[:], out_offset=None, in_=D[f'uv{l}'], in_offset=bass.IndirectOffsetOnAxis(ap=eidx[:, slot:slot + 1], axis=0)))
                K.op('dve', lambda: nc.vector.scalar_tensor_tensor(tmp[:], b_[:, 0:1024], 1.0, h2[:], ALU.mult, ALU.mult, accum_out=act[:, slot:slot + 1]),
                     reads=[bk, 'ph2'], writes=['junkP', 'ps_act'])
            sl = slice(g0, g0 + GRP)
            gelu_tanh(K, nc, None, wgt[:, sl], act[:, sl], 'ps_act', 'ps_wgt', gtmp[:, sl], 'ps_gtmp')
            K.op('dve', lambda: nc.vector.tensor_tensor(wgt[:, sl], wgt[:, sl], ggf[:, sl], ALU.mult), reads=['ps_wgt'] + T1, writes=['ps_wgt'])
            for si_, slot in enumerate(range(g0, g0 + GRP)):
                b_, bk = held[si_]
                vi = slot % 2
                K.op('act', lambda: nc.scalar.activation(vsc[vi][:], b_[:, 1024:2048], AF.Copy, scale=wgt[:, slot:slot + 1]), reads=[bk, 'ps_wgt'], writes=[f'pvsc{vi}'])
                for hh in range(2):
                    K.op('pe', lambda hh=hh: nc.tensor.matmul(pacc[hh][:], identR[:], vsc[vi][:, hh * 512:(hh + 1) * 512], start=(slot == 0), stop=(slot == 127)),
                         reads=['identR', f'pvsc{vi}'], writes=[f'ppacc{hh}'])
        for hh in range(2):
            K.op('dve', lambda hh=hh: nc.vector.tensor_tensor(tmp[:, hh * 512:(hh + 1) * 512], pacc[hh][:], bc[(w, 5)][:, hh * 512:(hh + 1) * 512], ALU.mult),
                 reads=[f'ppacc{hh}', f'bc{w}5'], writes=['junkP'])
        K.op('dve', lambda: nc.vector.tensor_tensor(xo[:], xt[i][:], tmp[:], ALU.add), reads=['junkP', f'pxt{i}'], writes=['pxo'])
        if not final:
            K.dma('sp', D['xres'][rows, :], xo[:], reads=['pxo'], writes=[('xres', tt)])
        else:
            ss, rs, junk = scr
            K.op('act', lambda: nc.scalar.activation(junk[:], xo[:], AF.Square, accum_out=ss[:]), reads=['pxo'], writes=['junkP', 'ssP'])
            K.op('dve', lambda: nc.vector.tensor_scalar(rs[:], ss[:], 1.0 / 1024.0, 1e-6, ALU.mult, ALU.add), reads=['ssP'], writes=['rsP'])
            K.op('act', lambda: nc.scalar.activation(rs[:], rs[:], AF.Sqrt), reads=['rsP'], writes=['rsP'])
            K.op('dve', lambda: nc.vector.reciprocal(rs[:], rs[:]), reads=['rsP'], writes=['rsP'])
            K.op('dve', lambda: nc.vector.scalar_tensor_tensor(tmp[:], xo[:], rs[:], fg[:], ALU.mult, ALU.mult), reads=['pxo', 'rsP', 'fg'], writes=['junkP'])
            K.dma('sp', D['out'][(tt - CT) * 128:(tt - CT + 1) * 128, :], tmp[:], reads=['junkP'], writes=[('out', tt)])
        K.maybe_barrier()
    K.pop()


def stage_tables(K, nc, D, cfg):
    K.push()
    src = [K.sb(f'tbs{i}', [128, 4, 1024]) for i in range(3)]
    dst = [K.sb(f'tbd{i}', [128, 4, 1024], BF16) for i in range(3)]
    n = 0
    for l in range(cfg.depth):
        for nm_s, nm_d, c0 in ((f'peer_u{l}', f'uv{l}', 0), (f'peer_v{l}', f'uv{l}', 1024)):
            for r0 in range(0, 16384, 512):
                i = n % 3
                n += 1
                K.dma('sp', src[i][:], D[nm_s][r0:r0 + 512, :].rearrange("(p j) c -> p j c", j=4), writes=[f'tbs{i}'])
                if i == 0:
                    K.op('dve', lambda: nc.vector.tensor_copy(dst[i][:], src[i][:]), reads=[f'tbs{i}'], writes=[f'tbd{i}'])
                elif i == 1:
                    K.op('pool', lambda: nc.gpsimd.tensor_copy(dst[i][:], src[i][:]), reads=[f'tbs{i}'], writes=[f'tbd{i}'])
                else:
                    K.op('act', lambda: nc.scalar.copy(dst[i][:], src[i][:]), reads=[f'tbs{i}'], writes=[f'tbd{i}'])
                K.dma('act', D[nm_d][r0:r0 + 512, c0:c0 + 1024].rearrange("(p j) c -> p j c", j=4), dst[i][:], reads=[f'tbd{i}'], writes=[(nm_d, r0, c0)])
    K.pop()


ALL_STAGES = ('inproj', 'prepass', 'diff', 'na', 'ssm', 'merge', 'peer')
_NC_CACHE = {}


def kernel(**inputs):
    inp = {k: np.asarray(v) for k, v in inputs.items()}
    B, L, _ = inp['x'].shape
    depth = inp['ada_w'].shape[0]
    key = (L, depth)
    if key not in _NC_CACHE:
        _NC_CACHE[key] = build(L, depth, stages=ALL_STAGES)
    nc = _NC_CACHE[key]
    n_cores = 8
    if PAIR_SPLIT:
        in_maps = []
        for b in range(B):
            m0 = core_inputs(inp, b, L, depth, 0)
            m1 = dict(m0)
            m1['rowidx'] = core_inputs_rowidx(L, 1)
            in_maps += [m0, m1]
        res = run_bass_kernel_spmd(nc, in_maps, core_ids=list(range(n_cores)))
        out = np.stack([np.concatenate([res.results[2 * b]['out'], res.results[2 * b + 1]['out']], 0) for b in range(B)], 0)
    else:
        maps = [core_inputs(inp, b, L, depth) for b in range(B)]
        in_maps = [maps[i % B] for i in range(n_cores)]
        res = run_bass_kernel_spmd(nc, in_maps, core_ids=list(range(n_cores)))
        out = np.stack([res.results[b]['out'] for b in range(B)], 0)
    return out.astype(np.float32)


def core_inputs_rowidx(L, half):
    nt2 = (L // 128) // 2
    return np.ascontiguousarray((CT * 128 + half * (L // 2) + np.arange(nt2)[None, :] * 128 + np.arange(128)[:, None]).astype(np.int32))
```

```python
import numpy as np
from contextlib import ExitStack
import concourse.bass as bass
import concourse.mybir as mybir
from concourse.bass_utils import run_bass_kernel_spmd

F32 = mybir.dt.float32
F32R = mybir.dt.float32r
BF16 = mybir.dt.bfloat16
QK_BF16 = True
AV_BF16 = True
BF_TABLES = True
TABLES_OVERLAP = True
USE_R = True
MM = F32R if USE_R else F32
U32 = mybir.dt.uint32
I32 = mybir.dt.int32
ALU = mybir.AluOpType
AF = mybir.ActivationFunctionType
AX = mybir.AxisListType

NSLOT = 12
RELAX_SAME = set()
PAIR_SPLIT = True
BAR_LIM = 12000
DEBUG_OUT = set()


class Sched:
    def __init__(self, nc, es):
        self.nc = nc
        self.es = es
        self.eng = {'pe': nc.tensor, 'dve': nc.vector, 'act': nc.scalar, 'pool': nc.gpsimd, 'sp': nc.sync}
        self.sem = {e: es.enter_context(nc.semaphore(f"s_{e}")) for e in self.eng}
        self.cnt = {e: 0 for e in self.eng}
        self.waited = {e: {} for e in self.eng}
        self.dq = {}
        for q in ('sp', 'pool', 'act'):
            self.dq[q] = dict(sems=[es.enter_context(nc.semaphore(f"d_{q}{i}")) for i in range(NSLOT)],
                              uses=[0] * NSLOT, nxt=0)
        self.lastw = {}
        self.readers = {}
        self.scopes = []
        self.nalloc = 0
        self.sem_arrive = es.enter_context(nc.semaphore("s_arrive"))
        self.sem_epoch = es.enter_context(nc.semaphore("s_epoch"))
        self.epoch = 0

    def push(self):
        s = ExitStack()
        self.scopes.append(s)

    def pop(self):
        self.barrier()
        self.scopes.pop().close()

    def _ctx(self):
        return self.scopes[-1] if self.scopes else self.es

    def sb(self, name, shape, dtype=F32):
        self.nalloc += 1
        return self._ctx().enter_context(self.nc.sbuf_tensor(f"{name}_{self.nalloc}", list(shape), dtype))

    def ps(self, name, shape, dtype=F32):
        self.nalloc += 1
        return self._ctx().enter_context(self.nc.psum_tensor(f"{name}_{self.nalloc}", list(shape), dtype))

    def _semof(self, key):
        if isinstance(key, str):
            return self.sem[key]
        return self.dq[key[1]]['sems'][key[2]]

    def _wait(self, e, tok):
        key, val = tok
        if key == e and (e in ('pe', 'sp') or e in RELAX_SAME):
            return
        if self.waited[e].get(key, 0) >= val:
            return
        self.eng[e].wait_ge(self._semof(key), val)
        self.waited[e][key] = val

    def _deps(self, e, reads, writes):
        for r in reads:
            if r in self.lastw:
                self._wait(e, self.lastw[r])
        for w in writes:
            if w in self.lastw:
                self._wait(e, self.lastw[w])
            for k, v in self.readers.get(w, {}).items():
                self._wait(e, (k, v))

    def _commit(self, tok, reads, writes):
        for r in reads:
            d = self.readers.setdefault(r, {})
            if d.get(tok[0], 0) < tok[1]:
                d[tok[0]] = tok[1]
        for w in writes:
            self.lastw[w] = tok
            self.readers[w] = {}

    def op(self, e, fn, reads=(), writes=()):
        self._deps(e, reads, writes)
        ins = fn()
        ins.then_inc(self.sem[e], 1)
        self.cnt[e] += 1
        self._commit((e, self.cnt[e]), reads, writes)

    def dma(self, q, out, in_, reads=(), writes=(), fn=None):
        d = self.dq[q]
        self._deps(q, reads, writes)
        s = d['nxt']
        d['nxt'] = (s + 1) % NSLOT
        if d['uses'][s] > 0:
            self._wait(q, (('dma', q, s), 16 * d['uses'][s]))
        if fn is None:
            ins = self.eng[q].dma_start(out=out, in_=in_)
        else:
            ins = fn()
        d['uses'][s] += 1
        ins.then_inc(d['sems'][s], 16)
        self._commit((('dma', q, s), 16 * d['uses'][s]), reads, writes)

    def _all_tokens(self):
        toks = [(e, c) for e, c in self.cnt.items() if c > 0]
        for q, d in self.dq.items():
            for s in range(NSLOT):
                if d['uses'][s] > 0:
                    toks.append((('dma', q, s), 16 * d['uses'][s]))
        return toks

    def barrier(self, reset=True):
        toks = self._all_tokens()
        if not toks:
            return
        for e in self.eng:
            for t in toks:
                if t[0] != e:
                    self._wait(e, t)
        if not reset:
            return
        for e in self.eng:
            if self.cnt[e] > BAR_LIM:
                self.epoch += 1
                self.sem[e] = self.es.enter_context(self.nc.semaphore(f"s_{e}_{self.epoch}"))
                self.cnt[e] = 0
                for e2 in self.eng:
                    self.waited[e2].pop(e, None)
                for k in list(self.lastw.keys()):
                    if self.lastw[k][0] == e:
                        del self.lastw[k]
                for k, d in self.readers.items():
                    d.pop(e, None)

    def maybe_barrier(self, lim=None):
        if max(self.cnt.values()) > (BAR_LIM if lim is None else lim):
            self.barrier()

    def finish(self):
        self.barrier(reset=False)


D_MODEL = 1024
IN_W = 5632
CTXL = 256
CT = 2
LAM_INIT = [0.8 - 0.6 * float(np.exp(-0.3 * l)) for l in range(8)]


class Cfg:
    def __init__(self, L, depth):
        self.L = L
        self.depth = depth
        self.NT = L // 128
        self.TT = self.NT + CT
        self.R = L // 64


def declare_io(nc, cfg):
    D = {}
    dp = cfg.depth

    def din(name, shape, dt=F32):
        D[name] = nc.dram_tensor(name, list(shape), dt, kind="ExternalInput").ap()

    def dsc(name, shape, dt=F32):
        kind = "ExternalOutput" if name in DEBUG_OUT else "Internal"
        D[name] = nc.dram_tensor(name, list(shape), dt, kind=kind).ap()

    din('x', [cfg.L, 1024]); din('ctx', [CTXL, 1024]); din('cc', [2, 1024])
    din('ada_w', [dp, 1024, 6144]); din('ada_b', [dp, 6144])
    din('norm_g', [dp, 2048])
    din('w_in', [dp, 1024, IN_W])
    din('ident', [128, 128]); din('sel', [2, 256])
    D['out'] = nc.dram_tensor('out', [cfg.L // 2 if PAIR_SPLIT else cfg.L, 1024], F32, kind="ExternalOutput").ap()
    if PAIR_SPLIT:
        din('rowidx', [128, cfg.NT // 2], I32)
    dsc('xres', [cfg.TT * 128, 1024])
    dsc('pl', [cfg.TT * 128, IN_W])
    dsc('qkT', [8, 128, cfg.TT * 128]); dsc('nqkT', [4, 128, cfg.TT * 128])
    dsc('ymix', [cfg.TT * 128, 1024])
    ntypes = len(na_pair_info(cfg.R)[1])
    din('rope', [cfg.L, 64]); din('nab', [dp, 4, ntypes * 5, 128, 128])
    din('diff_lambda', [dp, 256]); din('diff_subln_g', [dp, 128])
    din('jmat', [128, 128]); din('ssm_sc', [dp, 128, 48]); din('ssm_b', [dp, 2, 16, 128, 128]); din('ssm_c', [dp, 2, 16, 128, 128])
    din('ssm_dT', [dp, 128, 2]); din('ssm_glu_w', [dp, 256, 512])
    dsc('ysT', [2, 128, cfg.TT * 128])
    dsc('bcd', [2, 6, 128, 1024])
    if BF_TABLES:
        for i in range(dp):
            dsc(f'uv{i}', [16384, 2048], BF16)
    din('w_br_ssm', [dp, 256, 1024]); din('w_br_diff', [dp, 512, 1024]); din('w_br_na', [dp, 256, 1024]); din('w_out', [dp, 1024, 1024])
    din('peer_wq', [dp, 1024, 2048]); din('peer_sk', [dp, 16, 128, 128]); [din(f'peer_u{i}', [16384, 1024]) for i in range(dp)]; [din(f'peer_v{i}', [16384, 1024]) for i in range(dp)]
    din('final_g', [1, 1024]); din('iota16', [128, 16])
    if 'ydbg' in DEBUG_OUT:
        dsc('ydbg', [2, 128, cfg.TT * 128])
    return D


def xsrc(D, cfg, l, tt):
    if l == 0:
        if tt < CT:
            return D['ctx'][tt * 128:(tt + 1) * 128, :]
        return D['x'][(tt - CT) * 128:(tt - CT + 1) * 128, :]
    return D['xres'][tt * 128:(tt + 1) * 128, :]


def stage_consts(K, nc, D):
    ident = K.sb('ident', [128, 128])
    K.dma('sp', ident[:], D['ident'], writes=['ident'])
    sel = K.sb('sel', [2, 256])
    K.dma('sp', sel[:], D['sel'], writes=['sel'])
    return dict(ident=ident, sel=sel)


def stage_mods(K, nc, D, cfg, l, C, with_ctx):
    bc = {}
    who = ['l', 'c'] if with_ctx else ['l']
    K.push()
    for w in ['l', 'c']:
        for j in ([0, 1, 2, 3, 4, 5] if w in who else [0, 1]):
            bc[(w, j)] = K.sb(f'bc{w}{j}', [128, 1024])
    ident, sel = C['ident'], C['sel']
    cc = K.sb('cc', [2, 1024])
    K.dma('sp', cc[:], D['cc'], writes=['cc'])
    K.op('act', lambda: nc.scalar.activation(cc[:], cc[:], AF.Silu), reads=['cc'], writes=['cc'])
    pT = K.ps('pT', [128, 8, 2])
    for k in range(8):
        K.op('pe', lambda k=k: nc.tensor.transpose(pT[:, k, :], cc[:, k * 128:(k + 1) * 128], ident[0:2, 0:2]),
             reads=['cc', 'ident'], writes=['pT'])
    cT = K.sb('cT', [128, 8, 2])
    K.op('dve', lambda: nc.vector.tensor_copy(cT[:], pT[:]), reads=['pT'], writes=['cT'])
    mods = K.sb('mods', [2, 6144])
    ab = K.sb('ab', [2, 6144])
    K.dma('sp', ab[0:1, :], D['ada_b'][l:l + 1, :], writes=['ab'])
    K.dma('sp', ab[1:2, :], D['ada_b'][l:l + 1, :], writes=['ab'])
    gv = K.sb('gv', [1, 2048])
    K.dma('sp', gv[:], D['norm_g'][l:l + 1, :], writes=['gv'])
    wbuf = [K.sb(f'adaw{i}', [128, 8, 512]) for i in range(2)]
    pm = [K.ps(f'pm{i}', [2, 512]) for i in range(2)]
    for cch in range(12):
        wb = wbuf[cch % 2]
        wk = f'adaw{cch % 2}'
        K.dma('sp', wb[:], D['ada_w'][l, :, cch * 512:(cch + 1) * 512].rearrange("(k p) n -> p k n", p=128), writes=[wk])
        pk = f'pm{cch % 2}'
        for k in range(8):
            K.op('pe', lambda k=k, wb=wb, p=pm[cch % 2]: nc.tensor.matmul(p[:], cT[:, k, :], wb[:, k, :], start=(k == 0), stop=(k == 7)),
                 reads=['cT', wk], writes=[pk])
        K.op('dve', lambda p=pm[cch % 2], cch=cch: nc.vector.tensor_tensor(mods[:, cch * 512:(cch + 1) * 512], p[:], ab[:, cch * 512:(cch + 1) * 512], ALU.add),
             reads=[pk, 'ab'], writes=['mods'])
    for j in (1, 4):
        K.op('dve', lambda j=j: nc.vector.tensor_scalar(mods[:, j * 1024:(j + 1) * 1024], mods[:, j * 1024:(j + 1) * 1024], 1.0, None, ALU.add),
             reads=['mods'], writes=['mods'])
    pb = [K.ps(f'pb{i}', [128, 512]) for i in range(2)]
    gbc = K.sb('gbc', [128, 2048])
    n = 0
    for q in range(4):
        K.op('pe', lambda q=q, p=pb[n % 2]: nc.tensor.matmul(p[:], sel[0:1, 0:128], gv[:, q * 512:(q + 1) * 512], start=True, stop=True),
             reads=['sel', 'gv'], writes=[f'pb{n % 2}'])
        K.op('act', lambda q=q, p=pb[n % 2]: nc.scalar.copy(gbc[:, q * 512:(q + 1) * 512], p[:]), reads=[f'pb{n % 2}'], writes=['gbc'])
        n += 1
    for wi, w in enumerate(['l', 'c']):
        for mj in range(6):
            tj = {0: 1, 1: 0, 2: 2, 3: 4, 4: 3, 5: 5}[mj]
            if (w, tj) not in bc:
                continue
            for hh in range(2):
                K.op('pe', lambda p=pb[n % 2], wi=wi, mj=mj, hh=hh: nc.tensor.matmul(
                    p[:], sel[0:2, wi * 128:(wi + 1) * 128], mods[:, mj * 1024 + hh * 512: mj * 1024 + (hh + 1) * 512], start=True, stop=True),
                    reads=['sel', 'mods'], writes=[f'pb{n % 2}'])
                dst = bc[(w, tj)][:, hh * 512:(hh + 1) * 512]
                if tj in (0, 3):
                    goff = 0 if tj == 0 else 1024
                    K.op('dve', lambda p=pb[n % 2], dst=dst, goff=goff, hh=hh: nc.vector.tensor_tensor(
                        dst, p[:], gbc[:, goff + hh * 512: goff + (hh + 1) * 512], ALU.mult),
                        reads=[f'pb{n % 2}', 'gbc'], writes=[f'bc{w}{tj}'])
                else:
                    K.op('act', lambda p=pb[n % 2], dst=dst: nc.scalar.copy(dst, p[:]), reads=[f'pb{n % 2}'], writes=[f'bc{w}{tj}'])
                n += 1
    for (w, j), t in bc.items():
        K.dma('sp', D['bcd'][0 if w == 'l' else 1, j], t[:], reads=[f'bc{w}{j}'], writes=[('bcd', w, j)])
    K.pop()
    return None


def load_bc(K, D, w, j):
    t = K.sb(f'bc{w}{j}', [128, 1024])
    K.dma('sp', t[:], D['bcd'][0 if w == 'l' else 1, j], writes=[f'bc{w}{j}'])
    return t


def norm_mod(K, nc, xt, xkey, ht, hkey, gm, gmkey, sh, shkey, scr, tag):
    ss, rs, junk = scr
    K.op('act', lambda: nc.scalar.activation(junk[:], xt[:], AF.Square, accum_out=ss[:]), reads=[xkey], writes=[f'junk{tag}', f'ss{tag}'])
    K.op('dve', lambda: nc.vector.tensor_scalar(rs[:], ss[:], 1.0 / 1024.0, 1e-6, ALU.mult, ALU.add), reads=[f'ss{tag}'], writes=[f'rs{tag}'])
    K.op('act', lambda: nc.scalar.activation(rs[:], rs[:], AF.Sqrt), reads=[f'rs{tag}'], writes=[f'rs{tag}'])
    K.op('dve', lambda: nc.vector.reciprocal(rs[:], rs[:]), reads=[f'rs{tag}'], writes=[f'rs{tag}'])
    K.op('dve', lambda: nc.vector.scalar_tensor_tensor(ht[:], xt[:], rs[:], gm[:], ALU.mult, ALU.mult),
         reads=[xkey, f'rs{tag}', gmkey], writes=[hkey])
    K.op('dve', lambda: nc.vector.tensor_tensor(ht[:], ht[:], sh[:], ALU.add), reads=[hkey, shkey], writes=[hkey])


def stage_inproj(K, nc, D, cfg, l, C, bc, with_ctx):
    K.push()
    ident = C['ident']
    bc = {(w, j): load_bc(K, D, w, j) for w in 'lc' for j in (0, 1)}
    NB = 11
    tiles = list(range(cfg.TT))
    hT = K.sb('hT', [128, NB, 8, 128], MM)
    wst = [K.sb(f'wst{i}', [128, 8, 512]) for i in range(2)]
    xt = [K.sb(f'xt{i}', [128, 1024]) for i in range(2)]
    ht = [K.sb(f'ht{i}', [128, 1024]) for i in range(2)]
    scr = [(K.sb(f'ss{i}', [128, 1]), K.sb(f'rs{i}', [128, 1]), K.sb(f'junk{i}', [128, 1024])) for i in range(2)]
    ptr = [K.ps(f'ptr{i}', [128, 1024]) for i in range(2)]
    wb = [K.sb(f'win{i}', [128, 8, 512], MM) for i in range(2)]
    po = [K.ps(f'po{i}', [128, 512]) for i in range(2)]
    ot = [K.sb(f'ot{i}', [128, 512]) for i in range(3)]
    nw = 0
    no = 0
    tit = None
    if l == 0 and BF_TABLES and TABLES_OVERLAP and 'peer_u0' in D:
        tsrc = [K.sb(f'tbs{i}', [128, 4, 1024]) for i in range(2)]
        tdst = [K.sb(f'tbd{i}', [128, 4, 1024], BF16) for i in range(2)]
        tit = tables_iter(K, nc, D, cfg, tsrc, tdst)
    for b0 in range(0, len(tiles), NB):
        blk = tiles[b0:b0 + NB]
        for bi, tt in enumerate(blk):
            i = tt % 2
            K.dma('sp', xt[i][:], xsrc(D, cfg, l, tt), writes=[f'xt{i}'])
            w = 'c' if tt < CT else 'l'
            norm_mod(K, nc, xt[i], f'xt{i}', ht[i], f'ht{i}', bc[(w, 0)], f'bc{w}0', bc[(w, 1)], f'bc{w}1', scr[i], i)
            for k in range(8):
                K.op('pe', lambda k=k, i=i: nc.tensor.transpose(ptr[i][:, k * 128:(k + 1) * 128], ht[i][:, k * 128:(k + 1) * 128], ident[:]),
                     reads=[f'ht{i}', 'ident'], writes=[f'ptr{i}'])
            K.op('act', lambda i=i, bi=bi: nc.scalar.copy(hT[:, bi, :, :], ptr[i][:].rearrange("p (k t) -> p k t", k=8)),
                 reads=[f'ptr{i}'], writes=[('hT', bi)])
        for cch in range(IN_W // 512):
            wi = nw % 2
            nw += 1
            K.dma('sp', wst[wi][:], D['w_in'][l, :, cch * 512:(cch + 1) * 512].rearrange("(k p) n -> p k n", p=128), writes=[f'wst{wi}'])
            K.op('pool', lambda wi=wi: nc.gpsimd.tensor_copy(wb[wi][:], wst[wi][:]), reads=[f'wst{wi}'], writes=[f'win{wi}'])
            for bi, tt in enumerate(blk):
                pi = no % 2
                oi = no % 3
                no += 1
                for k in range(8):
                    K.op('pe', lambda k=k, bi=bi, pi=pi, wi=wi: nc.tensor.matmul(po[pi][:], hT[:, bi, k, :], wb[wi][:, k, :], start=(k == 0), stop=(k == 7)),
                         reads=[('hT', bi), f'win{wi}'], writes=[f'po{pi}'])
                K.op('act', lambda pi=pi, oi=oi: nc.scalar.copy(ot[oi][:], po[pi][:]), reads=[f'po{pi}'], writes=[f'ot{oi}'])
                K.dma('pool', D['pl'][tt * 128:(tt + 1) * 128, cch * 512:(cch + 1) * 512], ot[oi][:], reads=[f'ot{oi}'], writes=[('pl', tt, cch)])
                if tit is not None:
                    next(tit, None)
    if tit is not None:
        for _ in tit:
            pass
    K.pop()


def build(L, depth, stages=('mods', 'inproj'), nlayers=None):
    cfg = Cfg(L, depth)
    nc = bass.Bass("TRN2", target_bir_lowering=False)
    D = declare_io(nc, cfg)
    with ExitStack() as es:
        K = Sched(nc, es)
        C = stage_consts(K, nc, D)
        if BF_TABLES and 'peer' in stages and not TABLES_OVERLAP:
            stage_tables(K, nc, D, cfg)
        for l in range(depth if nlayers is None else nlayers):
            with_ctx = l < depth - 1
            K.push()
            bc = stage_mods(K, nc, D, cfg, l, C, with_ctx)
            if 'inproj' in stages:
                stage_inproj(K, nc, D, cfg, l, C, bc, with_ctx)
            if 'prepass' in stages:
                stage_prepass(K, nc, D, cfg, l, C, with_ctx)
            if 'diff' in stages:
                stage_diff(K, nc, D, cfg, l, C, with_ctx)
            if 'na' in stages:
                stage_na(K, nc, D, cfg, l, C, with_ctx)
            if 'ssm' in stages:
                stage_ssm(K, nc, D, cfg, l, C, with_ctx)
            if 'merge' in stages:
                stage_merge(K, nc, D, cfg, l, C, with_ctx)
            if 'peer' in stages:
                stage_peer(K, nc, D, cfg, l, C, with_ctx, final=(l == depth - 1))
            K.pop()
        K.finish()
    return nc


def na_pair_info(R):
    types = {}
    pairs = []
    for r in range(0, R, 2):
        base = int(np.clip(r - 4, 0, R - 10))
        ws0 = int(np.clip(r - 4, 0, R - 8))
        ws1 = int(np.clip(r + 1 - 4, 0, R - 8))
        key = (r - base, ws0 - base, ws1 - base)
        if key not in types:
            types[key] = len(types)
        pairs.append((r, base, types[key]))
    return pairs, list(types.keys())


def host_nab(rpb, R):
    pairs, types = na_pair_info(R)
    H = rpb.shape[0]
    cols = np.arange(64)
    col_start = np.clip(cols - 8, 0, 48)
    col_ok = (cols[None, :] >= col_start[:, None]) & (cols[None, :] < col_start[:, None] + 16)
    dc = np.clip(cols[None, :] - cols[:, None] + 15, 0, 30)
    out = np.full((H, len(types) * 5, 128, 128), -30000.0, np.float32)
    for ti, (dr0, w0, w1) in enumerate(types):
        for j in range(5):
            for kk in range(2):
                krel = 2 * j + kk
                for qq in range(2):
                    ws = (w0, w1)[qq]
                    if not (ws <= krel < ws + 8):
                        continue
                    dr = krel - (dr0 + qq) + 7
                    blk = np.where(col_ok, rpb[:, dr][:, dc], np.float32(-30000.0))
                    out[:, ti * 5 + j, kk * 64:(kk + 1) * 64, qq * 64:(qq + 1) * 64] = np.transpose(blk, (0, 2, 1))
    return out


def host_rope(L):
    t = np.arange(L)
    freqs = (10000.0 ** (-np.arange(16, dtype=np.float32) / 16)).astype(np.float32)
    ang_r = (t // 64).astype(np.float32)[:, None] * freqs
    ang_c = (t % 64).astype(np.float32)[:, None] * freqs
    ang = np.concatenate([ang_r, ang_c], 1).astype(np.float32)
    return np.concatenate([np.cos(ang), np.sin(ang)], 1).astype(np.float32)


def stage_prepass(K, nc, D, cfg, l, C, with_ctx):
    K.push()
    ident = C['ident']
    qk = [K.sb(f'qk{i}', [128, 1024]) for i in range(2)]
    qr = [K.sb(f'qr{i}', [128, 1024]) for i in range(2)]
    cs = [K.sb(f'cs{i}', [128, 64]) for i in range(2)]
    tmp = [K.sb(f'rt{i}', [128, 512]) for i in range(4)]
    nqk = [K.sb(f'nqk{i}', [128, 512]) for i in range(2)]
    pq = [K.ps(f'pq{i}', [128, 1024]) for i in range(2)]
    pn = [K.ps(f'pn{i}', [128, 512]) for i in range(2)]
    sq = [K.sb(f'sq{i}', [128, 1024]) for i in range(2)]
    sn = [K.sb(f'sn{i}', [128, 512]) for i in range(2)]
    for tt in range(cfg.TT):
        i = tt % 2
        rows = slice(tt * 128, (tt + 1) * 128)
        K.dma('sp', qk[i][:], D['pl'][rows, 256:1280], writes=[f'qk{i}'])
        K.dma('sp', nqk[i][:], D['pl'][rows, 1792:2304], writes=[f'nqk{i}'])
        src = qk[i]
        skey = f'qk{i}'
        if tt >= CT:
            K.dma('sp', cs[i][:], D['rope'][(tt - CT) * 128:(tt - CT + 1) * 128, :], writes=[f'cs{i}'])
            v = qk[i][:].rearrange("p (g rc x f) -> p g rc x f", g=16, rc=2, x=2, f=16)
            o = qr[i][:].rearrange("p (g rc x f) -> p g rc x f", g=16, rc=2, x=2, f=16)
            x1, x2 = v[:, :, :, 0, :], v[:, :, :, 1, :]
            cosb = cs[i][:, 0:32].rearrange("p (rc f) -> p rc f", rc=2).unsqueeze(1).to_broadcast([128, 16, 2, 16])
            sinb = cs[i][:, 32:64].rearrange("p (rc f) -> p rc f", rc=2).unsqueeze(1).to_broadcast([128, 16, 2, 16])
            tv = [t[:].rearrange("p (g rc f) -> p g rc f", g=16, rc=2, f=16) for t in tmp]
            rk = [skey, f'cs{i}']
            K.op('dve', lambda: nc.vector.tensor_tensor(tv[0], x1, cosb, ALU.mult), reads=rk, writes=['rt0'])
            K.op('dve', lambda: nc.vector.tensor_tensor(tv[1], x2, sinb, ALU.mult), reads=rk, writes=['rt1'])
            K.op('pool', lambda: nc.gpsimd.tensor_tensor(tv[2], x2, cosb, ALU.mult), reads=rk, writes=['rt2'])
            K.op('pool', lambda: nc.gpsimd.tensor_tensor(tv[3], x1, sinb, ALU.mult), reads=rk, writes=['rt3'])
            K.op('dve', lambda: nc.vector.tensor_tensor(o[:, :, :, 0, :], tv[0], tv[1], ALU.subtract), reads=['rt0', 'rt1'], writes=[f'qr{i}'])
            K.op('pool', lambda: nc.gpsimd.tensor_tensor(o[:, :, :, 1, :], tv[2], tv[3], ALU.add), reads=['rt2', 'rt3'], writes=[f'qr{i}'])
            src = qr[i]
            skey = f'qr{i}'
        for k in range(8):
            K.op('pe', lambda k=k, src=src: nc.tensor.transpose(pq[i][:, k * 128:(k + 1) * 128], src[:, k * 128:(k + 1) * 128], ident[:]),
                 reads=[skey, 'ident'], writes=[f'pq{i}'])
        K.op('act', lambda: nc.scalar.copy(sq[i][:], pq[i][:]), reads=[f'pq{i}'], writes=[f'sq{i}'])
        K.dma('pool', D['qkT'][:, :, rows].rearrange("b p t -> p b t"), sq[i][:].rearrange("p (b t) -> p b t", b=8),
              reads=[f'sq{i}'], writes=[('qkT', tt)])
        for k in range(4):
            K.op('pe', lambda k=k: nc.tensor.transpose(pn[i][:, k * 128:(k + 1) * 128], nqk[i][:, k * 128:(k + 1) * 128], ident[:]),
                 reads=[f'nqk{i}', 'ident'], writes=[f'pn{i}'])
        K.op('act', lambda: nc.scalar.copy(sn[i][:], pn[i][:]), reads=[f'pn{i}'], writes=[f'sn{i}'])
        K.dma('pool', D['nqkT'][:, :, rows].rearrange("b p t -> p b t"), sn[i][:].rearrange("p (b t) -> p b t", b=4),
              reads=[f'sn{i}'], writes=[('nqkT', tt)])
    K.pop()


def bcast_row(K, nc, C, row, rowkey, dst, dstkey, ncols, ps, pskey, scale=None):
    sel = C['sel']
    K.op('pe', lambda: nc.tensor.matmul(ps[:, 0:ncols], sel[0:1, 0:128], row, start=True, stop=True), reads=['sel', rowkey], writes=[pskey])
    if scale is None:
        K.op('dve', lambda: nc.vector.tensor_copy(dst, ps[:, 0:ncols]), reads=[pskey], writes=[dstkey])
    else:
        K.op('dve', lambda: nc.vector.tensor_scalar(dst, ps[:, 0:ncols], float(scale), None, ALU.mult), reads=[pskey], writes=[dstkey])


def stage_diff(K, nc, D, cfg, l, C, with_ctx):
    K.push()
    TT, NT = cfg.TT, cfg.NT
    NTOK = TT * 128
    lam_init = LAM_INIT[l]
    dl = K.sb('dl', [1, 256])
    K.dma('sp', dl[:], D['diff_lambda'][l:l + 1, :], writes=['dl'])
    pr = K.sb('pr', [1, 2, 64])
    dv = dl[:].rearrange("o (a b f) -> o a b f", a=2, b=2, f=64)
    K.op('dve', lambda: nc.vector.tensor_tensor(pr[:], dv[:, :, 0, :], dv[:, :, 1, :], ALU.mult), reads=['dl'], writes=['pr'])
    e2 = K.sb('e2', [1, 2])
    K.op('dve', lambda: nc.vector.reduce_sum(e2[:], pr[:], axis=AX.X), reads=['pr'], writes=['e2'])
    K.op('act', lambda: nc.scalar.activation(e2[:], e2[:], AF.Exp), reads=['e2'], writes=['e2'])
    nl = K.sb('nl', [1, 1])
    K.op('dve', lambda: nc.vector.tensor_tensor(nl[:], e2[:, 1:2], e2[:, 0:1], ALU.subtract), reads=['e2'], writes=['nl'])
    K.op('dve', lambda: nc.vector.tensor_scalar(nl[:], nl[:], -lam_init, None, ALU.add), reads=['nl'], writes=['nl'])
    psm = K.ps('psm', [128, 512])
    neglam = K.sb('neglam', [128, 1])
    bcast_row(K, nc, C, nl[:], 'nl', neglam[:], 'neglam', 1, psm, 'psm')
    sg = K.sb('sg', [1, 128])
    K.dma('sp', sg[:], D['diff_subln_g'][l:l + 1, :], writes=['sg'])
    gbc = K.sb('dgbc', [128, 128])
    bcast_row(K, nc, C, sg[:], 'sg', gbc[:], 'dgbc', 128, psm, 'psm', scale=1.0 - lam_init)
    qT = K.sb('dqT', [128, NTOK], BF16 if QK_BF16 else MM)
    kT = K.sb('dkT', [128, NTOK], BF16 if QK_BF16 else MM)
    va = K.sb('dva', [128, TT, 130], BF16 if AV_BF16 else MM)
    stg = [K.sb(f'dstg{i}', [128, 768]) for i in range(2)]
    nstg = [0]
    onez = K.sb('onez', [128, TT, 2])
    K.op('pool', lambda: nc.gpsimd.memset(onez[:, :, 0:1], 1.0), writes=['onez'])
    K.op('pool', lambda: nc.gpsimd.memset(onez[:, :, 1:2], 0.0), writes=['onez'])

    def load_round(dst_ap, src_ap, key, ncols):
        si = nstg[0] % 2
        nstg[0] += 1
        K.dma('sp', stg[si][:, 0:ncols], src_ap, writes=[f'dstg{si}'])
        eng = 'dve' if si == 0 else 'pool'
        cp = nc.vector.tensor_copy if si == 0 else nc.gpsimd.tensor_copy
        K.op(eng, lambda: cp(dst_ap, stg[si][:, 0:ncols]), reads=[f'dstg{si}'], writes=[key])
    st = [K.ps(f'dst{i}', [128, 512]) for i in range(2)]
    pt = [K.sb(f'dpt{i}', [128, 512], BF16 if AV_BF16 else MM) for i in range(2)]
    acc = [K.ps(f'dacc{i}', [128, 512]) for i in range(4)]
    om = [K.sb(f'dom{i}', [128, 4, 128]) for i in range(2)]
    rec = K.sb('drec', [128, 4])
    dd = K.sb('ddd', [128, 4, 128])
    yy = K.sb('dyy', [128, 4, 128])
    ss = K.sb('dss', [128, 4])
    junk = K.sb('djunk', [128, 128])
    n = 0
    for h in range(4):
        for c0 in range(0, NTOK, 768):
            c1 = min(NTOK, c0 + 768)
            load_round(qT[:, c0:c1], D['qkT'][h, :, c0:c1], 'dqT', c1 - c0)
            load_round(kT[:, c0:c1], D['qkT'][4 + h, :, c0:c1], 'dkT', c1 - c0)
        for t0 in range(0, TT, 6):
            t1 = min(TT, t0 + 6)
            si = nstg[0] % 2
            nstg[0] += 1
            sv_ = stg[si][:, 0:(t1 - t0) * 128].rearrange("p (t c) -> p t c", c=128)
            K.dma('sp', sv_, D['pl'][t0 * 128:t1 * 128, 1280 + h * 128:1280 + (h + 1) * 128].rearrange("(t p) c -> p t c", p=128), writes=[f'dstg{si}'])
            if si == 0:
                K.op('dve', lambda: nc.vector.tensor_copy(va[:, t0:t1, 0:128], sv_), reads=[f'dstg{si}'], writes=['dva'])
            else:
                K.op('pool', lambda: nc.gpsimd.tensor_copy(va[:, t0:t1, 0:128], sv_), reads=[f'dstg{si}'], writes=['dva'])
        K.op('dve', lambda: nc.vector.tensor_copy(va[:, :, 128:130], onez[:]), reads=['onez'], writes=['dva'])
        blocks = [(CT * 128 + qb * 512, 512, list(range(TT))) for qb in range(NT // 4)]
        if with_ctx:
            blocks.append((0, 256, [0, 1]))
        for (q0, N, kts) in blocks:
            nq = N // 128
            for m in range(2):
                ms = slice(m * 64, (m + 1) * 64)

                def S(kt, n):
                    K.op('pe', lambda: nc.tensor.matmul(st[n % 2][:, 0:N], kT[ms, kt * 128:(kt + 1) * 128], qT[ms, q0:q0 + N], start=True, stop=True),
                         reads=['dkT', 'dqT'], writes=[f'dst{n % 2}'])
                    K.op('act', lambda: nc.scalar.activation(pt[n % 2][:, 0:N], st[n % 2][:, 0:N], AF.Exp, scale=0.125),
                         reads=[f'dst{n % 2}'], writes=[f'dpt{n % 2}'])

                def AV(kt, n, first, last):
                    for qs in range(nq):
                        K.op('pe', lambda qs=qs: nc.tensor.matmul(acc[qs][:, 0:130], pt[n % 2][:, qs * 128:(qs + 1) * 128], va[:, kt, :], start=first, stop=last),
                             reads=[f'dpt{n % 2}', 'dva'], writes=[f'dacc{qs}'])
                S(kts[0], n)
                for ki, kt in enumerate(kts):
                    if ki + 1 < len(kts):
                        S(kts[ki + 1], n + 1)
                    AV(kt, n, ki == 0, ki == len(kts) - 1)
                    n += 1
                for qs in range(nq):
                    K.op('dve', lambda qs=qs: nc.vector.reciprocal(rec[:, qs:qs + 1], acc[qs][:, 128:129]), reads=[f'dacc{qs}'], writes=['drec'])
                    K.op('dve', lambda qs=qs: nc.vector.tensor_scalar(om[m][:, qs, :], acc[qs][:, 0:128], rec[:, qs:qs + 1], None, ALU.mult),
                         reads=[f'dacc{qs}', 'drec'], writes=[f'dom{m}'])
            for qs in range(nq):
                K.op('dve', lambda qs=qs: nc.vector.scalar_tensor_tensor(dd[:, qs, :], om[1][:, qs, :], neglam[:], om[0][:, qs, :], ALU.mult, ALU.add),
                     reads=['dom0', 'dom1', 'neglam'], writes=['ddd'])
                K.op('act', lambda qs=qs: nc.scalar.activation(junk[:], dd[:, qs, :], AF.Square, accum_out=ss[:, qs:qs + 1]), reads=['ddd'], writes=['djunk', 'dss'])
            K.op('dve', lambda: nc.vector.tensor_scalar(ss[:, 0:nq], ss[:, 0:nq], 1.0 / 128.0, 1e-6, ALU.mult, ALU.add), reads=['dss'], writes=['dss'])
            K.op('act', lambda: nc.scalar.activation(ss[:, 0:nq], ss[:, 0:nq], AF.Sqrt), reads=['dss'], writes=['dss'])
            K.op('dve', lambda: nc.vector.reciprocal(ss[:, 0:nq], ss[:, 0:nq]), reads=['dss'], writes=['dss'])
            for qs in range(nq):
                K.op('dve', lambda qs=qs: nc.vector.scalar_tensor_tensor(yy[:, qs, :], dd[:, qs, :], ss[:, qs:qs + 1], gbc[:], ALU.mult, ALU.mult),
                     reads=['ddd', 'dss', 'dgbc'], writes=['dyy'])
            K.dma('pool', D['ymix'][q0:q0 + N, 256 + h * 128:256 + (h + 1) * 128].rearrange("(s p) c -> p s c", p=128), yy[:, 0:nq, :],
                  reads=['dyy'], writes=[('ymix', 'd', h, q0)])
            K.maybe_barrier()
    K.pop()


def stage_na(K, nc, D, cfg, l, C, with_ctx):
    K.push()
    TT, NT, R = cfg.TT, cfg.NT, cfg.R
    NTOK = TT * 128
    pairs, types = na_pair_info(R)
    ident = C['ident']
    id8 = K.sb('id8', [128, 128])
    K.op('dve', lambda: nc.vector.tensor_scalar(id8[:], ident[:], 8.0, None, ALU.mult), reads=['ident'], writes=['id8'])
    nq = K.sb('nq', [64, NTOK])
    nk = K.sb('nk', [64, NTOK])
    nv = K.sb('nv', [128, TT, 65])
    nab = K.sb('nab', [128, len(types) * 5, 128])
    S = [K.ps(f'nS{i}', [128, 1024]) for i in range(2)]
    P = [K.sb(f'nP{i}', [128, 1024]) for i in range(2)]
    acc = [K.ps(f'nacc{i}', [128, 512]) for i in range(2)]
    rec = K.sb('nrec', [128, 1])
    ysb = [K.sb(f'nys{i}', [128, 64]) for i in range(2)]
    n = 0
    for h in range(4):
        K.dma('sp', nq[:], D['nqkT'][h // 2, (h % 2) * 64:(h % 2 + 1) * 64, :], writes=['nq'])
        K.dma('sp', nk[:], D['nqkT'][2 + h // 2, (h % 2) * 64:(h % 2 + 1) * 64, :], writes=['nk'])
        for t0 in range(0, TT, 6):
            t1 = min(TT, t0 + 6)
            K.dma('sp', nv[:, t0:t1, 0:64], D['pl'][t0 * 128:t1 * 128, 2304 + h * 64:2304 + (h + 1) * 64].rearrange("(t p) c -> p t c", p=128), writes=['nv'])
        K.op('pool', lambda: nc.gpsimd.memset(nv[:, :, 64:65], 1.0), writes=['nv'])
        K.dma('sp', nab[:], D['nab'][l, h].rearrange("j k q -> k j q"), writes=['nab'])
        jobs = []
        for (r, base, ty) in pairs:
            tl = [(CT + (base + 2 * j) // 2, ty * 5 + j) for j in range(5)] + [(0, None), (1, None)]
            jobs.append((CT * 128 + r * 64, tl))
        if with_ctx:
            for qt in range(2):
                jobs.append((qt * 128, [(0, None), (1, None)]))
        for (q0, tl) in jobs:
            i = n % 2
            n += 1
            nt = len(tl)
            for j, (kt, bj) in enumerate(tl):
                K.op('pe', lambda j=j, kt=kt, bj=bj: nc.tensor.matmul(S[i][:, j * 128:(j + 1) * 128], nk[:, kt * 128:(kt + 1) * 128], nq[:, q0:q0 + 128],
                                                                  start=True, stop=(bj is None)),
                     reads=['nk', 'nq'], writes=[f'nS{i}'])
                if bj is not None:
                    K.op('pe', lambda j=j, bj=bj: nc.tensor.matmul(S[i][:, j * 128:(j + 1) * 128], id8[:], nab[:, bj, :], start=False, stop=True),
                         reads=['id8', 'nab'], writes=[f'nS{i}'])
            for c0 in range(0, nt * 128, 512):
                c1 = min(nt * 128, c0 + 512)
                K.op('act', lambda c0=c0, c1=c1: nc.scalar.activation(P[i][:, c0:c1], S[i][:, c0:c1], AF.Exp, scale=0.125),
                     reads=[f'nS{i}'], writes=[f'nP{i}'])
            for j, (kt, bj) in enumerate(tl):
                K.op('pe', lambda j=j, kt=kt: nc.tensor.matmul(acc[i][:, 0:65], P[i][:, j * 128:(j + 1) * 128], nv[:, kt, :], start=(j == 0), stop=(j == nt - 1)),
                     reads=[f'nP{i}', 'nv'], writes=[f'nacc{i}'])
            K.op('dve', lambda: nc.vector.reciprocal(rec[:], acc[i][:, 64:65]), reads=[f'nacc{i}'], writes=['nrec'])
            K.op('dve', lambda: nc.vector.tensor_scalar(ysb[i][:], acc[i][:, 0:64], rec[:], None, ALU.mult), reads=[f'nacc{i}', 'nrec'], writes=[f'nys{i}'])
            K.dma('pool', D['ymix'][q0:q0 + 128, 768 + h * 64:768 + (h + 1) * 64], ysb[i][:], reads=[f'nys{i}'], writes=[('ymix', 'n', h, q0)])
    K.pop()


def core_inputs(inp, b, L, depth, half=0):
    R = L // 64
    sel = np.zeros((2, 256), np.float32)
    sel[0, :128] = 1
    sel[1, 128:] = 1
    f = lambda a: np.ascontiguousarray(a, dtype=np.float32)
    hs = [host_ssm(inp, l) for l in range(depth)]
    im = dict(
        x=inp['x'][b, :L], ctx=inp['ctx'][b], cc=np.stack([inp['c'][b], inp['c_ctx']]),
        ada_w=inp['ada_w'][:depth], ada_b=inp['ada_b'][:depth],
        norm_g=np.concatenate([inp['norm1_g'], inp['norm2_g']], 1)[:depth],
        w_in=inp['w_in'][:depth], ident=np.eye(128, dtype=np.float32), sel=sel,
        rope=host_rope(L), nab=np.stack([host_nab(inp['na_rpb'][l], R) for l in range(depth)]),
        diff_lambda=inp['diff_lambda'][:depth].reshape(depth, 256), diff_subln_g=inp['diff_subln_g'][:depth],
        jmat=np.eye(128, dtype=np.float32)[::-1],
        ssm_sc=np.stack([hs[l][0] for l in range(depth)]), ssm_b=np.stack([hs[l][1] for l in range(depth)]),
        ssm_c=np.stack([hs[l][2] for l in range(depth)]),
        ssm_dT=np.stack([inp['ssm_d'][l].reshape(2, 128).T for l in range(depth)]), ssm_glu_w=inp['ssm_glu_w'][:depth],
        w_br_ssm=inp['w_br_ssm'][:depth], w_br_diff=inp['w_br_diff'][:depth], w_br_na=inp['w_br_na'][:depth], w_out=inp['w_out'][:depth],
        peer_wq=inp['peer_wq'][:depth], peer_sk=inp['peer_subkeys'][:depth].reshape(depth, 16, 128, 128),
        final_g=inp['final_norm_g'].reshape(1, 1024),
        iota16=np.tile(np.arange(16, dtype=np.float32), (128, 1)),
    )
    out = {k: f(v) for k, v in im.items()}
    if PAIR_SPLIT:
        nt2 = (L // 128) // 2
        out['rowidx'] = np.ascontiguousarray((CT * 128 + half * (L // 2) + np.arange(nt2)[None, :] * 128 + np.arange(128)[:, None]).astype(np.int32))
    im = {}
    for i in range(depth):
        im[f'peer_u{i}'] = inp['peer_u'][i]
        im[f'peer_v{i}'] = inp['peer_v'][i]
    out.update({k: f(v) for k, v in im.items()})
    return out


TWO_PI = float(2 * np.pi)


def host_ssm(inp, l):
    lre, lim, ls = inp['ssm_lambda_re'][l], inp['ssm_lambda_im'][l], inp['ssm_log_step'][l]
    sc = np.zeros((128, 48), np.float32)
    bb = np.zeros((2, 16, 128, 128), np.float32)
    cm = np.zeros((2, 16, 128, 128), np.float32)
    for d in range(2):
        for st in range(8):
            col = (d * 8 + st) * 3
            for gl in range(2):
                g = 2 * st + gl
                rows = slice(gl * 64, (gl + 1) * 64)
                sc[rows, col + 0] = lre[d, g]
                sc[rows, col + 1] = lim[d, g]
                sc[rows, col + 2] = ls[d, g]
                c0 = (g - 8 * (st // 4)) * 16
                bb[0, d * 8 + st, rows, c0:c0 + 16] = inp['ssm_b_re'][l, d, g]
                bb[1, d * 8 + st, rows, c0:c0 + 16] = inp['ssm_b_im'][l, d, g]
                cm[0, d * 8 + st, rows, c0:c0 + 16] = inp['ssm_c_re'][l, d, g].T
                cm[1, d * 8 + st, rows, c0:c0 + 16] = inp['ssm_c_im'][l, d, g].T
    return sc, bb, cm


def gelu_tanh(K, nc, eng_a, out, x, xkey, outkey, t1, t1key):
    K.op('dve', lambda: nc.vector.tensor_tensor(t1, x, x, ALU.mult), reads=[xkey], writes=[t1key])
    K.op('dve', lambda: nc.vector.tensor_scalar(t1, t1, 0.044715, 1.0, ALU.mult, ALU.add), reads=[t1key], writes=[t1key])
    K.op('dve', lambda: nc.vector.tensor_tensor(t1, t1, x, ALU.mult), reads=[t1key, xkey], writes=[t1key])
    K.op('act', lambda: nc.scalar.activation(t1, t1, AF.Sigmoid, scale=1.5957691216057308), reads=[t1key], writes=[t1key])
    K.op('dve', lambda: nc.vector.tensor_tensor(out, x, t1, ALU.mult), reads=[xkey, t1key], writes=[outkey])


def stage_ssm(K, nc, D, cfg, l, C, with_ctx):
    K.push()
    TT, NT = cfg.TT, cfg.NT
    ident = C['ident']
    jm = K.sb('jm', [128, 128])
    K.dma('sp', jm[:], D['jmat'], writes=['jm'])
    T = 512
    sc = K.sb('ssc', [128, 16, 3])
    K.dma('sp', sc[:], D['ssm_sc'][l].rearrange("p (c j) -> p c j", j=3), writes=['ssc'])
    names = ['dtv', 'lre', 'a', 'th', 'rho', 'cos', 'sin', 'x', 'den', 'cfr', 'cfi', 'k', 'tmp', 'tmp2', 'nsin', 'ncfi']
    V = {nm: K.sb('sv_' + nm, [128, 16]) for nm in names}
    ki = K.sb('sv_ki', [128, 16], I32)
    lim = sc[:, :, 1]

    def dv(fn, reads, writes):
        K.op('dve', fn, reads=['sv_' + r if r != 'ssc' else r for r in reads], writes=['sv_' + w for w in writes])

    SV = ['svall', 'ssc']

    def d1(fn):
        K.op('dve', fn, reads=SV, writes=['svall'])

    def tt(o, a_, b_, op):
        d1(lambda: nc.vector.tensor_tensor(o, a_, b_, op))

    def ts(o, a_, s1, s2, op0, op1=None):
        if op1 is None:
            d1(lambda: nc.vector.tensor_scalar(o, a_, s1, None, op0))
        else:
            d1(lambda: nc.vector.tensor_scalar(o, a_, s1, s2, op0, op1))

    def to_int_float(dst, src):
        d1(lambda: nc.vector.tensor_copy(ki[:], src))
        d1(lambda: nc.vector.tensor_copy(dst, ki[:]))

    def horner(dst, z, coefs):
        ts(dst, z, float(coefs[-1]), 1.0, ALU.mult, ALU.add)
        for cf in reversed(coefs[:-1]):
            tt(dst, dst, z, ALU.mult)
            ts(dst, dst, float(cf), 1.0, ALU.mult, ALU.add)

    x_, k_, t_, t2_ = V['x'][:], V['k'][:], V['tmp'][:], V['tmp2'][:]
    ts(x_, sc[:, :, 2], 1.0 / 0.6931471805599453, None, ALU.mult)
    to_int_float(k_, x_)
    ts(t_, k_, -0.693359375, None, ALU.mult)
    tt(x_, sc[:, :, 2], t_, ALU.add)
    ts(t_, k_, 2.12194440e-4, None, ALU.mult)
    tt(x_, x_, t_, ALU.add)
    horner(V['dtv'][:], x_, [1.0 / i for i in range(1, 14)])
    for j in range(1, 17):
        ts(t_, k_, float(-j), -0.5, ALU.is_le, ALU.mult)
        ts(t_, t_, 1.0, None, ALU.add)
        tt(V['dtv'][:], V['dtv'][:], t_, ALU.mult)
    ts(V['lre'][:], sc[:, :, 0], -1e-4, None, ALU.min)
    tt(V['a'][:], V['lre'][:], V['dtv'][:], ALU.mult)
    tt(V['th'][:], lim, V['dtv'][:], ALU.mult)
    horner(V['rho'][:], V['a'][:], [1.0 / i for i in range(1, 10)])
    ts(x_, V['th'][:], 1.0 / TWO_PI, None, ALU.mult)
    to_int_float(k_, x_)
    ts(t_, k_, -6.28125, None, ALU.mult)
    tt(x_, V['th'][:], t_, ALU.add)
    ts(t_, k_, -1.9353071795864769e-3, None, ALU.mult)
    tt(x_, x_, t_, ALU.add)
    for sgn, cmpop, thr in ((-1.0, ALU.is_gt, float(np.pi)), (1.0, ALU.is_lt, -float(np.pi))):
        ts(t_, x_, thr, None, cmpop)
        ts(t2_, t_, sgn * 6.28125, None, ALU.mult)
        tt(x_, x_, t2_, ALU.add)
        ts(t2_, t_, sgn * 1.9353071795864769e-3, None, ALU.mult)
        tt(x_, x_, t2_, ALU.add)
    ts(x_, x_, 0.25, None, ALU.mult)
    tt(k_, x_, x_, ALU.mult)
    horner(V['cos'][:], k_, [-1.0 / ((2 * i) * (2 * i - 1)) for i in range(1, 9)])
    horner(V['sin'][:], k_, [-1.0 / ((2 * i) * (2 * i + 1)) for i in range(1, 9)])
    tt(V['sin'][:], V['sin'][:], x_, ALU.mult)
    for _ in range(2):
        tt(t_, V['cos'][:], V['cos'][:], ALU.mult)
        tt(t2_, V['sin'][:], V['sin'][:], ALU.mult)
        tt(V['sin'][:], V['sin'][:], V['cos'][:], ALU.mult)
        ts(V['sin'][:], V['sin'][:], 2.0, None, ALU.mult)
        tt(V['cos'][:], t_, t2_, ALU.subtract)
    tt(t_, V['cos'][:], V['cos'][:], ALU.mult)
    tt(t2_, V['sin'][:], V['sin'][:], ALU.mult)
    tt(t_, t_, t2_, ALU.add)
    ts(t_, t_, -0.5, 1.5, ALU.mult, ALU.add)
    tt(V['cos'][:], V['cos'][:], t_, ALU.mult)
    tt(V['sin'][:], V['sin'][:], t_, ALU.mult)

    def dv(fn, reads, writes):
        K.op('dve', fn, reads=SV, writes=['svall'])

    dv(lambda: nc.vector.tensor_tensor(V['tmp'][:], V['rho'][:], V['cos'][:], ALU.mult), ['rho', 'cos'], ['tmp'])
    dv(lambda: nc.vector.tensor_scalar(V['tmp'][:], V['tmp'][:], -1.0, None, ALU.add), ['tmp'], ['tmp'])
    dv(lambda: nc.vector.tensor_tensor(V['tmp2'][:], V['rho'][:], V['sin'][:], ALU.mult), ['rho', 'sin'], ['tmp2'])
    dv(lambda: nc.vector.tensor_tensor(V['den'][:], V['lre'][:], V['lre'][:], ALU.mult), ['lre'], ['den'])
    dv(lambda: nc.vector.tensor_tensor(V['k'][:], lim, lim, ALU.mult), ['ssc'], ['k'])
    dv(lambda: nc.vector.tensor_tensor(V['den'][:], V['den'][:], V['k'][:], ALU.add), ['den', 'k'], ['den'])
    dv(lambda: nc.vector.reciprocal(V['den'][:], V['den'][:]), ['den'], ['den'])
    dv(lambda: nc.vector.tensor_tensor(V['cfr'][:], V['tmp'][:], V['lre'][:], ALU.mult), ['tmp', 'lre'], ['cfr'])
    dv(lambda: nc.vector.tensor_tensor(V['k'][:], V['tmp2'][:], lim, ALU.mult), ['tmp2', 'ssc'], ['k'])
    dv(lambda: nc.vector.tensor_tensor(V['cfr'][:], V['cfr'][:], V['k'][:], ALU.add), ['cfr', 'k'], ['cfr'])
    dv(lambda: nc.vector.tensor_tensor(V['cfr'][:], V['cfr'][:], V['den'][:], ALU.mult), ['cfr', 'den'], ['cfr'])
    dv(lambda: nc.vector.tensor_tensor(V['cfi'][:], V['tmp2'][:], V['lre'][:], ALU.mult), ['tmp2', 'lre'], ['cfi'])
    dv(lambda: nc.vector.tensor_tensor(V['k'][:], V['tmp'][:], lim, ALU.mult), ['tmp', 'ssc'], ['k'])
    dv(lambda: nc.vector.tensor_tensor(V['cfi'][:], V['cfi'][:], V['k'][:], ALU.subtract), ['cfi', 'k'], ['cfi'])
    dv(lambda: nc.vector.tensor_tensor(V['cfi'][:], V['cfi'][:], V['den'][:], ALU.mult), ['cfi', 'den'], ['cfi'])
    dv(lambda: nc.vector.tensor_scalar(V['ncfi'][:], V['cfi'][:], -1.0, None, ALU.mult), ['cfi'], ['ncfi'])
    dv(lambda: nc.vector.tensor_scalar(V['nsin'][:], V['sin'][:], -1.0, None, ALU.mult), ['sin'], ['nsin'])
    Bt = K.sb('sBt', [128, 16, 2, 128])
    Cm = K.sb('sCm', [128, 16, 2, 128])
    K.dma('sp', Cm[:, :, 0, :], D['ssm_c'][l, 0].rearrange("c p f -> p c f"), writes=['sCm'])
    K.dma('sp', Cm[:, :, 1, :], D['ssm_c'][l, 1].rearrange("c p f -> p c f"), writes=['sCm'])
    K.op('pool', lambda: nc.gpsimd.tensor_scalar(Cm[:, :, 1, :], Cm[:, :, 1, :], -1.0, None, ALU.mult), reads=['sCm'], writes=['sCm'])
    K.push()
    braw = K.sb('sbraw', [128, 16, 2, 128])
    K.dma('sp', braw[:, :, 0, :], D['ssm_b'][l, 0].rearrange("c p f -> p c f"), writes=['sbraw'])
    K.dma('sp', braw[:, :, 1, :], D['ssm_b'][l, 1].rearrange("c p f -> p c f"), writes=['sbraw'])
    bbar = [K.sb(f'sbbar{i}', [128, 2, 128]) for i in range(2)]
    t4 = [K.sb(f'sbt{i}', [128, 128]) for i in range(2)]
    pbt = [K.ps(f'spbt{i}', [128, 256]) for i in range(2)]
    for c in range(16):
        i = c % 2
        cr, ci, nci = V['cfr'][:, c:c + 1], V['cfi'][:, c:c + 1], V['ncfi'][:, c:c + 1]
        K.op('dve', lambda: nc.vector.tensor_scalar(t4[0][:], braw[:, c, 1, :], nci, None, ALU.mult), reads=['sbraw', 'svall'], writes=['sbt0'])
        K.op('dve', lambda: nc.vector.scalar_tensor_tensor(bbar[i][:, 0, :], braw[:, c, 0, :], cr, t4[0][:], ALU.mult, ALU.add),
             reads=['sbraw', 'svall', 'sbt0'], writes=[f'sbbar{i}'])
        K.op('dve', lambda: nc.vector.tensor_scalar(t4[1][:], braw[:, c, 0, :], ci, None, ALU.mult), reads=['sbraw', 'svall'], writes=['sbt1'])
        K.op('dve', lambda: nc.vector.scalar_tensor_tensor(bbar[i][:, 1, :], braw[:, c, 1, :], cr, t4[1][:], ALU.mult, ALU.add),
             reads=['sbraw', 'svall', 'sbt1'], writes=[f'sbbar{i}'])
        for ri in range(2):
            K.op('pe', lambda ri=ri: nc.tensor.transpose(pbt[i][:, ri * 128:(ri + 1) * 128], bbar[i][:, ri, :], ident[:]),
                 reads=[f'sbbar{i}', 'ident'], writes=[f'spbt{i}'])
        K.op('act', lambda: nc.scalar.copy(Bt[:, c, :, :], pbt[i][:].rearrange("p (r f) -> p r f", r=2)), reads=[f'spbt{i}'], writes=['sBt'])
    K.pop()
    E = K.sb('sE', [128, 16, 2, T])
    et = [K.sb(f'set{i}', [128, T // 2]) for i in range(2)]
    for c in range(16):
        K.op('dve', lambda: nc.vector.tensor_copy(E[:, c, 0, 0:1], V['cos'][:, c:c + 1]), reads=['svall'], writes=[('sE', c)])
        K.op('dve', lambda: nc.vector.tensor_copy(E[:, c, 1, 0:1], V['sin'][:, c:c + 1]), reads=['svall'], writes=[('sE', c)])
        m = 1
        while m < T:
            wr, wi = E[:, c, 0, m - 1:m], E[:, c, 1, m - 1:m]
            er, ei = E[:, c, 0, 0:m], E[:, c, 1, 0:m]
            K.op('dve', lambda: nc.vector.tensor_scalar(et[0][:, 0:m], ei, wi, None, ALU.mult), reads=[('sE', c)], writes=['set0'])
            K.op('dve', lambda: nc.vector.tensor_scalar(et[1][:, 0:m], ei, wr, None, ALU.mult), reads=[('sE', c)], writes=['set1'])
            K.op('dve', lambda: nc.vector.scalar_tensor_tensor(E[:, c, 0, m:2 * m], er, wr, et[0][:, 0:m], ALU.mult, ALU.subtract),
                 reads=[('sE', c), 'set0'], writes=[('sE', c)])
            K.op('dve', lambda: nc.vector.scalar_tensor_tensor(E[:, c, 1, m:2 * m], er, wi, et[1][:, 0:m], ALU.mult, ALU.add),
                 reads=[('sE', c), 'set1'], writes=[('sE', c)])
            m *= 2
    dT = K.sb('sdT', [128, 2])
    K.dma('sp', dT[:], D['ssm_dT'][l], writes=['sdT'])
    gw = K.sb('sgw', [128, 2, 512])
    K.dma('sp', gw[:], D['ssm_glu_w'][l].rearrange("(k p) n -> p k n", p=128), writes=['sgw'])
    carry = K.sb('scarry', [128, 8, 2])
    ut = [K.sb(f'sut{i}', [128, 4, 256]) for i in range(1)]
    uT = [K.sb(f'suT{i}', [128, 2, T]) for i in range(2)]
    puT = K.ps('spuT', [128, 2, T])
    pbu = [K.ps(f'spbu{i}', [128, 2, T]) for i in range(1)]
    bu = [K.sb(f'sbu{i}', [128, 2, T]) for i in range(2)]
    X = [K.sb(f'sX{i}', [128, 2, T]) for i in range(2)]
    W = [K.sb(f'sW{i}', [128, 2, T]) for i in range(2)]
    S = [K.sb(f'sS{i}', [128, 2, T]) for i in range(2)]
    tq_all = [K.sb(f'stq{i}', [128, T]) for i in range(8)]
    py = K.ps('spy', [128, 2, T])
    yTf = K.sb('syTf', [128, 2, T])
    ytok = K.sb('sytok', [128, 4, 256])
    yb = K.sb('syb', [128, 2, T])
    g1 = K.sb('sg1', [128, 2, T])
    pz = K.ps('spz', [128, T])
    zs = K.sb('szs', [128, 512])
    zo = K.sb('szo', [128, 256])
    nu = 0
    for d in range(2):
        chunks = [([0, 1], True)] + [([CT + 4 * c + i for i in range(4)], False) for c in range(NT // 4)]
        if d == 1:
            chunks = [([1, 0], True)] + [([CT + NT - 1 - (4 * c + i) for i in range(4)], False) for c in range(NT // 4)]
        for ci, (tl, is_ctx) in enumerate(chunks):
            Tc = len(tl) * 128
            ui = ci % 2
            for j, tt in enumerate(tl):
                K.dma('sp', ut[0][:, j, :], D['pl'][tt * 128:(tt + 1) * 128, 0:256], writes=['sut0'])
            for j in range(len(tl)):
                for kc in range(2):
                    K.op('pe', lambda j=j, kc=kc: nc.tensor.matmul(puT[:, kc, j * 128:(j + 1) * 128], ut[0][:, j, kc * 128:(kc + 1) * 128],
                                                                 (ident if d == 0 else jm)[:], start=True, stop=True),
                         reads=['sut0', 'ident', 'jm'], writes=['spuT'])
            K.op('act', lambda: nc.scalar.copy(uT[ui][:, :, 0:Tc], puT[:, :, 0:Tc]), reads=['spuT'], writes=[f'suT{ui}'])
            need_out = (not is_ctx) or with_ctx
            for st in range(8):
                c = d * 8 + st
                kc = st // 4
                b = nu % 2
                nu += 1
                tq = tq_all[4 * b:4 * b + 4]
                tqk = [f'stq{4 * b + i}' for i in range(4)]
                for ri in range(2):
                    K.op('pe', lambda ri=ri: nc.tensor.matmul(pbu[0][:, ri, 0:Tc], Bt[:, c, ri, :], uT[ui][:, kc, 0:Tc], start=True, stop=True),
                         reads=['sBt', f'suT{ui}'], writes=['spbu0'])
                K.op('act', lambda: nc.scalar.copy(bu[b][:, :, 0:Tc], pbu[0][:, :, 0:Tc]), reads=['spbu0'], writes=[f'sbu{b}'])
                Er, Ei = E[:, c, 0, 0:Tc], E[:, c, 1, 0:Tc]
                br, bi = bu[b][:, 0, 0:Tc], bu[b][:, 1, 0:Tc]
                ek = ('sE', c)
                K.op('dve', lambda: nc.vector.tensor_tensor(tq[0][:, 0:Tc], Er, br, ALU.mult), reads=[ek, f'sbu{b}'], writes=[tqk[0]])
                K.op('dve', lambda: nc.vector.tensor_tensor(tq[1][:, 0:Tc], Ei, bi, ALU.mult), reads=[ek, f'sbu{b}'], writes=[tqk[1]])
                K.op('dve', lambda: nc.vector.tensor_tensor(X[b][:, 0, 0:Tc], tq[0][:, 0:Tc], tq[1][:, 0:Tc], ALU.add), reads=[tqk[0], tqk[1]], writes=[f'sX{b}'])
                K.op('pool', lambda: nc.gpsimd.tensor_tensor(tq[2][:, 0:Tc], Er, bi, ALU.mult), reads=[ek, f'sbu{b}'], writes=[tqk[2]])
                K.op('pool', lambda: nc.gpsimd.tensor_tensor(tq[3][:, 0:Tc], Ei, br, ALU.mult), reads=[ek, f'sbu{b}'], writes=[tqk[3]])
                K.op('pool', lambda: nc.gpsimd.tensor_tensor(X[b][:, 1, 0:Tc], tq[2][:, 0:Tc], tq[3][:, 0:Tc], ALU.subtract), reads=[tqk[2], tqk[3]], writes=[f'sX{b}'])
                rho_b = V['rho'][:, c:c + 1].to_broadcast([128, Tc])
                for ri in range(2):
                    init = 0.0 if ci == 0 else carry[:, st, ri:ri + 1]
                    K.op('dve', lambda ri=ri, init=init: nc.vector.tensor_tensor_scan(W[b][:, ri, 0:Tc], rho_b, X[b][:, ri, 0:Tc], init, ALU.mult, ALU.add),
                         reads=[f'sX{b}', 'svall', ('scarry', st)], writes=[f'sW{b}'])
                wr_, wi_ = W[b][:, 0, 0:Tc], W[b][:, 1, 0:Tc]
                K.op('dve', lambda: nc.vector.tensor_tensor(tq[0][:, 0:Tc], Er, wr_, ALU.mult), reads=[ek, f'sW{b}'], writes=[tqk[0]])
                K.op('dve', lambda: nc.vector.tensor_tensor(tq[1][:, 0:Tc], Ei, wi_, ALU.mult), reads=[ek, f'sW{b}'], writes=[tqk[1]])
                K.op('dve', lambda: nc.vector.tensor_tensor(S[b][:, 0, 0:Tc], tq[0][:, 0:Tc], tq[1][:, 0:Tc], ALU.subtract), reads=[tqk[0], tqk[1]], writes=[f'sS{b}'])
                K.op('pool', lambda: nc.gpsimd.tensor_tensor(tq[2][:, 0:Tc], Er, wi_, ALU.mult), reads=[ek, f'sW{b}'], writes=[tqk[2]])
                K.op('pool', lambda: nc.gpsimd.tensor_tensor(tq[3][:, 0:Tc], Ei, wr_, ALU.mult), reads=[ek, f'sW{b}'], writes=[tqk[3]])
                K.op('pool', lambda: nc.gpsimd.tensor_tensor(S[b][:, 1, 0:Tc], tq[2][:, 0:Tc], tq[3][:, 0:Tc], ALU.add), reads=[tqk[2], tqk[3]], writes=[f'sS{b}'])
                K.op('act', lambda: nc.scalar.copy(carry[:, st, :], S[b][:, :, Tc - 1]), reads=[f'sS{b}'], writes=[('scarry', st)])
                if not need_out:
                    continue
                first = (st % 4 == 0)
                last = (st % 4 == 3)
                for ri in range(2):
                    K.op('pe', lambda ri=ri: nc.tensor.matmul(py[:, kc, 0:Tc], Cm[:, c, ri, :], S[b][:, ri, 0:Tc], start=(first and ri == 0), stop=(last and ri == 1)),
                         reads=['sCm', f'sS{b}'], writes=['spy'])
            if not need_out:
                continue
            if d == 0:
                col0 = tl[0] * 128
                for ft in range(2):
                    K.op('dve', lambda ft=ft: nc.vector.scalar_tensor_tensor(yTf[:, ft, 0:Tc], uT[ui][:, ft, 0:Tc], dT[:, ft:ft + 1], py[:, ft, 0:Tc], ALU.mult, ALU.add),
                         reads=[f'suT{ui}', 'sdT', 'spy'], writes=['syTf'])
                K.dma('pool', D['ysT'][:, :, col0:col0 + Tc].rearrange("f p t -> p f t"), yTf[:, :, 0:Tc], reads=['syTf'], writes=[('ysT', col0)])
            else:
                nt_ = len(tl)
                K.op('act', lambda: nc.scalar.copy(yTf[:, :, 0:Tc], py[:, :, 0:Tc]), reads=['spy'], writes=['syTf'])
                pyt = puT[:].rearrange("p a t -> p (a t)").rearrange("p (s f) -> p s f", f=256)
                for j in range(nt_):
                    for ft in range(2):
                        K.op('pe', lambda j=j, ft=ft: nc.tensor.transpose(pyt[:, j, ft * 128:(ft + 1) * 128], yTf[:, ft, j * 128:(j + 1) * 128], ident[:]),
                             reads=['syTf', 'ident'], writes=['spuT'])
                K.op('act', lambda: nc.scalar.copy(ytok[:, 0:nt_, :], pyt[:, 0:nt_, :]), reads=['spuT'], writes=['sytok'])
                col0 = tl[-1] * 128
                K.dma('sp', yb[:, :, 0:Tc], D['ysT'][:, :, col0:col0 + Tc].rearrange("f p t -> p f t"), writes=['syb'])
                for j in range(nt_):
                    nj = nt_ - 1 - j
                    for ft in range(2):
                        K.op('pe', lambda j=j, nj=nj, ft=ft: nc.tensor.matmul(py[:, ft, nj * 128:(nj + 1) * 128], ytok[:, j, ft * 128:(ft + 1) * 128], jm[:], start=True, stop=True),
                             reads=['sytok', 'jm'], writes=['spy'])
                K.op('dve', lambda: nc.vector.tensor_tensor(yb[:, :, 0:Tc], yb[:, :, 0:Tc], py[:, :, 0:Tc], ALU.add), reads=['syb', 'spy'], writes=['syb'])
                if 'ydbg' in DEBUG_OUT:
                    K.dma('sp', D['ydbg'][:, :, col0:col0 + Tc].rearrange("f p t -> p f t"), yb[:, :, 0:Tc], reads=['syb'], writes=[('ydbg', col0)])
                gelu_tanh(K, nc, None, yb[:, :, 0:Tc], yb[:, :, 0:Tc], 'syb', 'syb', g1[:, :, 0:Tc], 'sg1')
                for j in range(nt_):
                    for ft in range(2):
                        K.op('pe', lambda j=j, ft=ft: nc.tensor.matmul(pz[:], yb[:, ft, j * 128:(j + 1) * 128], gw[:, ft, :], start=(ft == 0), stop=(ft == 1)),
                             reads=['syb', 'sgw'], writes=['spz'])
                    K.op('act', lambda: nc.scalar.activation(zs[:, 256:512], pz[:, 256:512], AF.Sigmoid), reads=['spz'], writes=['szs'])
                    K.op('dve', lambda: nc.vector.tensor_tensor(zo[:], pz[:, 0:256], zs[:, 256:512], ALU.mult), reads=['spz', 'szs'], writes=['szo'])
                    r0 = col0 + j * 128
                    K.dma('pool', D['ymix'][r0:r0 + 128, 0:256], zo[:], reads=['szo'], writes=[('ymix', 's', r0)])
        K.barrier()
    K.pop()


def stage_merge(K, nc, D, cfg, l, C, with_ctx):
    K.push()
    ident = C['ident']
    g1 = {'l': load_bc(K, D, 'l', 2)}
    if with_ctx:
        g1['c'] = load_bc(K, D, 'c', 2)
    wbr = K.sb('wbr', [128, 8, 1024], MM)
    wout = K.sb('wout', [128, 8, 1024], MM)
    K.push()
    mst = [K.sb(f'mst{i}', [128, 2, 1024]) for i in range(2)]
    srcs = [(wbr, 0, D['w_br_ssm'][l]), (wbr, 2, D['w_br_diff'][l][0:256]), (wbr, 4, D['w_br_diff'][l][256:512]), (wbr, 6, D['w_br_na'][l])] + \
           [(wout, 2 * j, D['w_out'][l][256 * j:256 * (j + 1)]) for j in range(4)]
    for si, (dst, k0, src) in enumerate(srcs):
        b_ = si % 2
        K.dma('sp', mst[b_][:], src.rearrange("(k p) n -> p k n", p=128), writes=[f'mst{b_}'])
        K.op('pool', lambda: nc.gpsimd.tensor_copy(dst[:, k0:k0 + 2, :], mst[b_][:]), reads=[f'mst{b_}'], writes=['wbr', 'wout'])
    K.pop()
    yt = [K.sb(f'myt{i}', [128, 1024]) for i in range(2)]
    gt = [K.sb(f'mgt{i}', [128, 3072]) for i in range(2)]
    xt = [K.sb(f'mxt{i}', [128, 1024]) for i in range(2)]
    yT = K.sb('myT', [128, 8, 128], MM)
    mm = K.sb('mmm', [128, 1024])
    mT = K.sb('mmT', [128, 8, 128], MM)
    tmp = K.sb('mtmp', [128, 512])
    xo = [K.sb(f'mxo{i}', [128, 1024]) for i in range(2)]
    ptr = K.ps('mptr', [128, 1024])
    pb = [K.ps(f'mpb{i}', [128, 512]) for i in range(2)]
    npb = 0
    tiles = list(range(cfg.TT)) if with_ctx else list(range(CT, cfg.TT))
    branches = [(0, [0, 1]), (1, [2, 3, 4, 5]), (2, [6, 7])]
    for tt in tiles:
        i = tt % 2
        w = 'c' if tt < CT else 'l'
        rows = slice(tt * 128, (tt + 1) * 128)
        K.dma('sp', yt[i][:], D['ymix'][rows, :], writes=[f'myt{i}'])
        K.dma('sp', gt[i][:], D['pl'][rows, 2560:5632], writes=[f'mgt{i}'])
        K.dma('sp', xt[i][:], xsrc(D, cfg, l, tt), writes=[f'mxt{i}'])
        K.op('act', lambda: nc.scalar.activation(gt[i][:], gt[i][:], AF.Sigmoid), reads=[f'mgt{i}'], writes=[f'mgt{i}'])
        for k in range(8):
            K.op('pe', lambda k=k: nc.tensor.transpose(ptr[:, k * 128:(k + 1) * 128], yt[i][:, k * 128:(k + 1) * 128], ident[:]),
                 reads=[f'myt{i}', 'ident'], writes=['mptr'])
        K.op('act', lambda: nc.scalar.copy(yT[:], ptr[:].rearrange("p (k t) -> p k t", k=8)), reads=['mptr'], writes=['myT'])
        for (br, ks) in branches:
            for hh in range(2):
                p = pb[npb % 2]
                pk = f'mpb{npb % 2}'
                npb += 1
                for ki, k in enumerate(ks):
                    K.op('pe', lambda k=k, ki=ki: nc.tensor.matmul(p[:], yT[:, k, :], wbr[:, k, hh * 512:(hh + 1) * 512], start=(ki == 0), stop=(ki == len(ks) - 1)),
                         reads=['myT', 'wbr'], writes=[pk])
                gsl = gt[i][:, br * 1024 + hh * 512: br * 1024 + (hh + 1) * 512]
                if br == 0:
                    K.op('dve', lambda: nc.vector.tensor_tensor(mm[:, hh * 512:(hh + 1) * 512], p[:], gsl, ALU.mult), reads=[pk, f'mgt{i}'], writes=[('mmm', hh)])
                else:
                    K.op('dve', lambda: nc.vector.tensor_tensor(tmp[:], p[:], gsl, ALU.mult), reads=[pk, f'mgt{i}'], writes=['mtmp'])
                    K.op('pool', lambda: nc.gpsimd.tensor_tensor(mm[:, hh * 512:(hh + 1) * 512], mm[:, hh * 512:(hh + 1) * 512], tmp[:], ALU.add),
                         reads=['mtmp', ('mmm', hh)], writes=[('mmm', hh)])
        for k in range(8):
            K.op('pe', lambda k=k: nc.tensor.transpose(ptr[:, k * 128:(k + 1) * 128], mm[:, k * 128:(k + 1) * 128], ident[:]),
                 reads=[('mmm', k // 4), 'ident'], writes=['mptr'])
        K.op('act', lambda: nc.scalar.copy(mT[:], ptr[:].rearrange("p (k t) -> p k t", k=8)), reads=['mptr'], writes=['mmT'])
        for hh in range(2):
            p = pb[npb % 2]
            pk = f'mpb{npb % 2}'
            npb += 1
            for k in range(8):
                K.op('pe', lambda k=k: nc.tensor.matmul(p[:], mT[:, k, :], wout[:, k, hh * 512:(hh + 1) * 512], start=(k == 0), stop=(k == 7)),
                     reads=['mmT', 'wout'], writes=[pk])
            K.op('dve', lambda: nc.vector.tensor_tensor(tmp[:], p[:], g1[w][:, hh * 512:(hh + 1) * 512], ALU.mult), reads=[pk, f'bc{w}2'], writes=['mtmp'])
            K.op('pool', lambda: nc.gpsimd.tensor_tensor(xo[i][:, hh * 512:(hh + 1) * 512], xt[i][:, hh * 512:(hh + 1) * 512], tmp[:], ALU.add),
                 reads=['mtmp', f'mxt{i}'], writes=[f'mxo{i}'])
        K.dma('pool', D['xres'][rows, :], xo[i][:], reads=[f'mxo{i}'], writes=[('xres', tt)])
    K.pop()


def stage_peer(K, nc, D, cfg, l, C, with_ctx, final):
    K.push()
    ident = C['ident']
    bc = {}
    for w in (['l', 'c'] if with_ctx else ['l']):
        for j in (3, 4, 5):
            bc[(w, j)] = load_bc(K, D, w, j)
    if final:
        fgr = K.sb('fgr', [1, 1024])
        K.dma('sp', fgr[:], D['final_g'], writes=['fgr'])
        fg = K.sb('fg', [128, 1024])
        K.push()
        with_ps = K.ps('fgps', [128, 512])
        for hh in range(2):
            bcast_row(K, nc, C, fgr[:, hh * 512:(hh + 1) * 512], 'fgr', fg[:, hh * 512:(hh + 1) * 512], 'fg', 512, with_ps, 'fgps')
        K.pop()
    wq = K.sb('wq', [128, 8, 2048])
    K.dma('sp', wq[:, :, 0:1024], D['peer_wq'][l, :, 0:1024].rearrange("(k p) n -> p k n", p=128), writes=['wq'])
    K.dma('sp', wq[:, :, 1024:2048], D['peer_wq'][l, :, 1024:2048].rearrange("(k p) n -> p k n", p=128), writes=['wq'])
    skT = K.sb('skT', [128, 16, 128])
    identR = K.sb('identR', [128, 128], MM)
    K.push()
    skr = K.sb('skr', [128, 16, 128])
    K.dma('sp', skr[:], D['peer_sk'][l].rearrange("c n d -> n c d"), writes=['skr'])
    pst = K.ps('pst', [128, 4, 128])
    K.op('dve', lambda: nc.vector.tensor_copy(identR[:], ident[:]), reads=['ident'], writes=['identR'])
    for c4 in range(4):
        for j in range(4):
            K.op('pe', lambda j=j: nc.tensor.transpose(pst[:, j, :], skr[:, c4 * 4 + j, :], ident[:]), reads=['skr', 'ident'], writes=['pst'])
        K.op('act', lambda: nc.scalar.copy(skT[:, c4 * 4:(c4 + 1) * 4, :], pst[:]), reads=['pst'], writes=['skT'])
    K.pop()
    iota16 = K.sb('iota16', [128, 16])
    K.dma('sp', iota16[:], D['iota16'], writes=['iota16'])
    xt = [K.sb(f'pxt{i}', [128, 1024]) for i in range(1)]
    h2 = K.sb('ph2', [128, 1024])
    tmp = K.sb('junkP', [128, 1024])
    scr = (K.sb('pss', [128, 1]), K.sb('prs', [128, 1]), tmp)
    ptr = K.ps('pptr', [128, 1024])
    h2T = K.sb('ph2T', [128, 8, 128])
    pq = [K.ps(f'ppq{i}', [128, 4, 128]) for i in range(2)]
    qT = K.sb('pqT', [128, 16, 128])
    psc = [K.ps(f'ppsc{i}', [128, 4, 128]) for i in range(1)]
    pacc = [K.ps(f'ppacc{i}', [128, 512]) for i in range(2)]
    vsc = [K.sb(f'pvsc{i}', [128, 1024], MM) for i in range(2)]
    s = K.sb('ps_s', [128, 16, 128])
    s2 = K.sb('ps_s2', [128, 16, 128])
    sv = K.sb('ps_sv', [128, 16, 16])
    si = K.sb('ps_si', [128, 16, 16], U32)
    sif = K.sb('ps_sif', [128, 16, 16])
    cand = s[:].rearrange("p a n -> p (a n)").rearrange("p (h c) -> p h c", c=256)
    cand2 = s2[:].rearrange("p a n -> p (a n)").rearrange("p (h c) -> p h c", c=256)
    best = K.sb('ps_best', [128, 8, 16])
    pos = K.sb('ps_pos', [128, 8, 16], U32)
    pa = K.sb('ps_pa', [128, 8, 16], U32)
    pbb = K.sb('ps_pb', [128, 8, 16], U32)
    paf = K.sb('ps_paf', [128, 8, 16])
    pbf = K.sb('ps_pbf', [128, 8, 16])
    oh = K.sb('ps_oh', [128, 8, 16, 16])
    ii = K.sb('ps_ii', [128, 8, 16])
    jj = K.sb('ps_jj', [128, 8, 16])
    eidx = K.sb('ps_eidx', [128, 128], I32)
    gg = K.sb('ps_g', [128, 8, 16])
    gs = K.sb('ps_gs', [128, 8])
    act = K.sb('ps_act', [128, 128])
    wgt = K.sb('ps_wgt', [128, 128])
    gtmp = K.sb('ps_gtmp', [128, 128])
    NG = 10
    gb = [K.sb(f'pgb{i}', [128, 2048], BF16) for i in range(NG)]
    xo = K.sb('pxo', [128, 1024])
    ng = 0
    nq_ = 0
    tiles = list(range(cfg.TT)) if with_ctx else list(range(CT, cfg.TT))
    split = final and PAIR_SPLIT
    if split:
        tiles = list(range(CT, CT + cfg.NT // 2))
        ridx = K.sb('ridx', [128, cfg.NT // 2], I32)
        K.dma('sp', ridx[:], D['rowidx'], writes=['ridx'])
    T1 = ['pk_all']
    for tt in tiles:
        i = 0
        w = 'c' if tt < CT else 'l'
        rows = slice(tt * 128, (tt + 1) * 128)
        if split:
            j_ = tt - CT
            K.dma('pool', None, None, reads=['ridx'], writes=[f'pxt{i}'], fn=lambda: nc.gpsimd.indirect_dma_start(
                out=xt[i][:], out_offset=None, in_=D['xres'], in_offset=bass.IndirectOffsetOnAxis(ap=ridx[:, j_:j_ + 1], axis=0)))
        else:
            K.dma('sp', xt[i][:], D['xres'][rows, :], writes=[f'pxt{i}'])
        norm_mod(K, nc, xt[i], f'pxt{i}', h2, 'ph2', bc[(w, 3)], f'bc{w}3', bc[(w, 4)], f'bc{w}4', scr, 'P')
        for k in range(8):
            K.op('pe', lambda k=k: nc.tensor.transpose(ptr[:, k * 128:(k + 1) * 128], h2[:, k * 128:(k + 1) * 128], ident[:]),
                 reads=['ph2', 'ident'], writes=['pptr'])
        K.op('act', lambda: nc.scalar.copy(h2T[:], ptr[:].rearrange("p (k t) -> p k t", k=8)), reads=['pptr'], writes=['ph2T'])
        for c4 in range(4):
            p = pq[nq_ % 2]
            pk = f'ppq{nq_ % 2}'
            nq_ += 1
            for j in range(4):
                hx = c4 * 4 + j
                for k in range(8):
                    K.op('pe', lambda j=j, k=k, hx=hx: nc.tensor.matmul(p[:, j, :], wq[:, k, hx * 128:(hx + 1) * 128], h2T[:, k, :], start=(k == 0), stop=(k == 7)),
                         reads=['wq', 'ph2T'], writes=[pk])
            K.op('act', lambda: nc.scalar.copy(qT[:, c4 * 4:(c4 + 1) * 4, :], p[:]), reads=[pk], writes=[('pqT', c4)])
        for c4 in range(4):
            p = psc[0]
            pk = 'ppsc0'
            for j in range(4):
                hx = c4 * 4 + j
                K.op('pe', lambda j=j, hx=hx: nc.tensor.matmul(p[:, j, :], qT[:, hx, :], skT[:, hx, :], start=True, stop=True),
                     reads=[('pqT', c4), 'skT'], writes=[pk])
            K.op('act', lambda: nc.scalar.copy(s[:, c4 * 4:(c4 + 1) * 4, :], p[:]), reads=[pk], writes=[('ps_s', c4)])
        for hx in range(16):
            sk_ = ('ps_s', hx // 4)
            K.op('dve', lambda: nc.vector.max(out=sv[:, hx, 0:8], in_=s[:, hx, :]), reads=[sk_], writes=T1)
            K.op('dve', lambda: nc.vector.max_index(out=si[:, hx, 0:8], in_max=sv[:, hx, 0:8], in_values=s[:, hx, :]), reads=[sk_] + T1, writes=T1)
            K.op('dve', lambda: nc.vector.match_replace(out=s2[:, hx, :], in_to_replace=sv[:, hx, 0:8], in_values=s[:, hx, :], imm_value=-1e30), reads=[sk_] + T1, writes=T1)
            K.op('dve', lambda: nc.vector.max(out=sv[:, hx, 8:16], in_=s2[:, hx, :]), reads=T1, writes=T1)
            K.op('dve', lambda: nc.vector.max_index(out=si[:, hx, 8:16], in_max=sv[:, hx, 8:16], in_values=s2[:, hx, :]), reads=T1, writes=T1)

        T2 = T1 + [('ps_s', c) for c in range(4)]

        def dv(fn):
            K.op('dve', fn, reads=T2, writes=T2)
        svv = sv[:].rearrange("p (h x) a -> p h x a", x=2)
        cv = cand.rearrange("p h (a b) -> p h a b", b=16)
        dv(lambda: nc.vector.tensor_tensor(cv, svv[:, :, 0, :].unsqueeze(3).to_broadcast([128, 8, 16, 16]),
                                           svv[:, :, 1, :].unsqueeze(2).to_broadcast([128, 8, 16, 16]), ALU.add))
        for h in range(8):
            dv(lambda: nc.vector.max(out=best[:, h, 0:8], in_=cand[:, h, :]))
            dv(lambda: nc.vector.max_index(out=pos[:, h, 0:8], in_max=best[:, h, 0:8], in_values=cand[:, h, :]))
            dv(lambda: nc.vector.match_replace(out=cand2[:, h, :], in_to_replace=best[:, h, 0:8], in_values=cand[:, h, :], imm_value=-1e30))
            dv(lambda: nc.vector.max(out=best[:, h, 8:16], in_=cand2[:, h, :]))
            dv(lambda: nc.vector.max_index(out=pos[:, h, 8:16], in_max=best[:, h, 8:16], in_values=cand2[:, h, :]))
        dv(lambda: nc.vector.tensor_tensor(gg[:], best[:], best[:, :, 0:1].to_broadcast([128, 8, 16]), ALU.subtract))
        K.op('act', lambda: nc.scalar.activation(gg[:], gg[:], AF.Exp), reads=T1, writes=T1)
        dv(lambda: nc.vector.reduce_sum(gs[:], gg[:], axis=AX.X))
        dv(lambda: nc.vector.reciprocal(gs[:], gs[:]))
        dv(lambda: nc.vector.tensor_tensor(gg[:], gg[:], gs[:].unsqueeze(2).to_broadcast([128, 8, 16]), ALU.mult))
        dv(lambda: nc.vector.tensor_scalar(pa[:], pos[:], 4, None, ALU.logical_shift_right))
        dv(lambda: nc.vector.tensor_scalar(pbb[:], pos[:], 15, None, ALU.bitwise_and))
        dv(lambda: nc.vector.tensor_copy(paf[:], pa[:]))
        dv(lambda: nc.vector.tensor_copy(pbf[:], pbb[:]))
        dv(lambda: nc.vector.tensor_copy(sif[:], si[:]))
        sfv = sif[:].rearrange("p (h x) a -> p h x a", x=2)
        io_b = iota16[:].unsqueeze(1).unsqueeze(1).to_broadcast([128, 8, 16, 16])
        for (pf, xsel, dst) in ((paf, 0, ii), (pbf, 1, jj)):
            dv(lambda: nc.vector.tensor_tensor(oh[:], pf[:].unsqueeze(3).to_broadcast([128, 8, 16, 16]), io_b, ALU.is_equal))
            dv(lambda: nc.vector.tensor_tensor(oh[:], oh[:], sfv[:, :, xsel, :].unsqueeze(2).to_broadcast([128, 8, 16, 16]), ALU.mult))
            dv(lambda: nc.vector.reduce_sum(dst[:], oh[:], axis=AX.X))
        dv(lambda: nc.vector.scalar_tensor_tensor(ii[:], ii[:], 128.0, jj[:], ALU.mult, ALU.add))
        dv(lambda: nc.vector.tensor_copy(eidx[:], ii[:].rearrange("p h k -> p (h k)")))
        GRP = 4
        ggf = gg[:].rearrange("p h k -> p (h k)")
        for g0 in range(0, 128, GRP):
            held = []
            for slot in range(g0, g0 + GRP):
                b_ = gb[ng % NG]
                bk = f'pgb{ng % NG}'
                ng += 1
                held.append((b_, bk))
                K.dma('pool', None, None, reads=T1, writes=[bk], fn=lambda: nc.gpsimd.indirect_dma_start(
                    out=b_[:], out_offset=None, in_=D[f'uv{l}'], in_offset=bass.IndirectOffsetOnAxis(ap=eidx[:, slot:slot + 1], axis=0)))
                K.op('dve', lambda: nc.vector.scalar_tensor_tensor(tmp[:], b_[:, 0:1024], 1.0, h2[:], ALU.mult, ALU.mult, accum_out=act[:, slot:slot + 1]),
                     reads=[bk, 'ph2'], writes=['junkP', 'ps_act'])
            sl = slice(g0, g0 + GRP)
            gelu_tanh(K, nc, None, wgt[:, sl], act[:, sl], 'ps_act', 'ps_wgt', gtmp[:, sl], 'ps_gtmp')
            K.op('dve', lambda: nc.vector.tensor_tensor(wgt[:, sl], wgt[:, sl], ggf[:, sl], ALU.mult), reads=['ps_wgt'] + T1, writes=['ps_wgt'])
            for si_, slot in enumerate(range(g0, g0 + GRP)):
                b_, bk = held[si_]
                vi = slot % 2
                K.op('act', lambda: nc.scalar.activation(vsc[vi][:], b_[:, 1024:2048], AF.Copy, scale=wgt[:, slot:slot + 1]), reads=[bk, 'ps_wgt'], writes=[f'pvsc{vi}'])
                for hh in range(2):
                    K.op('pe', lambda hh=hh: nc.tensor.matmul(pacc[hh][:], identR[:], vsc[vi][:, hh * 512:(hh + 1) * 512], start=(slot == 0), stop=(slot == 127)),
                         reads=['identR', f'pvsc{vi}'], writes=[f'ppacc{hh}'])
        for hh in range(2):
            K.op('dve', lambda hh=hh: nc.vector.tensor_tensor(tmp[:, hh * 512:(hh + 1) * 512], pacc[hh][:], bc[(w, 5)][:, hh * 512:(hh + 1) * 512], ALU.mult),
                 reads=[f'ppacc{hh}', f'bc{w}5'], writes=['junkP'])
        K.op('dve', lambda: nc.vector.tensor_tensor(xo[:], xt[i][:], tmp[:], ALU.add), reads=['junkP', f'pxt{i}'], writes=['pxo'])
        if not final:
            K.dma('sp', D['xres'][rows, :], xo[:], reads=['pxo'], writes=[('xres', tt)])
        else:
            ss, rs, junk = scr
            K.op('act', lambda: nc.scalar.activation(junk[:], xo[:], AF.Square, accum_out=ss[:]), reads=['pxo'], writes=['junkP', 'ssP'])
            K.op('dve', lambda: nc.vector.tensor_scalar(rs[:], ss[:], 1.0 / 1024.0, 1e-6, ALU.mult, ALU.add), reads=['ssP'], writes=['rsP'])
            K.op('act', lambda: nc.scalar.activation(rs[:], rs[:], AF.Sqrt), reads=['rsP'], writes=['rsP'])
            K.op('dve', lambda: nc.vector.reciprocal(rs[:], rs[:]), reads=['rsP'], writes=['rsP'])
            K.op('dve', lambda: nc.vector.scalar_tensor_tensor(tmp[:], xo[:], rs[:], fg[:], ALU.mult, ALU.mult), reads=['pxo', 'rsP', 'fg'], writes=['junkP'])
            K.dma('sp', D['out'][(tt - CT) * 128:(tt - CT + 1) * 128, :], tmp[:], reads=['junkP'], writes=[('out', tt)])
        K.maybe_barrier()
    K.pop()


def stage_tables(K, nc, D, cfg):
    K.push()
    src = [K.sb(f'tbs{i}', [128, 4, 1024]) for i in range(3)]
    dst = [K.sb(f'tbd{i}', [128, 4, 1024], BF16) for i in range(3)]
    n = 0
    for l in range(cfg.depth):
        for nm_s, nm_d, c0 in ((f'peer_u{l}', f'uv{l}', 0), (f'peer_v{l}', f'uv{l}', 1024)):
            for r0 in range(0, 16384, 512):
                i = n % 3
                n += 1
                K.dma('sp', src[i][:], D[nm_s][r0:r0 + 512, :].rearrange("(p j) c -> p j c", j=4), writes=[f'tbs{i}'])
                if i == 0:
                    K.op('dve', lambda: nc.vector.tensor_copy(dst[i][:], src[i][:]), reads=[f'tbs{i}'], writes=[f'tbd{i}'])
                elif i == 1:
                    K.op('pool', lambda: nc.gpsimd.tensor_copy(dst[i][:], src[i][:]), reads=[f'tbs{i}'], writes=[f'tbd{i}'])
                else:
                    K.op('act', lambda: nc.scalar.copy(dst[i][:], src[i][:]), reads=[f'tbs{i}'], writes=[f'tbd{i}'])
                K.dma('act', D[nm_d][r0:r0 + 512, c0:c0 + 1024].rearrange("(p j) c -> p j c", j=4), dst[i][:], reads=[f'tbd{i}'], writes=[(nm_d, r0, c0)])
    K.pop()


def tables_iter(K, nc, D, cfg, src, dst):
    n = 0
    for l in range(cfg.depth):
        for nm_s, nm_d, c0 in ((f'peer_u{l}', f'uv{l}', 0), (f'peer_v{l}', f'uv{l}', 1024)):
            for r0 in range(0, 16384, 512):
                i = n % len(src)
                n += 1
                K.dma('sp', src[i][:], D[nm_s][r0:r0 + 512, :].rearrange("(p j) c -> p j c", j=4), writes=[f'tbs{i}'])
                K.op('dve', lambda: nc.vector.tensor_copy(dst[i][:], src[i][:]), reads=[f'tbs{i}'], writes=[f'tbd{i}'])
                K.dma('pool', D[nm_d][r0:r0 + 512, c0:c0 + 1024].rearrange("(p j) c -> p j c", j=4), dst[i][:], reads=[f'tbd{i}'], writes=[(nm_d, r0, c0)])
                yield


ALL_STAGES = ('inproj', 'prepass', 'diff', 'na', 'ssm', 'merge', 'peer')
_NC_CACHE = {}


def kernel(**inputs):
    inp = {k: np.asarray(v) for k, v in inputs.items()}
    B, L, _ = inp['x'].shape
    depth = inp['ada_w'].shape[0]
    key = (L, depth)
    if key not in _NC_CACHE:
        _NC_CACHE[key] = build(L, depth, stages=ALL_STAGES)
    nc = _NC_CACHE[key]
    n_cores = 8
    if PAIR_SPLIT:
        in_maps = []
        for b in range(B):
            m0 = core_inputs(inp, b, L, depth, 0)
            m1 = dict(m0)
            m1['rowidx'] = core_inputs_rowidx(L, 1)
            in_maps += [m0, m1]
        res = run_bass_kernel_spmd(nc, in_maps, core_ids=list(range(n_cores)))
        out = np.stack([np.concatenate([res.results[2 * b]['out'], res.results[2 * b + 1]['out']], 0) for b in range(B)], 0)
    else:
        maps = [core_inputs(inp, b, L, depth) for b in range(B)]
        in_maps = [maps[i % B] for i in range(n_cores)]
        res = run_bass_kernel_spmd(nc, in_maps, core_ids=list(range(n_cores)))
        out = np.stack([res.results[b]['out'] for b in range(B)], 0)
    return out.astype(np.float32)


def core_inputs_rowidx(L, half):
    nt2 = (L // 128) // 2
    return np.ascontiguousarray((CT * 128 + half * (L // 2) + np.arange(nt2)[None, :] * 128 + np.arange(128)[:, None]).astype(np.int32))
```

```python
import numpy as np
from contextlib import ExitStack
import concourse.bass as bass
import concourse.mybir as mybir
from concourse.bass_utils import run_bass_kernel_spmd

F32 = mybir.dt.float32
F32R = mybir.dt.float32r
BF16 = mybir.dt.bfloat16
QK_BF16 = True
AV_BF16 = True
BF_TABLES = True
TABLES_OVERLAP = True
USE_R = True
MM = F32R if USE_R else F32
U32 = mybir.dt.uint32
I32 = mybir.dt.int32
ALU = mybir.AluOpType
AF = mybir.ActivationFunctionType
AX = mybir.AxisListType

NSLOT = 12
RELAX_SAME = set()
PAIR_SPLIT = True
BAR_LIM = 12000
DEBUG_OUT = set()


class Sched:
    def __init__(self, nc, es):
        self.nc = nc
        self.es = es
        self.eng = {'pe': nc.tensor, 'dve': nc.vector, 'act': nc.scalar, 'pool': nc.gpsimd, 'sp': nc.sync}
        self.sem = {e: es.enter_context(nc.semaphore(f"s_{e}")) for e in self.eng}
        self.cnt = {e: 0 for e in self.eng}
        self.waited = {e: {} for e in self.eng}
        self.dq = {}
        for q in ('sp', 'pool', 'act'):
            self.dq[q] = dict(sems=[es.enter_context(nc.semaphore(f"d_{q}{i}")) for i in range(NSLOT)],
                              uses=[0] * NSLOT, nxt=0)
        self.lastw = {}
        self.readers = {}
        self.scopes = []
        self.nalloc = 0
        self.sem_arrive = es.enter_context(nc.semaphore("s_arrive"))
        self.sem_epoch = es.enter_context(nc.semaphore("s_epoch"))
        self.epoch = 0

    def push(self):
        s = ExitStack()
        self.scopes.append(s)

    def pop(self):
        self.barrier()
        self.scopes.pop().close()

    def _ctx(self):
        return self.scopes[-1] if self.scopes else self.es

    def sb(self, name, shape, dtype=F32):
        self.nalloc += 1
        return self._ctx().enter_context(self.nc.sbuf_tensor(f"{name}_{self.nalloc}", list(shape), dtype))

    def ps(self, name, shape, dtype=F32):
        self.nalloc += 1
        return self._ctx().enter_context(self.nc.psum_tensor(f"{name}_{self.nalloc}", list(shape), dtype))

    def _semof(self, key):
        if isinstance(key, str):
            return self.sem[key]
        return self.dq[key[1]]['sems'][key[2]]

    def _wait(self, e, tok):
        key, val = tok
        if key == e and (e in ('pe', 'sp') or e in RELAX_SAME):
            return
        if self.waited[e].get(key, 0) >= val:
            return
        self.eng[e].wait_ge(self._semof(key), val)
        self.waited[e][key] = val

    def _deps(self, e, reads, writes):
        for r in reads:
            if r in self.lastw:
                self._wait(e, self.lastw[r])
        for w in writes:
            if w in self.lastw:
                self._wait(e, self.lastw[w])
            for k, v in self.readers.get(w, {}).items():
                self._wait(e, (k, v))

    def _commit(self, tok, reads, writes):
        for r in reads:
            d = self.readers.setdefault(r, {})
            if d.get(tok[0], 0) < tok[1]:
                d[tok[0]] = tok[1]
        for w in writes:
            self.lastw[w] = tok
            self.readers[w] = {}

    def op(self, e, fn, reads=(), writes=()):
        self._deps(e, reads, writes)
        ins = fn()
        ins.then_inc(self.sem[e], 1)
        self.cnt[e] += 1
        self._commit((e, self.cnt[e]), reads, writes)

    def dma(self, q, out, in_, reads=(), writes=(), fn=None):
        d = self.dq[q]
        self._deps(q, reads, writes)
        s = d['nxt']
        d['nxt'] = (s + 1) % NSLOT
        if d['uses'][s] > 0:
            self._wait(q, (('dma', q, s), 16 * d['uses'][s]))
        if fn is None:
            ins = self.eng[q].dma_start(out=out, in_=in_)
        else:
            ins = fn()
        d['uses'][s] += 1
        ins.then_inc(d['sems'][s], 16)
        self._commit((('dma', q, s), 16 * d['uses'][s]), reads, writes)

    def _all_tokens(self):
        toks = [(e, c) for e, c in self.cnt.items() if c > 0]
        for q, d in self.dq.items():
            for s in range(NSLOT):
                if d['uses'][s] > 0:
                    toks.append((('dma', q, s), 16 * d['uses'][s]))
        return toks

    def barrier(self, reset=True):
        toks = self._all_tokens()
        if not toks:
            return
        for e in self.eng:
            for t in toks:
                if t[0] != e:
                    self._wait(e, t)
        if not reset:
            return
        for e in self.eng:
            if self.cnt[e] > BAR_LIM:
                self.epoch += 1
                self.sem[e] = self.es.enter_context(self.nc.semaphore(f"s_{e}_{self.epoch}"))
                self.cnt[e] = 0
                for e2 in self.eng:
                    self.waited[e2].pop(e, None)
                for k in list(self.lastw.keys()):
                    if self.lastw[k][0] == e:
                        del self.lastw[k]
                for k, d in self.readers.items():
                    d.pop(e, None)

    def maybe_barrier(self, lim=None):
        if max(self.cnt.values()) > (BAR_LIM if lim is None else lim):
            self.barrier()

    def finish(self):
        self.barrier(reset=False)


D_MODEL = 1024
IN_W = 5632
CTXL = 256
CT = 2
LAM_INIT = [0.8 - 0.6 * float(np.exp(-0.3 * l)) for l in range(8)]


class Cfg:
    def __init__(self, L, depth):
        self.L = L
        self.depth = depth
        self.NT = L // 128
        self.TT = self.NT + CT
        self.R = L // 64


def declare_io(nc, cfg):
    D = {}
    dp = cfg.depth

    def din(name, shape, dt=F32):
        D[name] = nc.dram_tensor(name, list(shape), dt, kind="ExternalInput").ap()

    def dsc(name, shape, dt=F32):
        kind = "ExternalOutput" if name in DEBUG_OUT else "Internal"
        D[name] = nc.dram_tensor(name, list(shape), dt, kind=kind).ap()

    din('x', [cfg.L, 1024]); din('ctx', [CTXL, 1024]); din('cc', [2, 1024])
    din('ada_w', [dp, 1024, 6144]); din('ada_b', [dp, 6144])
    din('norm_g', [dp, 2048])
    din('w_in', [dp, 1024, IN_W])
    din('ident', [128, 128]); din('sel', [2, 256])
    D['out'] = nc.dram_tensor('out', [cfg.L // 2 if PAIR_SPLIT else cfg.L, 1024], F32, kind="ExternalOutput").ap()
    if PAIR_SPLIT:
        din('rowidx', [128, cfg.NT // 2], I32)
    dsc('xres', [cfg.TT * 128, 1024])
    dsc('pl', [cfg.TT * 128, IN_W])
    dsc('qkT', [8, 128, cfg.TT * 128]); dsc('nqkT', [4, 128, cfg.TT * 128])
    dsc('ymix', [cfg.TT * 128, 1024])
    ntypes = len(na_pair_info(cfg.R)[1])
    din('rope', [cfg.L, 64]); din('nab', [dp, 4, ntypes * 5, 128, 128])
    din('diff_lambda', [dp, 256]); din('diff_subln_g', [dp, 128])
    din('jmat', [128, 128]); din('ssm_sc', [dp, 128, 48]); din('ssm_b', [dp, 2, 16, 128, 128]); din('ssm_c', [dp, 2, 16, 128, 128])
    din('ssm_dT', [dp, 128, 2]); din('ssm_glu_w', [dp, 256, 512])
    dsc('ysT', [2, 128, cfg.TT * 128])
    dsc('bcd', [2, 6, 128, 1024])
    if BF_TABLES:
        for i in range(dp):
            dsc(f'uv{i}', [16384, 2048], BF16)
    din('w_br_ssm', [dp, 256, 1024]); din('w_br_diff', [dp, 512, 1024]); din('w_br_na', [dp, 256, 1024]); din('w_out', [dp, 1024, 1024])
    din('peer_wq', [dp, 1024, 2048]); din('peer_sk', [dp, 16, 128, 128]); [din(f'peer_u{i}', [16384, 1024]) for i in range(dp)]; [din(f'peer_v{i}', [16384, 1024]) for i in range(dp)]
    din('final_g', [1, 1024]); din('iota16', [128, 16])
    if 'ydbg' in DEBUG_OUT:
        dsc('ydbg', [2, 128, cfg.TT * 128])
    return D


def xsrc(D, cfg, l, tt):
    if l == 0:
        if tt < CT:
            return D['ctx'][tt * 128:(tt + 1) * 128, :]
        return D['x'][(tt - CT) * 128:(tt - CT + 1) * 128, :]
    return D['xres'][tt * 128:(tt + 1) * 128, :]


def stage_consts(K, nc, D):
    ident = K.sb('ident', [128, 128])
    K.dma('sp', ident[:], D['ident'], writes=['ident'])
    sel = K.sb('sel', [2, 256])
    K.dma('sp', sel[:], D['sel'], writes=['sel'])
    return dict(ident=ident, sel=sel)


def stage_mods(K, nc, D, cfg, l, C, with_ctx):
    bc = {}
    who = ['l', 'c'] if with_ctx else ['l']
    K.push()
    for w in ['l', 'c']:
        for j in ([0, 1, 2, 3, 4, 5] if w in who else [0, 1]):
            bc[(w, j)] = K.sb(f'bc{w}{j}', [128, 1024])
    ident, sel = C['ident'], C['sel']
    cc = K.sb('cc', [2, 1024])
    K.dma('sp', cc[:], D['cc'], writes=['cc'])
    K.op('act', lambda: nc.scalar.activation(cc[:], cc[:], AF.Silu), reads=['cc'], writes=['cc'])
    pT = K.ps('pT', [128, 8, 2])
    for k in range(8):
        K.op('pe', lambda k=k: nc.tensor.transpose(pT[:, k, :], cc[:, k * 128:(k + 1) * 128], ident[0:2, 0:2]),
             reads=['cc', 'ident'], writes=['pT'])
    cT = K.sb('cT', [128, 8, 2])
    K.op('dve', lambda: nc.vector.tensor_copy(cT[:], pT[:]), reads=['pT'], writes=['cT'])
    mods = K.sb('mods', [2, 6144])
    ab = K.sb('ab', [2, 6144])
    K.dma('sp', ab[0:1, :], D['ada_b'][l:l + 1, :], writes=['ab'])
    K.dma('sp', ab[1:2, :], D['ada_b'][l:l + 1, :], writes=['ab'])
    gv = K.sb('gv', [1, 2048])
    K.dma('sp', gv[:], D['norm_g'][l:l + 1, :], writes=['gv'])
    wbuf = [K.sb(f'adaw{i}', [128, 8, 512]) for i in range(2)]
    pm = [K.ps(f'pm{i}', [2, 512]) for i in range(2)]
    for cch in range(12):
        wb = wbuf[cch % 2]
        wk = f'adaw{cch % 2}'
        K.dma('sp', wb[:], D['ada_w'][l, :, cch * 512:(cch + 1) * 512].rearrange("(k p) n -> p k n", p=128), writes=[wk])
        pk = f'pm{cch % 2}'
        for k in range(8):
            K.op('pe', lambda k=k, wb=wb, p=pm[cch % 2]: nc.tensor.matmul(p[:], cT[:, k, :], wb[:, k, :], start=(k == 0), stop=(k == 7)),
                 reads=['cT', wk], writes=[pk])
        K.op('dve', lambda p=pm[cch % 2], cch=cch: nc.vector.tensor_tensor(mods[:, cch * 512:(cch + 1) * 512], p[:], ab[:, cch * 512:(cch + 1) * 512], ALU.add),
             reads=[pk, 'ab'], writes=['mods'])
    for j in (1, 4):
        K.op('dve', lambda j=j: nc.vector.tensor_scalar(mods[:, j * 1024:(j + 1) * 1024], mods[:, j * 1024:(j + 1) * 1024], 1.0, None, ALU.add),
             reads=['mods'], writes=['mods'])
    pb = [K.ps(f'pb{i}', [128, 512]) for i in range(2)]
    gbc = K.sb('gbc', [128, 2048])
    n = 0
    for q in range(4):
        K.op('pe', lambda q=q, p=pb[n % 2]: nc.tensor.matmul(p[:], sel[0:1, 0:128], gv[:, q * 512:(q + 1) * 512], start=True, stop=True),
             reads=['sel', 'gv'], writes=[f'pb{n % 2}'])
        K.op('act', lambda q=q, p=pb[n % 2]: nc.scalar.copy(gbc[:, q * 512:(q + 1) * 512], p[:]), reads=[f'pb{n % 2}'], writes=['gbc'])
        n += 1
    for wi, w in enumerate(['l', 'c']):
        for mj in range(6):
            tj = {0: 1, 1: 0, 2: 2, 3: 4, 4: 3, 5: 5}[mj]
            if (w, tj) not in bc:
                continue
            for hh in range(2):
                K.op('pe', lambda p=pb[n % 2], wi=wi, mj=mj, hh=hh: nc.tensor.matmul(
                    p[:], sel[0:2, wi * 128:(wi + 1) * 128], mods[:, mj * 1024 + hh * 512: mj * 1024 + (hh + 1) * 512], start=True, stop=True),
                    reads=['sel', 'mods'], writes=[f'pb{n % 2}'])
                dst = bc[(w, tj)][:, hh * 512:(hh + 1) * 512]
                if tj in (0, 3):
                    goff = 0 if tj == 0 else 1024
                    K.op('dve', lambda p=pb[n % 2], dst=dst, goff=goff, hh=hh: nc.vector.tensor_tensor(
                        dst, p[:], gbc[:, goff + hh * 512: goff + (hh + 1) * 512], ALU.mult),
                        reads=[f'pb{n % 2}', 'gbc'], writes=[f'bc{w}{tj}'])
                else:
                    K.op('act', lambda p=pb[n % 2], dst=dst: nc.scalar.copy(dst, p[:]), reads=[f'pb{n % 2}'], writes=[f'bc{w}{tj}'])
                n += 1
    for (w, j), t in bc.items():
        K.dma('sp', D['bcd'][0 if w == 'l' else 1, j], t[:], reads=[f'bc{w}{j}'], writes=[('bcd', w, j)])
    K.pop()
    return None


def load_bc(K, D, w, j):
    t = K.sb(f'bc{w}{j}', [128, 1024])
    K.dma('sp', t[:], D['bcd'][0 if w == 'l' else 1, j], writes=[f'bc{w}{j}'])
    return t


def norm_mod(K, nc, xt, xkey, ht, hkey, gm, gmkey, sh, shkey, scr, tag):
    ss, rs, junk = scr
    K.op('act', lambda: nc.scalar.activation(junk[:], xt[:], AF.Square, accum_out=ss[:]), reads=[xkey], writes=[f'junk{tag}', f'ss{tag}'])
    K.op('dve', lambda: nc.vector.tensor_scalar(rs[:], ss[:], 1.0 / 1024.0, 1e-6, ALU.mult, ALU.add), reads=[f'ss{tag}'], writes=[f'rs{tag}'])
    K.op('act', lambda: nc.scalar.activation(rs[:], rs[:], AF.Sqrt), reads=[f'rs{tag}'], writes=[f'rs{tag}'])
    K.op('dve', lambda: nc.vector.reciprocal(rs[:], rs[:]), reads=[f'rs{tag}'], writes=[f'rs{tag}'])
    K.op('dve', lambda: nc.vector.scalar_tensor_tensor(ht[:], xt[:], rs[:], gm[:], ALU.mult, ALU.mult),
         reads=[xkey, f'rs{tag}', gmkey], writes=[hkey])
    K.op('dve', lambda: nc.vector.tensor_tensor(ht[:], ht[:], sh[:], ALU.add), reads=[hkey, shkey], writes=[hkey])


def stage_inproj(K, nc, D, cfg, l, C, bc, with_ctx):
    K.push()
    ident = C['ident']
    bc = {(w, j): load_bc(K, D, w, j) for w in 'lc' for j in (0, 1)}
    NB = 11
    tiles = list(range(cfg.TT))
    hT = K.sb('hT', [128, NB, 8, 128], MM)
    wst = [K.sb(f'wst{i}', [128, 8, 512]) for i in range(2)]
    xt = [K.sb(f'xt{i}', [128, 1024]) for i in range(2)]
    ht = [K.sb(f'ht{i}', [128, 1024]) for i in range(2)]
    scr = [(K.sb(f'ss{i}', [128, 1]), K.sb(f'rs{i}', [128, 1]), K.sb(f'junk{i}', [128, 1024])) for i in range(2)]
    ptr = [K.ps(f'ptr{i}', [128, 1024]) for i in range(2)]
    wb = [K.sb(f'win{i}', [128, 8, 512], MM) for i in range(2)]
    po = [K.ps(f'po{i}', [128, 512]) for i in range(2)]
    ot = [K.sb(f'ot{i}', [128, 512]) for i in range(3)]
    nw = 0
    no = 0
    tit = None
    if l == 0 and BF_TABLES and TABLES_OVERLAP and 'peer_u0' in D:
        tsrc = [K.sb(f'tbs{i}', [128, 4, 1024]) for i in range(2)]
        tdst = [K.sb(f'tbd{i}', [128, 4, 1024], BF16) for i in range(2)]
        tit = tables_iter(K, nc, D, cfg, tsrc, tdst)
    for b0 in range(0, len(tiles), NB):
        blk = tiles[b0:b0 + NB]
        for bi, tt in enumerate(blk):
            i = tt % 2
            K.dma('sp', xt[i][:], xsrc(D, cfg, l, tt), writes=[f'xt{i}'])
            w = 'c' if tt < CT else 'l'
            norm_mod(K, nc, xt[i], f'xt{i}', ht[i], f'ht{i}', bc[(w, 0)], f'bc{w}0', bc[(w, 1)], f'bc{w}1', scr[i], i)
            for k in range(8):
                K.op('pe', lambda k=k, i=i: nc.tensor.transpose(ptr[i][:, k * 128:(k + 1) * 128], ht[i][:, k * 128:(k + 1) * 128], ident[:]),
                     reads=[f'ht{i}', 'ident'], writes=[f'ptr{i}'])
            K.op('act', lambda i=i, bi=bi: nc.scalar.copy(hT[:, bi, :, :], ptr[i][:].rearrange("p (k t) -> p k t", k=8)),
                 reads=[f'ptr{i}'], writes=[('hT', bi)])
        for cch in range(IN_W // 512):
            wi = nw % 2
            nw += 1
            K.dma('sp', wst[wi][:], D['w_in'][l, :, cch * 512:(cch + 1) * 512].rearrange("(k p) n -> p k n", p=128), writes=[f'wst{wi}'])
            K.op('pool', lambda wi=wi: nc.gpsimd.tensor_copy(wb[wi][:], wst[wi][:]), reads=[f'wst{wi}'], writes=[f'win{wi}'])
            for bi, tt in enumerate(blk):
                pi = no % 2
                oi = no % 3
                no += 1
                for k in range(8):
                    K.op('pe', lambda k=k, bi=bi, pi=pi, wi=wi: nc.tensor.matmul(po[pi][:], hT[:, bi, k, :], wb[wi][:, k, :], start=(k == 0), stop=(k == 7)),
                         reads=[('hT', bi), f'win{wi}'], writes=[f'po{pi}'])
                K.op('act', lambda pi=pi, oi=oi: nc.scalar.copy(ot[oi][:], po[pi][:]), reads=[f'po{pi}'], writes=[f'ot{oi}'])
                K.dma('pool', D['pl'][tt * 128:(tt + 1) * 128, cch * 512:(cch + 1) * 512], ot[oi][:], reads=[f'ot{oi}'], writes=[('pl', tt, cch)])
                if tit is not None:
                    next(tit, None)
    if tit is not None:
        for _ in tit:
            pass
    K.pop()


def build(L, depth, stages=('mods', 'inproj'), nlayers=None):
    cfg = Cfg(L, depth)
    nc = bass.Bass("TRN2", target_bir_lowering=False)
    D = declare_io(nc, cfg)
    with ExitStack() as es:
        K = Sched(nc, es)
        C = stage_consts(K, nc, D)
        if BF_TABLES and 'peer' in stages and not TABLES_OVERLAP:
            stage_tables(K, nc, D, cfg)
        for l in range(depth if nlayers is None else nlayers):
            with_ctx = l < depth - 1
            K.push()
            bc = stage_mods(K, nc, D, cfg, l, C, with_ctx)
            if 'inproj' in stages:
                stage_inproj(K, nc, D, cfg, l, C, bc, with_ctx)
            if 'prepass' in stages:
                stage_prepass(K, nc, D, cfg, l, C, with_ctx)
            if 'diff' in stages:
                stage_diff(K, nc, D, cfg, l, C, with_ctx)
            if 'na' in stages:
                stage_na(K, nc, D, cfg, l, C, with_ctx)
            if 'ssm' in stages:
                stage_ssm(K, nc, D, cfg, l, C, with_ctx)
            if 'merge' in stages:
                stage_merge(K, nc, D, cfg, l, C, with_ctx)
            if 'peer' in stages:
                stage_peer(K, nc, D, cfg, l, C, with_ctx, final=(l == depth - 1))
            K.pop()
        K.finish()
    return nc


def na_pair_info(R):
    types = {}
    pairs = []
    for r in range(0, R, 2):
        base = int(np.clip(r - 4, 0, R - 10))
        ws0 = int(np.clip(r - 4, 0, R - 8))
        ws1 = int(np.clip(r + 1 - 4, 0, R - 8))
        key = (r - base, ws0 - base, ws1 - base)
        if key not in types:
            types[key] = len(types)
        pairs.append((r, base, types[key]))
    return pairs, list(types.keys())


def host_nab(rpb, R):
    pairs, types = na_pair_info(R)
    H = rpb.shape[0]
    cols = np.arange(64)
    col_start = np.clip(cols - 8, 0, 48)
    col_ok = (cols[None, :] >= col_start[:, None]) & (cols[None, :] < col_start[:, None] + 16)
    dc = np.clip(cols[None, :] - cols[:, None] + 15, 0, 30)
    out = np.full((H, len(types) * 5, 128, 128), -30000.0, np.float32)
    for ti, (dr0, w0, w1) in enumerate(types):
        for j in range(5):
            for kk in range(2):
                krel = 2 * j + kk
                for qq in range(2):
                    ws = (w0, w1)[qq]
                    if not (ws <= krel < ws + 8):
                        continue
                    dr = krel - (dr0 + qq) + 7
                    blk = np.where(col_ok, rpb[:, dr][:, dc], np.float32(-30000.0))
                    out[:, ti * 5 + j, kk * 64:(kk + 1) * 64, qq * 64:(qq + 1) * 64] = np.transpose(blk, (0, 2, 1))
    return out


def host_rope(L):
    t = np.arange(L)
    freqs = (10000.0 ** (-np.arange(16, dtype=np.float32) / 16)).astype(np.float32)
    ang_r = (t // 64).astype(np.float32)[:, None] * freqs
    ang_c = (t % 64).astype(np.float32)[:, None] * freqs
    ang = np.concatenate([ang_r, ang_c], 1).astype(np.float32)
    return np.concatenate([np.cos(ang), np.sin(ang)], 1).astype(np.float32)


def stage_prepass(K, nc, D, cfg, l, C, with_ctx):
    K.push()
    ident = C['ident']
    qk = [K.sb(f'qk{i}', [128, 1024]) for i in range(2)]
    qr = [K.sb(f'qr{i}', [128, 1024]) for i in range(2)]
    cs = [K.sb(f'cs{i}', [128, 64]) for i in range(2)]
    tmp = [K.sb(f'rt{i}', [128, 512]) for i in range(4)]
    nqk = [K.sb(f'nqk{i}', [128, 512]) for i in range(2)]
    pq = [K.ps(f'pq{i}', [128, 1024]) for i in range(2)]
    pn = [K.ps(f'pn{i}', [128, 512]) for i in range(2)]
    sq = [K.sb(f'sq{i}', [128, 1024]) for i in range(2)]
    sn = [K.sb(f'sn{i}', [128, 512]) for i in range(2)]
    for tt in range(cfg.TT):
        i = tt % 2
        rows = slice(tt * 128, (tt + 1) * 128)
        K.dma('sp', qk[i][:], D['pl'][rows, 256:1280], writes=[f'qk{i}'])
        K.dma('sp', nqk[i][:], D['pl'][rows, 1792:2304], writes=[f'nqk{i}'])
        src = qk[i]
        skey = f'qk{i}'
        if tt >= CT:
            K.dma('sp', cs[i][:], D['rope'][(tt - CT) * 128:(tt - CT + 1) * 128, :], writes=[f'cs{i}'])
            v = qk[i][:].rearrange("p (g rc x f) -> p g rc x f", g=16, rc=2, x=2, f=16)
            o = qr[i][:].rearrange("p (g rc x f) -> p g rc x f", g=16, rc=2, x=2, f=16)
            x1, x2 = v[:, :, :, 0, :], v[:, :, :, 1, :]
            cosb = cs[i][:, 0:32].rearrange("p (rc f) -> p rc f", rc=2).unsqueeze(1).to_broadcast([128, 16, 2, 16])
            sinb = cs[i][:, 32:64].rearrange("p (rc f) -> p rc f", rc=2).unsqueeze(1).to_broadcast([128, 16, 2, 16])
            tv = [t[:].rearrange("p (g rc f) -> p g rc f", g=16, rc=2, f=16) for t in tmp]
            rk = [skey, f'cs{i}']
            K.op('dve', lambda: nc.vector.tensor_tensor(tv[0], x1, cosb, ALU.mult), reads=rk, writes=['rt0'])
            K.op('dve', lambda: nc.vector.tensor_tensor(tv[1], x2, sinb, ALU.mult), reads=rk, writes=['rt1'])
            K.op('pool', lambda: nc.gpsimd.tensor_tensor(tv[2], x2, cosb, ALU.mult), reads=rk, writes=['rt2'])
            K.op('pool', lambda: nc.gpsimd.tensor_tensor(tv[3], x1, sinb, ALU.mult), reads=rk, writes=['rt3'])
            K.op('dve', lambda: nc.vector.tensor_tensor(o[:, :, :, 0, :], tv[0], tv[1], ALU.subtract), reads=['rt0', 'rt1'], writes=[f'qr{i}'])
            K.op('pool', lambda: nc.gpsimd.tensor_tensor(o[:, :, :, 1, :], tv[2], tv[3], ALU.add), reads=['rt2', 'rt3'], writes=[f'qr{i}'])
            src = qr[i]
            skey = f'qr{i}'
        for k in range(8):
            K.op('pe', lambda k=k, src=src: nc.tensor.transpose(pq[i][:, k * 128:(k + 1) * 128], src[:, k * 128:(k + 1) * 128], ident[:]),
                 reads=[skey, 'ident'], writes=[f'pq{i}'])
        K.op('act', lambda: nc.scalar.copy(sq[i][:], pq[i][:]), reads=[f'pq{i}'], writes=[f'sq{i}'])
        K.dma('pool', D['qkT'][:, :, rows].rearrange("b p t -> p b t"), sq[i][:].rearrange("p (b t) -> p b t", b=8),
              reads=[f'sq{i}'], writes=[('qkT', tt)])
        for k in range(4):
            K.op('pe', lambda k=k: nc.tensor.transpose(pn[i][:, k * 128:(k + 1) * 128], nqk[i][:, k * 128:(k + 1) * 128], ident[:]),
                 reads=[f'nqk{i}', 'ident'], writes=[f'pn{i}'])
        K.op('act', lambda: nc.scalar.copy(sn[i][:], pn[i][:]), reads=[f'pn{i}'], writes=[f'sn{i}'])
        K.dma('pool', D['nqkT'][:, :, rows].rearrange("b p t -> p b t"), sn[i][:].rearrange("p (b t) -> p b t", b=4),
              reads=[f'sn{i}'], writes=[('nqkT', tt)])
    K.pop()


def bcast_row(K, nc, C, row, rowkey, dst, dstkey, ncols, ps, pskey, scale=None):
    sel = C['sel']
    K.op('pe', lambda: nc.tensor.matmul(ps[:, 0:ncols], sel[0:1, 0:128], row, start=True, stop=True), reads=['sel', rowkey], writes=[pskey])
    if scale is None:
        K.op('dve', lambda: nc.vector.tensor_copy(dst, ps[:, 0:ncols]), reads=[pskey], writes=[dstkey])
    else:
        K.op('dve', lambda: nc.vector.tensor_scalar(dst, ps[:, 0:ncols], float(scale), None, ALU.mult), reads=[pskey], writes=[dstkey])


def stage_diff(K, nc, D, cfg, l, C, with_ctx):
    K.push()
    TT, NT = cfg.TT, cfg.NT
    NTOK = TT * 128
    lam_init = LAM_INIT[l]
    dl = K.sb('dl', [1, 256])
    K.dma('sp', dl[:], D['diff_lambda'][l:l + 1, :], writes=['dl'])
    pr = K.sb('pr', [1, 2, 64])
    dv = dl[:].rearrange("o (a b f) -> o a b f", a=2, b=2, f=64)
    K.op('dve', lambda: nc.vector.tensor_tensor(pr[:], dv[:, :, 0, :], dv[:, :, 1, :], ALU.mult), reads=['dl'], writes=['pr'])
    e2 = K.sb('e2', [1, 2])
    K.op('dve', lambda: nc.vector.reduce_sum(e2[:], pr[:], axis=AX.X), reads=['pr'], writes=['e2'])
    K.op('act', lambda: nc.scalar.activation(e2[:], e2[:], AF.Exp), reads=['e2'], writes=['e2'])
    nl = K.sb('nl', [1, 1])
    K.op('dve', lambda: nc.vector.tensor_tensor(nl[:], e2[:, 1:2], e2[:, 0:1], ALU.subtract), reads=['e2'], writes=['nl'])
    K.op('dve', lambda: nc.vector.tensor_scalar(nl[:], nl[:], -lam_init, None, ALU.add), reads=['nl'], writes=['nl'])
    psm = K.ps('psm', [128, 512])
    neglam = K.sb('neglam', [128, 1])
    bcast_row(K, nc, C, nl[:], 'nl', neglam[:], 'neglam', 1, psm, 'psm')
    sg = K.sb('sg', [1, 128])
    K.dma('sp', sg[:], D['diff_subln_g'][l:l + 1, :], writes=['sg'])
    gbc = K.sb('dgbc', [128, 128])
    bcast_row(K, nc, C, sg[:], 'sg', gbc[:], 'dgbc', 128, psm, 'psm', scale=1.0 - lam_init)
    qT = K.sb('dqT', [128, NTOK], BF16 if QK_BF16 else MM)
    kT = K.sb('dkT', [128, NTOK], BF16 if QK_BF16 else MM)
    va = K.sb('dva', [128, TT, 130], BF16 if AV_BF16 else MM)
    stg = [K.sb(f'dstg{i}', [128, 768]) for i in range(2)]
    nstg = [0]
    onez = K.sb('onez', [128, TT, 2])
    K.op('pool', lambda: nc.gpsimd.memset(onez[:, :, 0:1], 1.0), writes=['onez'])
    K.op('pool', lambda: nc.gpsimd.memset(onez[:, :, 1:2], 0.0), writes=['onez'])

    def load_round(dst_ap, src_ap, key, ncols):
        si = nstg[0] % 2
        nstg[0] += 1
        K.dma('sp', stg[si][:, 0:ncols], src_ap, writes=[f'dstg{si}'])
        eng = 'dve' if si == 0 else 'pool'
        cp = nc.vector.tensor_copy if si == 0 else nc.gpsimd.tensor_copy
        K.op(eng, lambda: cp(dst_ap, stg[si][:, 0:ncols]), reads=[f'dstg{si}'], writes=[key])
    st = [K.ps(f'dst{i}', [128, 512]) for i in range(2)]
    pt = [K.sb(f'dpt{i}', [128, 512], BF16 if AV_BF16 else MM) for i in range(2)]
    acc = [K.ps(f'dacc{i}', [128, 512]) for i in range(4)]
    om = [K.sb(f'dom{i}', [128, 4, 128]) for i in range(2)]
    rec = K.sb('drec', [128, 4])
    dd = K.sb('ddd', [128, 4, 128])
    yy = K.sb('dyy', [128, 4, 128])
    ss = K.sb('dss', [128, 4])
    junk = K.sb('djunk', [128, 128])
    n = 0
    for h in range(4):
        for c0 in range(0, NTOK, 768):
            c1 = min(NTOK, c0 + 768)
            load_round(qT[:, c0:c1], D['qkT'][h, :, c0:c1], 'dqT', c1 - c0)
            load_round(kT[:, c0:c1], D['qkT'][4 + h, :, c0:c1], 'dkT', c1 - c0)
        for t0 in range(0, TT, 6):
            t1 = min(TT, t0 + 6)
            si = nstg[0] % 2
            nstg[0] += 1
            sv_ = stg[si][:, 0:(t1 - t0) * 128].rearrange("p (t c) -> p t c", c=128)
            K.dma('sp', sv_, D['pl'][t0 * 128:t1 * 128, 1280 + h * 128:1280 + (h + 1) * 128].rearrange("(t p) c -> p t c", p=128), writes=[f'dstg{si}'])
            if si == 0:
                K.op('dve', lambda: nc.vector.tensor_copy(va[:, t0:t1, 0:128], sv_), reads=[f'dstg{si}'], writes=['dva'])
            else:
                K.op('pool', lambda: nc.gpsimd.tensor_copy(va[:, t0:t1, 0:128], sv_), reads=[f'dstg{si}'], writes=['dva'])
        K.op('dve', lambda: nc.vector.tensor_copy(va[:, :, 128:130], onez[:]), reads=['onez'], writes=['dva'])
        blocks = [(CT * 128 + qb * 512, 512, list(range(TT))) for qb in range(NT // 4)]
        if with_ctx:
            blocks.append((0, 256, [0, 1]))
        for (q0, N, kts) in blocks:
            nq = N // 128
            for m in range(2):
                ms = slice(m * 64, (m + 1) * 64)

                def S(kt, n):
                    K.op('pe', lambda: nc.tensor.matmul(st[n % 2][:, 0:N], kT[ms, kt * 128:(kt + 1) * 128], qT[ms, q0:q0 + N], start=True, stop=True),
                         reads=['dkT', 'dqT'], writes=[f'dst{n % 2}'])
                    K.op('act', lambda: nc.scalar.activation(pt[n % 2][:, 0:N], st[n % 2][:, 0:N], AF.Exp, scale=0.125),
                         reads=[f'dst{n % 2}'], writes=[f'dpt{n % 2}'])

                def AV(kt, n, first, last):
                    for qs in range(nq):
                        K.op('pe', lambda qs=qs: nc.tensor.matmul(acc[qs][:, 0:130], pt[n % 2][:, qs * 128:(qs + 1) * 128], va[:, kt, :], start=first, stop=last),
                             reads=[f'dpt{n % 2}', 'dva'], writes=[f'dacc{qs}'])
                S(kts[0], n)
                for ki, kt in enumerate(kts):
                    if ki + 1 < len(kts):
                        S(kts[ki + 1], n + 1)
                    AV(kt, n, ki == 0, ki == len(kts) - 1)
                    n += 1
                for qs in range(nq):
                    K.op('dve', lambda qs=qs: nc.vector.reciprocal(rec[:, qs:qs + 1], acc[qs][:, 128:129]), reads=[f'dacc{qs}'], writes=['drec'])
                    K.op('dve', lambda qs=qs: nc.vector.tensor_scalar(om[m][:, qs, :], acc[qs][:, 0:128], rec[:, qs:qs + 1], None, ALU.mult),
                         reads=[f'dacc{qs}', 'drec'], writes=[f'dom{m}'])
            for qs in range(nq):
                K.op('dve', lambda qs=qs: nc.vector.scalar_tensor_tensor(dd[:, qs, :], om[1][:, qs, :], neglam[:], om[0][:, qs, :], ALU.mult, ALU.add),
                     reads=['dom0', 'dom1', 'neglam'], writes=['ddd'])
                K.op('act', lambda qs=qs: nc.scalar.activation(junk[:], dd[:, qs, :], AF.Square, accum_out=ss[:, qs:qs + 1]), reads=['ddd'], writes=['djunk', 'dss'])
            K.op('dve', lambda: nc.vector.tensor_scalar(ss[:, 0:nq], ss[:, 0:nq], 1.0 / 128.0, 1e-6, ALU.mult, ALU.add), reads=['dss'], writes=['dss'])
            K.op('act', lambda: nc.scalar.activation(ss[:, 0:nq], ss[:, 0:nq], AF.Sqrt), reads=['dss'], writes=['dss'])
            K.op('dve', lambda: nc.vector.reciprocal(ss[:, 0:nq], ss[:, 0:nq]), reads=['dss'], writes=['dss'])
            for qs in range(nq):
                K.op('dve', lambda qs=qs: nc.vector.scalar_tensor_tensor(yy[:, qs, :], dd[:, qs, :], ss[:, qs:qs + 1], gbc[:], ALU.mult, ALU.mult),
                     reads=['ddd', 'dss', 'dgbc'], writes=['dyy'])
            K.dma('pool', D['ymix'][q0:q0 + N, 256 + h * 128:256 + (h + 1) * 128].rearrange("(s p) c -> p s c", p=128), yy[:, 0:nq, :],
                  reads=['dyy'], writes=[('ymix', 'd', h, q0)])
            K.maybe_barrier()
    K.pop()


def stage_na(K, nc, D, cfg, l, C, with_ctx):
    K.push()
    TT, NT, R = cfg.TT, cfg.NT, cfg.R
    NTOK = TT * 128
    pairs, types = na_pair_info(R)
    ident = C['ident']
    id8 = K.sb('id8', [128, 128], BF16)
    K.op('dve', lambda: nc.vector.tensor_scalar(id8[:], ident[:], 8.0, None, ALU.mult), reads=['ident'], writes=['id8'])
    nq = K.sb('nq', [64, NTOK], BF16)
    nk = K.sb('nk', [64, NTOK], BF16)
    nv = K.sb('nv', [128, TT, 66], BF16)
    nab = K.sb('nab', [128, len(types) * 5, 128], BF16)
    nstg = [K.sb(f'nstg{i}', [128, 1024]) for i in range(2)]
    nsn = [0]
    onez = K.sb('nonez', [128, TT, 2])
    K.op('pool', lambda: nc.gpsimd.memset(onez[:, :, 0:1], 1.0), writes=['nonez'])
    K.op('pool', lambda: nc.gpsimd.memset(onez[:, :, 1:2], 0.0), writes=['nonez'])

    def load_cvt(dst_ap, src_ap, key, np_, shape_view=None):
        si = nsn[0] % 2
        nsn[0] += 1
        ncols = 1
        for d_ in dst_ap.shape[1:]:
            ncols *= d_
        sv_ = nstg[si][0:np_, 0:ncols]
        if shape_view is not None:
            sv_ = sv_.rearrange(shape_view[0], **shape_view[1])
        K.dma('sp', sv_, src_ap, writes=[f'nstg{si}'])
        if si == 0:
            K.op('dve', lambda: nc.vector.tensor_copy(dst_ap, sv_), reads=[f'nstg{si}'], writes=[key])
        else:
            K.op('pool', lambda: nc.gpsimd.tensor_copy(dst_ap, sv_), reads=[f'nstg{si}'], writes=[key])
    S = [K.ps(f'nS{i}', [128, 1024]) for i in range(2)]
    P = [K.sb(f'nP{i}', [128, 1024], BF16) for i in range(2)]
    acc = [K.ps(f'nacc{i}', [128, 512]) for i in range(2)]
    rec = K.sb('nrec', [128, 1])
    ysb = [K.sb(f'nys{i}', [128, 64]) for i in range(2)]
    n = 0
    for h in range(4):
        for c0 in range(0, NTOK, 1024):
            c1 = min(NTOK, c0 + 1024)
            load_cvt(nq[:, c0:c1], D['nqkT'][h // 2, (h % 2) * 64:(h % 2 + 1) * 64, c0:c1], 'nq', 64)
            load_cvt(nk[:, c0:c1], D['nqkT'][2 + h // 2, (h % 2) * 64:(h % 2 + 1) * 64, c0:c1], 'nk', 64)
        for t0 in range(0, TT, 8):
            t1 = min(TT, t0 + 8)
            load_cvt(nv[:, t0:t1, 0:64], D['pl'][t0 * 128:t1 * 128, 2304 + h * 64:2304 + (h + 1) * 64].rearrange("(t p) c -> p t c", p=128), 'nv', 128,
                     ("p (t c) -> p t c", dict(c=64)))
        K.op('dve', lambda: nc.vector.tensor_copy(nv[:, :, 64:66], onez[:]), reads=['nonez'], writes=['nv'])
        nj = len(types) * 5
        for j0 in range(0, nj, 8):
            j1 = min(nj, j0 + 8)
            load_cvt(nab[:, j0:j1, :], D['nab'][l, h, j0:j1].rearrange("j k q -> k j q"), 'nab', 128, ("p (j q) -> p j q", dict(q=128)))
        jobs = []
        for (r, base, ty) in pairs:
            tl = [(CT + (base + 2 * j) // 2, ty * 5 + j) for j in range(5)] + [(0, None), (1, None)]
            jobs.append((CT * 128 + r * 64, tl))
        if with_ctx:
            for qt in range(2):
                jobs.append((qt * 128, [(0, None), (1, None)]))
        for (q0, tl) in jobs:
            i = n % 2
            n += 1
            nt = len(tl)
            for j, (kt, bj) in enumerate(tl):
                K.op('pe', lambda j=j, kt=kt, bj=bj: nc.tensor.matmul(S[i][:, j * 128:(j + 1) * 128], nk[:, kt * 128:(kt + 1) * 128], nq[:, q0:q0 + 128],
                                                                  start=True, stop=(bj is None)),
                     reads=['nk', 'nq'], writes=[f'nS{i}'])
                if bj is not None:
                    K.op('pe', lambda j=j, bj=bj: nc.tensor.matmul(S[i][:, j * 128:(j + 1) * 128], id8[:], nab[:, bj, :], start=False, stop=True),
                         reads=['id8', 'nab'], writes=[f'nS{i}'])
            for c0 in range(0, nt * 128, 512):
                c1 = min(nt * 128, c0 + 512)
                K.op('act', lambda c0=c0, c1=c1: nc.scalar.activation(P[i][:, c0:c1], S[i][:, c0:c1], AF.Exp, scale=0.125),
                     reads=[f'nS{i}'], writes=[f'nP{i}'])
            for j, (kt, bj) in enumerate(tl):
                K.op('pe', lambda j=j, kt=kt: nc.tensor.matmul(acc[i][:, 0:66], P[i][:, j * 128:(j + 1) * 128], nv[:, kt, :], start=(j == 0), stop=(j == nt - 1)),
                     reads=[f'nP{i}', 'nv'], writes=[f'nacc{i}'])
            K.op('dve', lambda: nc.vector.reciprocal(rec[:], acc[i][:, 64:65]), reads=[f'nacc{i}'], writes=['nrec'])
            K.op('dve', lambda: nc.vector.tensor_scalar(ysb[i][:], acc[i][:, 0:64], rec[:], None, ALU.mult), reads=[f'nacc{i}', 'nrec'], writes=[f'nys{i}'])
            K.dma('pool', D['ymix'][q0:q0 + 128, 768 + h * 64:768 + (h + 1) * 64], ysb[i][:], reads=[f'nys{i}'], writes=[('ymix', 'n', h, q0)])
    K.pop()


def core_inputs(inp, b, L, depth, half=0):
    R = L // 64
    sel = np.zeros((2, 256), np.float32)
    sel[0, :128] = 1
    sel[1, 128:] = 1
    f = lambda a: np.ascontiguousarray(a, dtype=np.float32)
    hs = [host_ssm(inp, l) for l in range(depth)]
    im = dict(
        x=inp['x'][b, :L], ctx=inp['ctx'][b], cc=np.stack([inp['c'][b], inp['c_ctx']]),
        ada_w=inp['ada_w'][:depth], ada_b=inp['ada_b'][:depth],
        norm_g=np.concatenate([inp['norm1_g'], inp['norm2_g']], 1)[:depth],
        w_in=inp['w_in'][:depth], ident=np.eye(128, dtype=np.float32), sel=sel,
        rope=host_rope(L), nab=np.stack([host_nab(inp['na_rpb'][l], R) for l in range(depth)]),
        diff_lambda=inp['diff_lambda'][:depth].reshape(depth, 256), diff_subln_g=inp['diff_subln_g'][:depth],
        jmat=np.eye(128, dtype=np.float32)[::-1],
        ssm_sc=np.stack([hs[l][0] for l in range(depth)]), ssm_b=np.stack([hs[l][1] for l in range(depth)]),
        ssm_c=np.stack([hs[l][2] for l in range(depth)]),
        ssm_dT=np.stack([inp['ssm_d'][l].reshape(2, 128).T for l in range(depth)]), ssm_glu_w=inp['ssm_glu_w'][:depth],
        w_br_ssm=inp['w_br_ssm'][:depth], w_br_diff=inp['w_br_diff'][:depth], w_br_na=inp['w_br_na'][:depth], w_out=inp['w_out'][:depth],
        peer_wq=inp['peer_wq'][:depth], peer_sk=inp['peer_subkeys'][:depth].reshape(depth, 16, 128, 128),
        final_g=inp['final_norm_g'].reshape(1, 1024),
        iota16=np.tile(np.arange(16, dtype=np.float32), (128, 1)),
    )
    out = {k: f(v) for k, v in im.items()}
    if PAIR_SPLIT:
        nt2 = (L // 128) // 2
        out['rowidx'] = np.ascontiguousarray((CT * 128 + half * (L // 2) + np.arange(nt2)[None, :] * 128 + np.arange(128)[:, None]).astype(np.int32))
    im = {}
    for i in range(depth):
        im[f'peer_u{i}'] = inp['peer_u'][i]
        im[f'peer_v{i}'] = inp['peer_v'][i]
    out.update({k: f(v) for k, v in im.items()})
    return out


TWO_PI = float(2 * np.pi)


def host_ssm(inp, l):
    lre, lim, ls = inp['ssm_lambda_re'][l], inp['ssm_lambda_im'][l], inp['ssm_log_step'][l]
    sc = np.zeros((128, 48), np.float32)
    bb = np.zeros((2, 16, 128, 128), np.float32)
    cm = np.zeros((2, 16, 128, 128), np.float32)
    for d in range(2):
        for st in range(8):
            col = (d * 8 + st) * 3
            for gl in range(2):
                g = 2 * st + gl
                rows = slice(gl * 64, (gl + 1) * 64)
                sc[rows, col + 0] = lre[d, g]
                sc[rows, col + 1] = lim[d, g]
                sc[rows, col + 2] = ls[d, g]
                c0 = (g - 8 * (st // 4)) * 16
                bb[0, d * 8 + st, rows, c0:c0 + 16] = inp['ssm_b_re'][l, d, g]
                bb[1, d * 8 + st, rows, c0:c0 + 16] = inp['ssm_b_im'][l, d, g]
                cm[0, d * 8 + st, rows, c0:c0 + 16] = inp['ssm_c_re'][l, d, g].T
                cm[1, d * 8 + st, rows, c0:c0 + 16] = inp['ssm_c_im'][l, d, g].T
    return sc, bb, cm


def gelu_tanh(K, nc, eng_a, out, x, xkey, outkey, t1, t1key):
    K.op('dve', lambda: nc.vector.tensor_tensor(t1, x, x, ALU.mult), reads=[xkey], writes=[t1key])
    K.op('dve', lambda: nc.vector.tensor_scalar(t1, t1, 0.044715, 1.0, ALU.mult, ALU.add), reads=[t1key], writes=[t1key])
    K.op('dve', lambda: nc.vector.tensor_tensor(t1, t1, x, ALU.mult), reads=[t1key, xkey], writes=[t1key])
    K.op('act', lambda: nc.scalar.activation(t1, t1, AF.Sigmoid, scale=1.5957691216057308), reads=[t1key], writes=[t1key])
    K.op('dve', lambda: nc.vector.tensor_tensor(out, x, t1, ALU.mult), reads=[xkey, t1key], writes=[outkey])


def stage_ssm(K, nc, D, cfg, l, C, with_ctx):
    K.push()
    TT, NT = cfg.TT, cfg.NT
    ident = C['ident']
    jm = K.sb('jm', [128, 128])
    K.dma('sp', jm[:], D['jmat'], writes=['jm'])
    T = 512
    sc = K.sb('ssc', [128, 16, 3])
    K.dma('sp', sc[:], D['ssm_sc'][l].rearrange("p (c j) -> p c j", j=3), writes=['ssc'])
    names = ['dtv', 'lre', 'a', 'th', 'rho', 'cos', 'sin', 'x', 'den', 'cfr', 'cfi', 'k', 'tmp', 'tmp2', 'nsin', 'ncfi']
    V = {nm: K.sb('sv_' + nm, [128, 16]) for nm in names}
    ki = K.sb('sv_ki', [128, 16], I32)
    lim = sc[:, :, 1]

    def dv(fn, reads, writes):
        K.op('dve', fn, reads=['sv_' + r if r != 'ssc' else r for r in reads], writes=['sv_' + w for w in writes])

    SV = ['svall', 'ssc']

    def d1(fn):
        K.op('dve', fn, reads=SV, writes=['svall'])

    def tt(o, a_, b_, op):
        d1(lambda: nc.vector.tensor_tensor(o, a_, b_, op))

    def ts(o, a_, s1, s2, op0, op1=None):
        if op1 is None:
            d1(lambda: nc.vector.tensor_scalar(o, a_, s1, None, op0))
        else:
            d1(lambda: nc.vector.tensor_scalar(o, a_, s1, s2, op0, op1))

    def to_int_float(dst, src):
        d1(lambda: nc.vector.tensor_copy(ki[:], src))
        d1(lambda: nc.vector.tensor_copy(dst, ki[:]))

    def horner(dst, z, coefs):
        ts(dst, z, float(coefs[-1]), 1.0, ALU.mult, ALU.add)
        for cf in reversed(coefs[:-1]):
            tt(dst, dst, z, ALU.mult)
            ts(dst, dst, float(cf), 1.0, ALU.mult, ALU.add)

    x_, k_, t_, t2_ = V['x'][:], V['k'][:], V['tmp'][:], V['tmp2'][:]
    ts(x_, sc[:, :, 2], 1.0 / 0.6931471805599453, None, ALU.mult)
    to_int_float(k_, x_)
    ts(t_, k_, -0.693359375, None, ALU.mult)
    tt(x_, sc[:, :, 2], t_, ALU.add)
    ts(t_, k_, 2.12194440e-4, None, ALU.mult)
    tt(x_, x_, t_, ALU.add)
    horner(V['dtv'][:], x_, [1.0 / i for i in range(1, 14)])
    for j in range(1, 17):
        ts(t_, k_, float(-j), -0.5, ALU.is_le, ALU.mult)
        ts(t_, t_, 1.0, None, ALU.add)
        tt(V['dtv'][:], V['dtv'][:], t_, ALU.mult)
    ts(V['lre'][:], sc[:, :, 0], -1e-4, None, ALU.min)
    tt(V['a'][:], V['lre'][:], V['dtv'][:], ALU.mult)
    tt(V['th'][:], lim, V['dtv'][:], ALU.mult)
    horner(V['rho'][:], V['a'][:], [1.0 / i for i in range(1, 10)])
    ts(x_, V['th'][:], 1.0 / TWO_PI, None, ALU.mult)
    to_int_float(k_, x_)
    ts(t_, k_, -6.28125, None, ALU.mult)
    tt(x_, V['th'][:], t_, ALU.add)
    ts(t_, k_, -1.9353071795864769e-3, None, ALU.mult)
    tt(x_, x_, t_, ALU.add)
    for sgn, cmpop, thr in ((-1.0, ALU.is_gt, float(np.pi)), (1.0, ALU.is_lt, -float(np.pi))):
        ts(t_, x_, thr, None, cmpop)
        ts(t2_, t_, sgn * 6.28125, None, ALU.mult)
        tt(x_, x_, t2_, ALU.add)
        ts(t2_, t_, sgn * 1.9353071795864769e-3, None, ALU.mult)
        tt(x_, x_, t2_, ALU.add)
    ts(x_, x_, 0.25, None, ALU.mult)
    tt(k_, x_, x_, ALU.mult)
    horner(V['cos'][:], k_, [-1.0 / ((2 * i) * (2 * i - 1)) for i in range(1, 9)])
    horner(V['sin'][:], k_, [-1.0 / ((2 * i) * (2 * i + 1)) for i in range(1, 9)])
    tt(V['sin'][:], V['sin'][:], x_, ALU.mult)
    for _ in range(2):
        tt(t_, V['cos'][:], V['cos'][:], ALU.mult)
        tt(t2_, V['sin'][:], V['sin'][:], ALU.mult)
        tt(V['sin'][:], V['sin'][:], V['cos'][:], ALU.mult)
        ts(V['sin'][:], V['sin'][:], 2.0, None, ALU.mult)
        tt(V['cos'][:], t_, t2_, ALU.subtract)
    tt(t_, V['cos'][:], V['cos'][:], ALU.mult)
    tt(t2_, V['sin'][:], V['sin'][:], ALU.mult)
    tt(t_, t_, t2_, ALU.add)
    ts(t_, t_, -0.5, 1.5, ALU.mult, ALU.add)
    tt(V['cos'][:], V['cos'][:], t_, ALU.mult)
    tt(V['sin'][:], V['sin'][:], t_, ALU.mult)

    def dv(fn, reads, writes):
        K.op('dve', fn, reads=SV, writes=['svall'])

    dv(lambda: nc.vector.tensor_tensor(V['tmp'][:], V['rho'][:], V['cos'][:], ALU.mult), ['rho', 'cos'], ['tmp'])
    dv(lambda: nc.vector.tensor_scalar(V['tmp'][:], V['tmp'][:], -1.0, None, ALU.add), ['tmp'], ['tmp'])
    dv(lambda: nc.vector.tensor_tensor(V['tmp2'][:], V['rho'][:], V['sin'][:], ALU.mult), ['rho', 'sin'], ['tmp2'])
    dv(lambda: nc.vector.tensor_tensor(V['den'][:], V['lre'][:], V['lre'][:], ALU.mult), ['lre'], ['den'])
    dv(lambda: nc.vector.tensor_tensor(V['k'][:], lim, lim, ALU.mult), ['ssc'], ['k'])
    dv(lambda: nc.vector.tensor_tensor(V['den'][:], V['den'][:], V['k'][:], ALU.add), ['den', 'k'], ['den'])
    dv(lambda: nc.vector.reciprocal(V['den'][:], V['den'][:]), ['den'], ['den'])
    dv(lambda: nc.vector.tensor_tensor(V['cfr'][:], V['tmp'][:], V['lre'][:], ALU.mult), ['tmp', 'lre'], ['cfr'])
    dv(lambda: nc.vector.tensor_tensor(V['k'][:], V['tmp2'][:], lim, ALU.mult), ['tmp2', 'ssc'], ['k'])
    dv(lambda: nc.vector.tensor_tensor(V['cfr'][:], V['cfr'][:], V['k'][:], ALU.add), ['cfr', 'k'], ['cfr'])
    dv(lambda: nc.vector.tensor_tensor(V['cfr'][:], V['cfr'][:], V['den'][:], ALU.mult), ['cfr', 'den'], ['cfr'])
    dv(lambda: nc.vector.tensor_tensor(V['cfi'][:], V['tmp2'][:], V['lre'][:], ALU.mult), ['tmp2', 'lre'], ['cfi'])
    dv(lambda: nc.vector.tensor_tensor(V['k'][:], V['tmp'][:], lim, ALU.mult), ['tmp', 'ssc'], ['k'])
    dv(lambda: nc.vector.tensor_tensor(V['cfi'][:], V['cfi'][:], V['k'][:], ALU.subtract), ['cfi', 'k'], ['cfi'])
    dv(lambda: nc.vector.tensor_tensor(V['cfi'][:], V['cfi'][:], V['den'][:], ALU.mult), ['cfi', 'den'], ['cfi'])
    dv(lambda: nc.vector.tensor_scalar(V['ncfi'][:], V['cfi'][:], -1.0, None, ALU.mult), ['cfi'], ['ncfi'])
    dv(lambda: nc.vector.tensor_scalar(V['nsin'][:], V['sin'][:], -1.0, None, ALU.mult), ['sin'], ['nsin'])
    Bt = K.sb('sBt', [128, 16, 2, 128])
    Cm = K.sb('sCm', [128, 16, 2, 128])
    K.dma('sp', Cm[:, :, 0, :], D['ssm_c'][l, 0].rearrange("c p f -> p c f"), writes=['sCm'])
    K.dma('sp', Cm[:, :, 1, :], D['ssm_c'][l, 1].rearrange("c p f -> p c f"), writes=['sCm'])
    K.op('pool', lambda: nc.gpsimd.tensor_scalar(Cm[:, :, 1, :], Cm[:, :, 1, :], -1.0, None, ALU.mult), reads=['sCm'], writes=['sCm'])
    K.push()
    braw = K.sb('sbraw', [128, 16, 2, 128])
    K.dma('sp', braw[:, :, 0, :], D['ssm_b'][l, 0].rearrange("c p f -> p c f"), writes=['sbraw'])
    K.dma('sp', braw[:, :, 1, :], D['ssm_b'][l, 1].rearrange("c p f -> p c f"), writes=['sbraw'])
    bbar = [K.sb(f'sbbar{i}', [128, 2, 128]) for i in range(2)]
    t4 = [K.sb(f'sbt{i}', [128, 128]) for i in range(2)]
    pbt = [K.ps(f'spbt{i}', [128, 256]) for i in range(2)]
    for c in range(16):
        i = c % 2
        cr, ci, nci = V['cfr'][:, c:c + 1], V['cfi'][:, c:c + 1], V['ncfi'][:, c:c + 1]
        K.op('dve', lambda: nc.vector.tensor_scalar(t4[0][:], braw[:, c, 1, :], nci, None, ALU.mult), reads=['sbraw', 'svall'], writes=['sbt0'])
        K.op('dve', lambda: nc.vector.scalar_tensor_tensor(bbar[i][:, 0, :], braw[:, c, 0, :], cr, t4[0][:], ALU.mult, ALU.add),
             reads=['sbraw', 'svall', 'sbt0'], writes=[f'sbbar{i}'])
        K.op('dve', lambda: nc.vector.tensor_scalar(t4[1][:], braw[:, c, 0, :], ci, None, ALU.mult), reads=['sbraw', 'svall'], writes=['sbt1'])
        K.op('dve', lambda: nc.vector.scalar_tensor_tensor(bbar[i][:, 1, :], braw[:, c, 1, :], cr, t4[1][:], ALU.mult, ALU.add),
             reads=['sbraw', 'svall', 'sbt1'], writes=[f'sbbar{i}'])
        for ri in range(2):
            K.op('pe', lambda ri=ri: nc.tensor.transpose(pbt[i][:, ri * 128:(ri + 1) * 128], bbar[i][:, ri, :], ident[:]),
                 reads=[f'sbbar{i}', 'ident'], writes=[f'spbt{i}'])
        K.op('act', lambda: nc.scalar.copy(Bt[:, c, :, :], pbt[i][:].rearrange("p (r f) -> p r f", r=2)), reads=[f'spbt{i}'], writes=['sBt'])
    K.pop()
    E = K.sb('sE', [128, 16, 2, T])
    et = [K.sb(f'set{i}', [128, T // 2]) for i in range(2)]
    for c in range(16):
        K.op('dve', lambda: nc.vector.tensor_copy(E[:, c, 0, 0:1], V['cos'][:, c:c + 1]), reads=['svall'], writes=[('sE', c)])
        K.op('dve', lambda: nc.vector.tensor_copy(E[:, c, 1, 0:1], V['sin'][:, c:c + 1]), reads=['svall'], writes=[('sE', c)])
        m = 1
        while m < T:
            wr, wi = E[:, c, 0, m - 1:m], E[:, c, 1, m - 1:m]
            er, ei = E[:, c, 0, 0:m], E[:, c, 1, 0:m]
            K.op('dve', lambda: nc.vector.tensor_scalar(et[0][:, 0:m], ei, wi, None, ALU.mult), reads=[('sE', c)], writes=['set0'])
            K.op('dve', lambda: nc.vector.tensor_scalar(et[1][:, 0:m], ei, wr, None, ALU.mult), reads=[('sE', c)], writes=['set1'])
            K.op('dve', lambda: nc.vector.scalar_tensor_tensor(E[:, c, 0, m:2 * m], er, wr, et[0][:, 0:m], ALU.mult, ALU.subtract),
                 reads=[('sE', c), 'set0'], writes=[('sE', c)])
            K.op('dve', lambda: nc.vector.scalar_tensor_tensor(E[:, c, 1, m:2 * m], er, wi, et[1][:, 0:m], ALU.mult, ALU.add),
                 reads=[('sE', c), 'set1'], writes=[('sE', c)])
            m *= 2
    dT = K.sb('sdT', [128, 2])
    K.dma('sp', dT[:], D['ssm_dT'][l], writes=['sdT'])
    gw = K.sb('sgw', [128, 2, 512])
    K.dma('sp', gw[:], D['ssm_glu_w'][l].rearrange("(k p) n -> p k n", p=128), writes=['sgw'])
    carry = K.sb('scarry', [128, 8, 2])
    ut = [K.sb(f'sut{i}', [128, 4, 256]) for i in range(1)]
    uT = [K.sb(f'suT{i}', [128, 2, T]) for i in range(2)]
    puT = K.ps('spuT', [128, 2, T])
    pbu = [K.ps(f'spbu{i}', [128, 2, T]) for i in range(1)]
    bu = [K.sb(f'sbu{i}', [128, 2, T]) for i in range(2)]
    X = [K.sb(f'sX{i}', [128, 2, T]) for i in range(2)]
    W = [K.sb(f'sW{i}', [128, 2, T]) for i in range(2)]
    S = [K.sb(f'sS{i}', [128, 2, T]) for i in range(2)]
    tq_all = [K.sb(f'stq{i}', [128, T]) for i in range(8)]
    py = K.ps('spy', [128, 2, T])
    yTf = K.sb('syTf', [128, 2, T])
    ytok = K.sb('sytok', [128, 4, 256])
    yb = K.sb('syb', [128, 2, T])
    g1 = K.sb('sg1', [128, 2, T])
    pz = K.ps('spz', [128, T])
    zs = K.sb('szs', [128, 512])
    zo = K.sb('szo', [128, 256])
    nu = 0
    for d in range(2):
        chunks = [([0, 1], True)] + [([CT + 4 * c + i for i in range(4)], False) for c in range(NT // 4)]
        if d == 1:
            chunks = [([1, 0], True)] + [([CT + NT - 1 - (4 * c + i) for i in range(4)], False) for c in range(NT // 4)]
        for ci, (tl, is_ctx) in enumerate(chunks):
            Tc = len(tl) * 128
            ui = ci % 2
            for j, tt in enumerate(tl):
                K.dma('sp', ut[0][:, j, :], D['pl'][tt * 128:(tt + 1) * 128, 0:256], writes=['sut0'])
            for j in range(len(tl)):
                for kc in range(2):
                    K.op('pe', lambda j=j, kc=kc: nc.tensor.matmul(puT[:, kc, j * 128:(j + 1) * 128], ut[0][:, j, kc * 128:(kc + 1) * 128],
                                                                 (ident if d == 0 else jm)[:], start=True, stop=True),
                         reads=['sut0', 'ident', 'jm'], writes=['spuT'])
            K.op('act', lambda: nc.scalar.copy(uT[ui][:, :, 0:Tc], puT[:, :, 0:Tc]), reads=['spuT'], writes=[f'suT{ui}'])
            need_out = (not is_ctx) or with_ctx
            for st in range(8):
                c = d * 8 + st
                kc = st // 4
                b = nu % 2
                nu += 1
                tq = tq_all[4 * b:4 * b + 4]
                tqk = [f'stq{4 * b + i}' for i in range(4)]
                for ri in range(2):
                    K.op('pe', lambda ri=ri: nc.tensor.matmul(pbu[0][:, ri, 0:Tc], Bt[:, c, ri, :], uT[ui][:, kc, 0:Tc], start=True, stop=True),
                         reads=['sBt', f'suT{ui}'], writes=['spbu0'])
                K.op('act', lambda: nc.scalar.copy(bu[b][:, :, 0:Tc], pbu[0][:, :, 0:Tc]), reads=['spbu0'], writes=[f'sbu{b}'])
                Er, Ei = E[:, c, 0, 0:Tc], E[:, c, 1, 0:Tc]
                br, bi = bu[b][:, 0, 0:Tc], bu[b][:, 1, 0:Tc]
                ek = ('sE', c)
                K.op('dve', lambda: nc.vector.tensor_tensor(tq[0][:, 0:Tc], Er, br, ALU.mult), reads=[ek, f'sbu{b}'], writes=[tqk[0]])
                K.op('dve', lambda: nc.vector.tensor_tensor(tq[1][:, 0:Tc], Ei, bi, ALU.mult), reads=[ek, f'sbu{b}'], writes=[tqk[1]])
                K.op('dve', lambda: nc.vector.tensor_tensor(X[b][:, 0, 0:Tc], tq[0][:, 0:Tc], tq[1][:, 0:Tc], ALU.add), reads=[tqk[0], tqk[1]], writes=[f'sX{b}'])
                K.op('pool', lambda: nc.gpsimd.tensor_tensor(tq[2][:, 0:Tc], Er, bi, ALU.mult), reads=[ek, f'sbu{b}'], writes=[tqk[2]])
                K.op('pool', lambda: nc.gpsimd.tensor_tensor(tq[3][:, 0:Tc], Ei, br, ALU.mult), reads=[ek, f'sbu{b}'], writes=[tqk[3]])
                K.op('pool', lambda: nc.gpsimd.tensor_tensor(X[b][:, 1, 0:Tc], tq[2][:, 0:Tc], tq[3][:, 0:Tc], ALU.subtract), reads=[tqk[2], tqk[3]], writes=[f'sX{b}'])
                rho_b = V['rho'][:, c:c + 1].to_broadcast([128, Tc])
                for ri in range(2):
                    init = 0.0 if ci == 0 else carry[:, st, ri:ri + 1]
                    K.op('dve', lambda ri=ri, init=init: nc.vector.tensor_tensor_scan(W[b][:, ri, 0:Tc], rho_b, X[b][:, ri, 0:Tc], init, ALU.mult, ALU.add),
                         reads=[f'sX{b}', 'svall', ('scarry', st)], writes=[f'sW{b}'])
                wr_, wi_ = W[b][:, 0, 0:Tc], W[b][:, 1, 0:Tc]
                K.op('dve', lambda: nc.vector.tensor_tensor(tq[0][:, 0:Tc], Er, wr_, ALU.mult), reads=[ek, f'sW{b}'], writes=[tqk[0]])
                K.op('dve', lambda: nc.vector.tensor_tensor(tq[1][:, 0:Tc], Ei, wi_, ALU.mult), reads=[ek, f'sW{b}'], writes=[tqk[1]])
                K.op('dve', lambda: nc.vector.tensor_tensor(S[b][:, 0, 0:Tc], tq[0][:, 0:Tc], tq[1][:, 0:Tc], ALU.subtract), reads=[tqk[0], tqk[1]], writes=[f'sS{b}'])
                K.op('pool', lambda: nc.gpsimd.tensor_tensor(tq[2][:, 0:Tc], Er, wi_, ALU.mult), reads=[ek, f'sW{b}'], writes=[tqk[2]])
                K.op('pool', lambda: nc.gpsimd.tensor_tensor(tq[3][:, 0:Tc], Ei, wr_, ALU.mult), reads=[ek, f'sW{b}'], writes=[tqk[3]])
                K.op('pool', lambda: nc.gpsimd.tensor_tensor(S[b][:, 1, 0:Tc], tq[2][:, 0:Tc], tq[3][:, 0:Tc], ALU.add), reads=[tqk[2], tqk[3]], writes=[f'sS{b}'])
                K.op('act', lambda: nc.scalar.copy(carry[:, st, :], S[b][:, :, Tc - 1]), reads=[f'sS{b}'], writes=[('scarry', st)])
                if not need_out:
                    continue
                first = (st % 4 == 0)
                last = (st % 4 == 3)
                for ri in range(2):
                    K.op('pe', lambda ri=ri: nc.tensor.matmul(py[:, kc, 0:Tc], Cm[:, c, ri, :], S[b][:, ri, 0:Tc], start=(first and ri == 0), stop=(last and ri == 1)),
                         reads=['sCm', f'sS{b}'], writes=['spy'])
            if not need_out:
                continue
            if d == 0:
                col0 = tl[0] * 128
                for ft in range(2):
                    K.op('dve', lambda ft=ft: nc.vector.scalar_tensor_tensor(yTf[:, ft, 0:Tc], uT[ui][:, ft, 0:Tc], dT[:, ft:ft + 1], py[:, ft, 0:Tc], ALU.mult, ALU.add),
                         reads=[f'suT{ui}', 'sdT', 'spy'], writes=['syTf'])
                K.dma('pool', D['ysT'][:, :, col0:col0 + Tc].rearrange("f p t -> p f t"), yTf[:, :, 0:Tc], reads=['syTf'], writes=[('ysT', col0)])
            else:
                nt_ = len(tl)
                K.op('act', lambda: nc.scalar.copy(yTf[:, :, 0:Tc], py[:, :, 0:Tc]), reads=['spy'], writes=['syTf'])
                pyt = puT[:].rearrange("p a t -> p (a t)").rearrange("p (s f) -> p s f", f=256)
                for j in range(nt_):
                    for ft in range(2):
                        K.op('pe', lambda j=j, ft=ft: nc.tensor.transpose(pyt[:, j, ft * 128:(ft + 1) * 128], yTf[:, ft, j * 128:(j + 1) * 128], ident[:]),
                             reads=['syTf', 'ident'], writes=['spuT'])
                K.op('act', lambda: nc.scalar.copy(ytok[:, 0:nt_, :], pyt[:, 0:nt_, :]), reads=['spuT'], writes=['sytok'])
                col0 = tl[-1] * 128
                K.dma('sp', yb[:, :, 0:Tc], D['ysT'][:, :, col0:col0 + Tc].rearrange("f p t -> p f t"), writes=['syb'])
                for j in range(nt_):
                    nj = nt_ - 1 - j
                    for ft in range(2):
                        K.op('pe', lambda j=j, nj=nj, ft=ft: nc.tensor.matmul(py[:, ft, nj * 128:(nj + 1) * 128], ytok[:, j, ft * 128:(ft + 1) * 128], jm[:], start=True, stop=True),
                             reads=['sytok', 'jm'], writes=['spy'])
                K.op('dve', lambda: nc.vector.tensor_tensor(yb[:, :, 0:Tc], yb[:, :, 0:Tc], py[:, :, 0:Tc], ALU.add), reads=['syb', 'spy'], writes=['syb'])
                if 'ydbg' in DEBUG_OUT:
                    K.dma('sp', D['ydbg'][:, :, col0:col0 + Tc].rearrange("f p t -> p f t"), yb[:, :, 0:Tc], reads=['syb'], writes=[('ydbg', col0)])
                gelu_tanh(K, nc, None, yb[:, :, 0:Tc], yb[:, :, 0:Tc], 'syb', 'syb', g1[:, :, 0:Tc], 'sg1')
                for j in range(nt_):
                    for ft in range(2):
                        K.op('pe', lambda j=j, ft=ft: nc.tensor.matmul(pz[:], yb[:, ft, j * 128:(j + 1) * 128], gw[:, ft, :], start=(ft == 0), stop=(ft == 1)),
                             reads=['syb', 'sgw'], writes=['spz'])
                    K.op('act', lambda: nc.scalar.activation(zs[:, 256:512], pz[:, 256:512], AF.Sigmoid), reads=['spz'], writes=['szs'])
                    K.op('dve', lambda: nc.vector.tensor_tensor(zo[:], pz[:, 0:256], zs[:, 256:512], ALU.mult), reads=['spz', 'szs'], writes=['szo'])
                    r0 = col0 + j * 128
                    K.dma('pool', D['ymix'][r0:r0 + 128, 0:256], zo[:], reads=['szo'], writes=[('ymix', 's', r0)])
        K.barrier()
    K.pop()


def stage_merge(K, nc, D, cfg, l, C, with_ctx):
    K.push()
    ident = C['ident']
    g1 = {'l': load_bc(K, D, 'l', 2)}
    if with_ctx:
        g1['c'] = load_bc(K, D, 'c', 2)
    wbr = K.sb('wbr', [128, 8, 1024], MM)
    wout = K.sb('wout', [128, 8, 1024], MM)
    K.push()
    mst = [K.sb(f'mst{i}', [128, 2, 1024]) for i in range(2)]
    srcs = [(wbr, 0, D['w_br_ssm'][l]), (wbr, 2, D['w_br_diff'][l][0:256]), (wbr, 4, D['w_br_diff'][l][256:512]), (wbr, 6, D['w_br_na'][l])] + \
           [(wout, 2 * j, D['w_out'][l][256 * j:256 * (j + 1)]) for j in range(4)]
    for si, (dst, k0, src) in enumerate(srcs):
        b_ = si % 2
        K.dma('sp', mst[b_][:], src.rearrange("(k p) n -> p k n", p=128), writes=[f'mst{b_}'])
        K.op('pool', lambda: nc.gpsimd.tensor_copy(dst[:, k0:k0 + 2, :], mst[b_][:]), reads=[f'mst{b_}'], writes=['wbr', 'wout'])
    K.pop()
    yt = [K.sb(f'myt{i}', [128, 1024]) for i in range(2)]
    gt = [K.sb(f'mgt{i}', [128, 3072]) for i in range(2)]
    xt = [K.sb(f'mxt{i}', [128, 1024]) for i in range(2)]
    yT = K.sb('myT', [128, 8, 128], MM)
    mm = K.sb('mmm', [128, 1024])
    mT = K.sb('mmT', [128, 8, 128], MM)
    tmp = K.sb('mtmp', [128, 512])
    xo = [K.sb(f'mxo{i}', [128, 1024]) for i in range(2)]
    ptr = K.ps('mptr', [128, 1024])
    pb = [K.ps(f'mpb{i}', [128, 512]) for i in range(2)]
    npb = 0
    tiles = list(range(cfg.TT)) if with_ctx else list(range(CT, cfg.TT))
    branches = [(0, [0, 1]), (1, [2, 3, 4, 5]), (2, [6, 7])]
    for tt in tiles:
        i = tt % 2
        w = 'c' if tt < CT else 'l'
        rows = slice(tt * 128, (tt + 1) * 128)
        K.dma('sp', yt[i][:], D['ymix'][rows, :], writes=[f'myt{i}'])
        K.dma('sp', gt[i][:], D['pl'][rows, 2560:5632], writes=[f'mgt{i}'])
        K.dma('sp', xt[i][:], xsrc(D, cfg, l, tt), writes=[f'mxt{i}'])
        K.op('act', lambda: nc.scalar.activation(gt[i][:], gt[i][:], AF.Sigmoid), reads=[f'mgt{i}'], writes=[f'mgt{i}'])
        for k in range(8):
            K.op('pe', lambda k=k: nc.tensor.transpose(ptr[:, k * 128:(k + 1) * 128], yt[i][:, k * 128:(k + 1) * 128], ident[:]),
                 reads=[f'myt{i}', 'ident'], writes=['mptr'])
        K.op('act', lambda: nc.scalar.copy(yT[:], ptr[:].rearrange("p (k t) -> p k t", k=8)), reads=['mptr'], writes=['myT'])
        for (br, ks) in branches:
            for hh in range(2):
                p = pb[npb % 2]
                pk = f'mpb{npb % 2}'
                npb += 1
                for ki, k in enumerate(ks):
                    K.op('pe', lambda k=k, ki=ki: nc.tensor.matmul(p[:], yT[:, k, :], wbr[:, k, hh * 512:(hh + 1) * 512], start=(ki == 0), stop=(ki == len(ks) - 1)),
                         reads=['myT', 'wbr'], writes=[pk])
                gsl = gt[i][:, br * 1024 + hh * 512: br * 1024 + (hh + 1) * 512]
                if br == 0:
                    K.op('dve', lambda: nc.vector.tensor_tensor(mm[:, hh * 512:(hh + 1) * 512], p[:], gsl, ALU.mult), reads=[pk, f'mgt{i}'], writes=[('mmm', hh)])
                else:
                    K.op('dve', lambda: nc.vector.tensor_tensor(tmp[:], p[:], gsl, ALU.mult), reads=[pk, f'mgt{i}'], writes=['mtmp'])
                    K.op('pool', lambda: nc.gpsimd.tensor_tensor(mm[:, hh * 512:(hh + 1) * 512], mm[:, hh * 512:(hh + 1) * 512], tmp[:], ALU.add),
                         reads=['mtmp', ('mmm', hh)], writes=[('mmm', hh)])
        for k in range(8):
            K.op('pe', lambda k=k: nc.tensor.transpose(ptr[:, k * 128:(k + 1) * 128], mm[:, k * 128:(k + 1) * 128], ident[:]),
                 reads=[('mmm', k // 4), 'ident'], writes=['mptr'])
        K.op('act', lambda: nc.scalar.copy(mT[:], ptr[:].rearrange("p (k t) -> p k t", k=8)), reads=['mptr'], writes=['mmT'])
        for hh in range(2):
            p = pb[npb % 2]
            pk = f'mpb{npb % 2}'
            npb += 1
            for k in range(8):
                K.op('pe', lambda k=k: nc.tensor.matmul(p[:], mT[:, k, :], wout[:, k, hh * 512:(hh + 1) * 512], start=(k == 0), stop=(k == 7)),
                     reads=['mmT', 'wout'], writes=[pk])
            K.op('dve', lambda: nc.vector.tensor_tensor(tmp[:], p[:], g1[w][:, hh * 512:(hh + 1) * 512], ALU.mult), reads=[pk, f'bc{w}2'], writes=['mtmp'])
            K.op('pool', lambda: nc.gpsimd.tensor_tensor(xo[i][:, hh * 512:(hh + 1) * 512], xt[i][:, hh * 512:(hh + 1) * 512], tmp[:], ALU.add),
                 reads=['mtmp', f'mxt{i}'], writes=[f'mxo{i}'])
        K.dma('pool', D['xres'][rows, :], xo[i][:], reads=[f'mxo{i}'], writes=[('xres', tt)])
    K.pop()


def stage_peer(K, nc, D, cfg, l, C, with_ctx, final):
    K.push()
    ident = C['ident']
    bc = {}
    for w in (['l', 'c'] if with_ctx else ['l']):
        for j in (3, 4, 5):
            bc[(w, j)] = load_bc(K, D, w, j)
    if final:
        fgr = K.sb('fgr', [1, 1024])
        K.dma('sp', fgr[:], D['final_g'], writes=['fgr'])
        fg = K.sb('fg', [128, 1024])
        K.push()
        with_ps = K.ps('fgps', [128, 512])
        for hh in range(2):
            bcast_row(K, nc, C, fgr[:, hh * 512:(hh + 1) * 512], 'fgr', fg[:, hh * 512:(hh + 1) * 512], 'fg', 512, with_ps, 'fgps')
        K.pop()
    wq = K.sb('wq', [128, 8, 2048])
    K.dma('sp', wq[:, :, 0:1024], D['peer_wq'][l, :, 0:1024].rearrange("(k p) n -> p k n", p=128), writes=['wq'])
    K.dma('sp', wq[:, :, 1024:2048], D['peer_wq'][l, :, 1024:2048].rearrange("(k p) n -> p k n", p=128), writes=['wq'])
    skT = K.sb('skT', [128, 16, 128])
    identR = K.sb('identR', [128, 128], MM)
    K.push()
    skr = K.sb('skr', [128, 16, 128])
    K.dma('sp', skr[:], D['peer_sk'][l].rearrange("c n d -> n c d"), writes=['skr'])
    pst = K.ps('pst', [128, 4, 128])
    K.op('dve', lambda: nc.vector.tensor_copy(identR[:], ident[:]), reads=['ident'], writes=['identR'])
    for c4 in range(4):
        for j in range(4):
            K.op('pe', lambda j=j: nc.tensor.transpose(pst[:, j, :], skr[:, c4 * 4 + j, :], ident[:]), reads=['skr', 'ident'], writes=['pst'])
        K.op('act', lambda: nc.scalar.copy(skT[:, c4 * 4:(c4 + 1) * 4, :], pst[:]), reads=['pst'], writes=['skT'])
    K.pop()
    iota16 = K.sb('iota16', [128, 16])
    K.dma('sp', iota16[:], D['iota16'], writes=['iota16'])
    xt = [K.sb(f'pxt{i}', [128, 1024]) for i in range(1)]
    h2 = K.sb('ph2', [128, 1024])
    tmp = K.sb('junkP', [128, 1024])
    scr = (K.sb('pss', [128, 1]), K.sb('prs', [128, 1]), tmp)
    ptr = K.ps('pptr', [128, 1024])
    h2T = K.sb('ph2T', [128, 8, 128])
    pq = [K.ps(f'ppq{i}', [128, 4, 128]) for i in range(2)]
    qT = K.sb('pqT', [128, 16, 128])
    psc = [K.ps(f'ppsc{i}', [128, 4, 128]) for i in range(1)]
    pacc = [K.ps(f'ppacc{i}', [128, 512]) for i in range(2)]
    vsc = [K.sb(f'pvsc{i}', [128, 1024], MM) for i in range(2)]
    s = K.sb('ps_s', [128, 16, 128])
    s2 = K.sb('ps_s2', [128, 16, 128])
    sv = K.sb('ps_sv', [128, 16, 16])
    si = K.sb('ps_si', [128, 16, 16], U32)
    sif = K.sb('ps_sif', [128, 16, 16])
    cand = s[:].rearrange("p a n -> p (a n)").rearrange("p (h c) -> p h c", c=256)
    cand2 = s2[:].rearrange("p a n -> p (a n)").rearrange("p (h c) -> p h c", c=256)
    best = K.sb('ps_best', [128, 8, 16])
    pos = K.sb('ps_pos', [128, 8, 16], U32)
    pa = K.sb('ps_pa', [128, 8, 16], U32)
    pbb = K.sb('ps_pb', [128, 8, 16], U32)
    paf = K.sb('ps_paf', [128, 8, 16])
    pbf = K.sb('ps_pbf', [128, 8, 16])
    oh = K.sb('ps_oh', [128, 8, 16, 16])
    ii = K.sb('ps_ii', [128, 8, 16])
    jj = K.sb('ps_jj', [128, 8, 16])
    eidx = K.sb('ps_eidx', [128, 128], I32)
    gg = K.sb('ps_g', [128, 8, 16])
    gs = K.sb('ps_gs', [128, 8])
    act = K.sb('ps_act', [128, 128])
    wgt = K.sb('ps_wgt', [128, 128])
    gtmp = K.sb('ps_gtmp', [128, 128])
    NG = 10
    gb = [K.sb(f'pgb{i}', [128, 2048], BF16) for i in range(NG)]
    xo = K.sb('pxo', [128, 1024])
    ng = 0
    nq_ = 0
    tiles = list(range(cfg.TT)) if with_ctx else list(range(CT, cfg.TT))
    split = final and PAIR_SPLIT
    if split:
        tiles = list(range(CT, CT + cfg.NT // 2))
        ridx = K.sb('ridx', [128, cfg.NT // 2], I32)
        K.dma('sp', ridx[:], D['rowidx'], writes=['ridx'])
    T1 = ['pk_all']
    for tt in tiles:
        i = 0
        w = 'c' if tt < CT else 'l'
        rows = slice(tt * 128, (tt + 1) * 128)
        if split:
            j_ = tt - CT
            K.dma('pool', None, None, reads=['ridx'], writes=[f'pxt{i}'], fn=lambda: nc.gpsimd.indirect_dma_start(
                out=xt[i][:], out_offset=None, in_=D['xres'], in_offset=bass.IndirectOffsetOnAxis(ap=ridx[:, j_:j_ + 1], axis=0)))
        else:
            K.dma('sp', xt[i][:], D['xres'][rows, :], writes=[f'pxt{i}'])
        norm_mod(K, nc, xt[i], f'pxt{i}', h2, 'ph2', bc[(w, 3)], f'bc{w}3', bc[(w, 4)], f'bc{w}4', scr, 'P')
        for k in range(8):
            K.op('pe', lambda k=k: nc.tensor.transpose(ptr[:, k * 128:(k + 1) * 128], h2[:, k * 128:(k + 1) * 128], ident[:]),
                 reads=['ph2', 'ident'], writes=['pptr'])
        K.op('act', lambda: nc.scalar.copy(h2T[:], ptr[:].rearrange("p (k t) -> p k t", k=8)), reads=['pptr'], writes=['ph2T'])
        for c4 in range(4):
            p = pq[nq_ % 2]
            pk = f'ppq{nq_ % 2}'
            nq_ += 1
            for j in range(4):
                hx = c4 * 4 + j
                for k in range(8):
                    K.op('pe', lambda j=j, k=k, hx=hx: nc.tensor.matmul(p[:, j, :], wq[:, k, hx * 128:(hx + 1) * 128], h2T[:, k, :], start=(k == 0), stop=(k == 7)),
                         reads=['wq', 'ph2T'], writes=[pk])
            K.op('act', lambda: nc.scalar.copy(qT[:, c4 * 4:(c4 + 1) * 4, :], p[:]), reads=[pk], writes=[('pqT', c4)])
        for c4 in range(4):
            p = psc[0]
            pk = 'ppsc0'
            for j in range(4):
                hx = c4 * 4 + j
                K.op('pe', lambda j=j, hx=hx: nc.tensor.matmul(p[:, j, :], qT[:, hx, :], skT[:, hx, :], start=True, stop=True),
                     reads=[('pqT', c4), 'skT'], writes=[pk])
            K.op('act', lambda: nc.scalar.copy(s[:, c4 * 4:(c4 + 1) * 4, :], p[:]), reads=[pk], writes=[('ps_s', c4)])
        for hx in range(16):
            sk_ = ('ps_s', hx // 4)
            K.op('dve', lambda: nc.vector.max(out=sv[:, hx, 0:8], in_=s[:, hx, :]), reads=[sk_], writes=T1)
            K.op('dve', lambda: nc.vector.max_index(out=si[:, hx, 0:8], in_max=sv[:, hx, 0:8], in_values=s[:, hx, :]), reads=[sk_] + T1, writes=T1)
            K.op('dve', lambda: nc.vector.match_replace(out=s2[:, hx, :], in_to_replace=sv[:, hx, 0:8], in_values=s[:, hx, :], imm_value=-1e30), reads=[sk_] + T1, writes=T1)
            K.op('dve', lambda: nc.vector.max(out=sv[:, hx, 8:16], in_=s2[:, hx, :]), reads=T1, writes=T1)
            K.op('dve', lambda: nc.vector.max_index(out=si[:, hx, 8:16], in_max=sv[:, hx, 8:16], in_values=s2[:, hx, :]), reads=T1, writes=T1)

        T2 = T1 + [('ps_s', c) for c in range(4)]

        def dv(fn):
            K.op('dve', fn, reads=T2, writes=T2)
        svv = sv[:].rearrange("p (h x) a -> p h x a", x=2)
        cv = cand.rearrange("p h (a b) -> p h a b", b=16)
        dv(lambda: nc.vector.tensor_tensor(cv, svv[:, :, 0, :].unsqueeze(3).to_broadcast([128, 8, 16, 16]),
                                           svv[:, :, 1, :].unsqueeze(2).to_broadcast([128, 8, 16, 16]), ALU.add))
        for h in range(8):
            dv(lambda: nc.vector.max(out=best[:, h, 0:8], in_=cand[:, h, :]))
            dv(lambda: nc.vector.max_index(out=pos[:, h, 0:8], in_max=best[:, h, 0:8], in_values=cand[:, h, :]))
            dv(lambda: nc.vector.match_replace(out=cand2[:, h, :], in_to_replace=best[:, h, 0:8], in_values=cand[:, h, :], imm_value=-1e30))
            dv(lambda: nc.vector.max(out=best[:, h, 8:16], in_=cand2[:, h, :]))
            dv(lambda: nc.vector.max_index(out=pos[:, h, 8:16], in_max=best[:, h, 8:16], in_values=cand2[:, h, :]))
        dv(lambda: nc.vector.tensor_tensor(gg[:], best[:], best[:, :, 0:1].to_broadcast([128, 8, 16]), ALU.subtract))
        K.op('act', lambda: nc.scalar.activation(gg[:], gg[:], AF.Exp), reads=T1, writes=T1)
        dv(lambda: nc.vector.reduce_sum(gs[:], gg[:], axis=AX.X))
        dv(lambda: nc.vector.reciprocal(gs[:], gs[:]))
        dv(lambda: nc.vector.tensor_tensor(gg[:], gg[:], gs[:].unsqueeze(2).to_broadcast([128, 8, 16]), ALU.mult))
        dv(lambda: nc.vector.tensor_scalar(pa[:], pos[:], 4, None, ALU.logical_shift_right))
        dv(lambda: nc.vector.tensor_scalar(pbb[:], pos[:], 15, None, ALU.bitwise_and))
        dv(lambda: nc.vector.tensor_copy(paf[:], pa[:]))
        dv(lambda: nc.vector.tensor_copy(pbf[:], pbb[:]))
        dv(lambda: nc.vector.tensor_copy(sif[:], si[:]))
        sfv = sif[:].rearrange("p (h x) a -> p h x a", x=2)
        io_b = iota16[:].unsqueeze(1).unsqueeze(1).to_broadcast([128, 8, 16, 16])
        for (pf, xsel, dst) in ((paf, 0, ii), (pbf, 1, jj)):
            dv(lambda: nc.vector.tensor_tensor(oh[:], pf[:].unsqueeze(3).to_broadcast([128, 8, 16, 16]), io_b, ALU.is_equal))
            dv(lambda: nc.vector.tensor_tensor(oh[:], oh[:], sfv[:, :, xsel, :].unsqueeze(2).to_broadcast([128, 8, 16, 16]), ALU.mult))
            dv(lambda: nc.vector.reduce_sum(dst[:], oh[:], axis=AX.X))
        dv(lambda: nc.vector.scalar_tensor_tensor(ii[:], ii[:], 128.0, jj[:], ALU.mult, ALU.add))
        dv(lambda: nc.vector.tensor_copy(eidx[:], ii[:].rearrange("p h k -> p (h k)")))
        GRP = 4
        ggf = gg[:].rearrange("p h k -> p (h k)")
        for g0 in range(0, 128, GRP):
            held = []
            for slot in range(g0, g0 + GRP):
                b_ = gb[ng % NG]
                bk = f'pgb{ng % NG}'
                ng += 1
                held.append((b_, bk))
                K.dma('pool', None, None, reads=T1, writes=[bk], fn=lambda: nc.gpsimd.indirect_dma_start(
                    out=b_[:], out_offset=None, in_=D[f'uv{l}'], in_offset=bass.IndirectOffsetOnAxis(ap=eidx[:, slot:slot + 1], axis=0)))
                K.op('dve', lambda: nc.vector.scalar_tensor_tensor(tmp[:], b_[:, 0:1024], 1.0, h2[:], ALU.mult, ALU.mult, accum_out=act[:, slot:slot + 1]),
                     reads=[bk, 'ph2'], writes=['junkP', 'ps_act'])
            sl = slice(g0, g0 + GRP)
            gelu_tanh(K, nc, None, wgt[:, sl], act[:, sl], 'ps_act', 'ps_wgt', gtmp[:, sl], 'ps_gtmp')
            K.op('dve', lambda: nc.vector.tensor_tensor(wgt[:, sl], wgt[:, sl], ggf[:, sl], ALU.mult), reads=['ps_wgt'] + T1, writes=['ps_wgt'])
            for si_, slot in enumerate(range(g0, g0 + GRP)):
                b_, bk = held[si_]
                vi = slot % 2
                K.op('act', lambda: nc.scalar.activation(vsc[vi][:], b_[:, 1024:2048], AF.Copy, scale=wgt[:, slot:slot + 1]), reads=[bk, 'ps_wgt'], writes=[f'pvsc{vi}'])
                for hh in range(2):
                    K.op('pe', lambda hh=hh: nc.tensor.matmul(pacc[hh][:], identR[:], vsc[vi][:, hh * 512:(hh + 1) * 512], start=(slot == 0), stop=(slot == 127)),
                         reads=['identR', f'pvsc{vi}'], writes=[f'ppacc{hh}'])
        for hh in range(2):
            K.op('dve', lambda hh=hh: nc.vector.tensor_tensor(tmp[:, hh * 512:(hh + 1) * 512], pacc[hh][:], bc[(w, 5)][:, hh * 512:(hh + 1) * 512], ALU.mult),
                 reads=[f'ppacc{hh}', f'bc{w}5'], writes=['junkP'])
        K.op('dve', lambda: nc.vector.tensor_tensor(xo[:], xt[i][:], tmp[:], ALU.add), reads=['junkP', f'pxt{i}'], writes=['pxo'])
        if not final:
            K.dma('sp', D['xres'][rows, :], xo[:], reads=['pxo'], writes=[('xres', tt)])
        else:
            ss, rs, junk = scr
            K.op('act', lambda: nc.scalar.activation(junk[:], xo[:], AF.Square, accum_out=ss[:]), reads=['pxo'], writes=['junkP', 'ssP'])
            K.op('dve', lambda: nc.vector.tensor_scalar(rs[:], ss[:], 1.0 / 1024.0, 1e-6, ALU.mult, ALU.add), reads=['ssP'], writes=['rsP'])
            K.op('act', lambda: nc.scalar.activation(rs[:], rs[:], AF.Sqrt), reads=['rsP'], writes=['rsP'])
            K.op('dve', lambda: nc.vector.reciprocal(rs[:], rs[:]), reads=['rsP'], writes=['rsP'])
            K.op('dve', lambda: nc.vector.scalar_tensor_tensor(tmp[:], xo[:], rs[:], fg[:], ALU.mult, ALU.mult), reads=['pxo', 'rsP', 'fg'], writes=['junkP'])
            K.dma('sp', D['out'][(tt - CT) * 128:(tt - CT + 1) * 128, :], tmp[:], reads=['junkP'], writes=[('out', tt)])
        K.maybe_barrier()
    K.pop()


def stage_tables(K, nc, D, cfg):
    K.push()
    src = [K.sb(f'tbs{i}', [128, 4, 1024]) for i in range(3)]
    dst = [K.sb(f'tbd{i}', [128, 4, 1024], BF16) for i in range(3)]
    n = 0
    for l in range(cfg.depth):
        for nm_s, nm_d, c0 in ((f'peer_u{l}', f'uv{l}', 0), (f'peer_v{l}', f'uv{l}', 1024)):
            for r0 in range(0, 16384, 512):
                i = n % 3
                n += 1
                K.dma('sp', src[i][:], D[nm_s][r0:r0 + 512, :].rearrange("(p j) c -> p j c", j=4), writes=[f'tbs{i}'])
                if i == 0:
                    K.op('dve', lambda: nc.vector.tensor_copy(dst[i][:], src[i][:]), reads=[f'tbs{i}'], writes=[f'tbd{i}'])
                elif i == 1:
                    K.op('pool', lambda: nc.gpsimd.tensor_copy(dst[i][:], src[i][:]), reads=[f'tbs{i}'], writes=[f'tbd{i}'])
                else:
                    K.op('act', lambda: nc.scalar.copy(dst[i][:], src[i][:]), reads=[f'tbs{i}'], writes=[f'tbd{i}'])
                K.dma('act', D[nm_d][r0:r0 + 512, c0:c0 + 1024].rearrange("(p j) c -> p j c", j=4), dst[i][:], reads=[f'tbd{i}'], writes=[(nm_d, r0, c0)])
    K.pop()


def tables_iter(K, nc, D, cfg, src, dst):
    n = 0
    for l in range(cfg.depth):
        for nm_s, nm_d, c0 in ((f'peer_u{l}', f'uv{l}', 0), (f'peer_v{l}', f'uv{l}', 1024)):
            for r0 in range(0, 16384, 512):
                i = n % len(src)
                n += 1
                K.dma('sp', src[i][:], D[nm_s][r0:r0 + 512, :].rearrange("(p j) c -> p j c", j=4), writes=[f'tbs{i}'])
                K.op('dve', lambda: nc.vector.tensor_copy(dst[i][:], src[i][:]), reads=[f'tbs{i}'], writes=[f'tbd{i}'])
                K.dma('pool', D[nm_d][r0:r0 + 512, c0:c0 + 1024].rearrange("(p j) c -> p j c", j=4), dst[i][:], reads=[f'tbd{i}'], writes=[(nm_d, r0, c0)])
                yield


ALL_STAGES = ('inproj', 'prepass', 'diff', 'na', 'ssm', 'merge', 'peer')
_NC_CACHE = {}


def kernel(**inputs):
    inp = {k: np.asarray(v) for k, v in inputs.items()}
    B, L, _ = inp['x'].shape
    depth = inp['ada_w'].shape[0]
    key = (L, depth)
    if key not in _NC_CACHE:
        _NC_CACHE[key] = build(L, depth, stages=ALL_STAGES)
    nc = _NC_CACHE[key]
    n_cores = 8
    if PAIR_SPLIT:
        in_maps = []
        for b in range(B):
            m0 = core_inputs(inp, b, L, depth, 0)
            m1 = dict(m0)
            m1['rowidx'] = core_inputs_rowidx(L, 1)
            in_maps += [m0, m1]
        res = run_bass_kernel_spmd(nc, in_maps, core_ids=list(range(n_cores)))
        out = np.stack([np.concatenate([res.results[2 * b]['out'], res.results[2 * b + 1]['out']], 0) for b in range(B)], 0)
    else:
        maps = [core_inputs(inp, b, L, depth) for b in range(B)]
        in_maps = [maps[i % B] for i in range(n_cores)]
        res = run_bass_kernel_spmd(nc, in_maps, core_ids=list(range(n_cores)))
        out = np.stack([res.results[b]['out'] for b in range(B)], 0)
    return out.astype(np.float32)


def core_inputs_rowidx(L, half):
    nt2 = (L // 128) // 2
    return np.ascontiguousarray((CT * 128 + half * (L // 2) + np.arange(nt2)[None, :] * 128 + np.arange(128)[:, None]).astype(np.int32))
```
